# Optimizing a Trainium2 kernel written in Bass

```python
import math
import jax, jax.numpy as jnp
from jax import lax
import numpy as np

D_MODEL = 2048
BATCH = 4
SEQ = 4096
DEPTH = 1

N_META = 16
D_CONV = 2048
CONV_K = 3
D_POOL = 1024
POOL_WINDOWS = (2, 4, 8, 16)
N_POOL_GROUPS = len(POOL_WINDOWS)
POOL_GROUP_IN = D_POOL // N_POOL_GROUPS
POOL_GROUP_OUT = D_MODEL // N_POOL_GROUPS
N_BRANCH = 2
D_IN_PROJ = 3 * D_CONV + D_POOL + N_BRANCH * D_MODEL
N_EXPERTS = 32
TOP_K = 4
D_EXPERT = 2048
SWIGLU_LIMIT = 7.0
SWIGLU_ALPHA = 1.702
LN_EPS = 1e-5
DEEPNORM_ALPHA = (2.0 * DEPTH) ** 0.25
DEEPNORM_BETA = (8.0 * DEPTH) ** -0.25

kernel_name = "hybrid_conv_pool_moe_deepnorm_layer"


def layer_norm(x, g, b):
    xf = x.astype(jnp.float32)
    mu = jnp.mean(xf, axis=-1, keepdims=True)
    var = jnp.mean(jnp.square(xf - mu), axis=-1, keepdims=True)
    y = (xf - mu) * lax.rsqrt(var + LN_EPS) * g.astype(jnp.float32) + b.astype(jnp.float32)
    return y.astype(x.dtype)


def causal_short_conv(z, w):
    L = z.shape[1]
    zp = jnp.pad(z, ((0, 0), (CONV_K - 1, 0), (0, 0)))
    y = w[0] * zp[:, 0:L]
    for k in range(1, CONV_K):
        y = y + w[k] * zp[:, k:k + L]
    return y


def multiscale_pool(v, pool_w, pool_scale):
    B, L, _ = v.shape
    vf = v.astype(jnp.float32).reshape(B, L, N_POOL_GROUPS, POOL_GROUP_IN)
    cs = jnp.concatenate([jnp.zeros_like(vf[:, :1]), jnp.cumsum(vf, axis=1)], axis=1)
    t = jnp.arange(L, dtype=jnp.int32)[:, None]
    win = jnp.asarray(POOL_WINDOWS, dtype=jnp.int32)[None, :]
    lo = jnp.maximum(t + 1 - win, 0)
    cs_lo = cs[:, lo, jnp.arange(N_POOL_GROUPS)[None, :], :]
    count = (t + 1 - lo).astype(jnp.float32)
    pooled = (cs[:, 1:] - cs_lo) / count[None, :, :, None] - vf
    pooled = pooled.astype(v.dtype)
    y = jnp.einsum('blgc,gcd->blgd', pooled, pool_w)
    return y.reshape(B, L, D_MODEL) * pool_scale


def token_mixer(h, w_in, conv_w, w_a_out, pool_w, pool_scale, w_o):
    proj = jnp.einsum('bld,de->ble', h, w_in)
    b_gate, c_gate, u, v, gates = jnp.split(
        proj, [D_CONV, 2 * D_CONV, 3 * D_CONV, 3 * D_CONV + D_POOL], axis=-1)
    y_a = jnp.einsum('blc,cd->bld', b_gate * causal_short_conv(c_gate * u, conv_w), w_a_out)
    y_b = multiscale_pool(v, pool_w, pool_scale)
    g = jax.nn.sigmoid(gates)
    merged = g[..., :D_MODEL] * y_a + g[..., D_MODEL:] * y_b
    return jnp.einsum('bld,de->ble', merged, w_o)


def moe(h, router_w, router_b, w_gate_up, b_gate_up, w_down, b_down):
    B, L, D = h.shape
    tok = h.reshape(B * L, D)
    logits = (tok @ router_w + router_b).astype(jnp.float32)
    top_v, top_i = lax.top_k(logits, TOP_K)
    wts = jax.nn.softmax(top_v, axis=-1)
    flat_e = top_i.reshape(-1)
    order = jnp.argsort(flat_e)
    e_sorted = flat_e[order]
    tok_idx = order // TOP_K
    xs = tok[tok_idx]
    group_sizes = jnp.bincount(flat_e, length=N_EXPERTS).astype(jnp.int32)
    gu = lax.ragged_dot(xs, w_gate_up, group_sizes) + b_gate_up[e_sorted]
    gate = jnp.minimum(gu[:, :D_EXPERT], SWIGLU_LIMIT)
    lin = jnp.clip(gu[:, D_EXPERT:], -SWIGLU_LIMIT, SWIGLU_LIMIT)
    act = (lin + 1.0) * gate * jax.nn.sigmoid(SWIGLU_ALPHA * gate)
    out = lax.ragged_dot(act, w_down, group_sizes) + b_down[e_sorted]
    out = out * wts.reshape(-1)[order][:, None].astype(out.dtype)
    y = jnp.zeros_like(tok).at[tok_idx].add(out)
    return y.reshape(B, L, D)


def setup_inputs(seed: int = 0) -> dict:
    key = jax.random.key(seed)
    ks = jax.random.split(key, 20)
    nrm = lambda k, shape, s: jax.random.normal(k, shape, jnp.float32) * s
    Lyr = DEPTH
    return {
        "x": nrm(ks[0], (BATCH, SEQ, D_MODEL), 1.0),
        "meta_tokens": nrm(ks[1], (N_META, D_MODEL), 1.0),
        "ln_in_g": 1.0 + nrm(ks[2], (D_MODEL,), 0.02),
        "ln_in_b": nrm(ks[3], (D_MODEL,), 0.02),
        "w_in": nrm(ks[4], (Lyr, D_MODEL, D_IN_PROJ), D_MODEL ** -0.5),
        "conv_w": nrm(ks[5], (Lyr, CONV_K, D_CONV), CONV_K ** -0.5),
        "w_a_out": nrm(ks[6], (Lyr, D_CONV, D_MODEL), D_CONV ** -0.5 * DEEPNORM_BETA),
        "pool_w": nrm(ks[7], (Lyr, N_POOL_GROUPS, POOL_GROUP_IN, POOL_GROUP_OUT), POOL_GROUP_IN ** -0.5 * DEEPNORM_BETA),
        "pool_scale": 1.0 + nrm(ks[8], (Lyr, D_MODEL), 0.02),
        "w_o": nrm(ks[9], (Lyr, D_MODEL, D_MODEL), D_MODEL ** -0.5 * DEEPNORM_BETA),
        "ln1_g": 1.0 + nrm(ks[10], (Lyr, D_MODEL), 0.02),
        "ln1_b": nrm(ks[11], (Lyr, D_MODEL), 0.02),
        "router_w": nrm(ks[12], (Lyr, D_MODEL, N_EXPERTS), D_MODEL ** -0.5),
        "router_b": nrm(ks[13], (Lyr, N_EXPERTS), 0.01),
        "w_gate_up": nrm(ks[14], (Lyr, N_EXPERTS, D_MODEL, 2 * D_EXPERT), D_MODEL ** -0.5),
        "b_gate_up": nrm(ks[15], (Lyr, N_EXPERTS, 2 * D_EXPERT), 0.02),
        "w_down": nrm(ks[16], (Lyr, N_EXPERTS, D_EXPERT, D_MODEL), D_EXPERT ** -0.5 * DEEPNORM_BETA),
        "b_down": nrm(ks[17], (Lyr, N_EXPERTS, D_MODEL), 0.02),
        "ln2_g": 1.0 + nrm(ks[18], (Lyr, D_MODEL), 0.02),
        "ln2_b": nrm(ks[19], (Lyr, D_MODEL), 0.02),
    }


def reference(x, meta_tokens, ln_in_g, ln_in_b, w_in, conv_w, w_a_out, pool_w, pool_scale, w_o,
              ln1_g, ln1_b, router_w, router_b, w_gate_up, b_gate_up, w_down, b_down, ln2_g, ln2_b):
    B = x.shape[0]
    meta = jnp.broadcast_to(meta_tokens[None].astype(x.dtype), (B, N_META, D_MODEL))
    h = jnp.concatenate([meta, x], axis=1)
    h = layer_norm(h, ln_in_g, ln_in_b)
    for i in range(DEPTH):
        mix = token_mixer(h, w_in[i], conv_w[i], w_a_out[i], pool_w[i], pool_scale[i], w_o[i])
        h = layer_norm(DEEPNORM_ALPHA * h + mix, ln1_g[i], ln1_b[i])
        ffn = moe(h, router_w[i], router_b[i], w_gate_up[i], b_gate_up[i], w_down[i], b_down[i])
        h = layer_norm(DEEPNORM_ALPHA * h + ffn, ln2_g[i], ln2_b[i])
    return h[:, N_META:]
```

```python
import numpy as np
from contextlib import ExitStack
import concourse.bass as bass
import concourse.mybir as mybir
from concourse.bass_utils import run_bass_kernel_spmd

F32 = mybir.dt.float32
BF16 = mybir.dt.bfloat16
U32 = mybir.dt.uint32
I32 = mybir.dt.int32
AF = mybir.ActivationFunctionType
ALU = mybir.AluOpType
AX = mybir.AxisListType

D = 2048
KC = 16
DIN = 11264
NMETA = 16
HALO = 16
TOPK = 4
LN_EPS = 1e-5
ALPHA = 2.0 ** 0.25
LIMIT = 7.0
SW_ALPHA = 1.702
WCOLS = 256
NWR = 5


class Buf:
    __slots__ = ("name", "w", "r")

    def __init__(self, name):
        self.name = name
        self.w = None
        self.r = {}


class Sched:
    def __init__(self, nc, es):
        self.nc = nc
        self.es = es
        self.ops = {k: [] for k in ("pe", "act", "dve", "pool", "sp")}
        self.sem = {k: es.enter_context(nc.semaphore("s_" + k)) for k in ("pe", "act", "dve", "poolc")}
        self.cnt = {k: 0 for k in self.sem}
        self.nds = 8
        self.dsem = {q: [es.enter_context(nc.semaphore(f"d_{q}{i}")) for i in range(self.nds)] for q in ("sp", "pool")}
        self.dcnt = {q: [0] * self.nds for q in ("sp", "pool")}
        self.dnext = {q: 0 for q in ("sp", "pool")}

    def _deps(self, reads, writes):
        deps = []
        for b in reads:
            if b.w is not None:
                deps.append(b.w)
        for b in writes:
            if b.w is not None:
                deps.append(b.w)
            deps.extend(b.r.values())
        return deps

    def _commit(self, tok, reads, writes):
        for b in writes:
            b.w = tok
            b.r = {}
        for b in reads:
            if b in writes:
                continue
            s, v = tok
            if b.r.get(id(s), (None, 0))[1] < v:
                b.r[id(s)] = (s, v)

    def op(self, eng, fn, reads=(), writes=()):
        skey = "poolc" if eng == "pool" else eng
        deps = self._deps(reads, writes)
        self.cnt[skey] += 1
        tok = (self.sem[skey], self.cnt[skey])
        self.ops[eng].append((fn, deps, tok, 1))
        self._commit(tok, reads, writes)
        return tok

    def dma(self, q, fn, reads=(), writes=()):
        deps = self._deps(reads, writes)
        i = self.dnext[q]
        self.dnext[q] = (i + 1) % self.nds
        sem = self.dsem[q][i]
        if self.dcnt[q][i] > 0:
            deps.append((sem, self.dcnt[q][i]))
        self.dcnt[q][i] += 16
        tok = (sem, self.dcnt[q][i])
        self.ops[q].append((fn, deps, tok, 16))
        self._commit(tok, reads, writes)
        return tok

    def emit(self, eng, e, final_waits=()):
        waited = {}
        for fn, deps, tok, inc in self.ops[eng]:
            for s, v in deps:
                k = id(s)
                if waited.get(k, 0) < v:
                    e.wait_ge(s, v)
                    waited[k] = v
            ins = fn(e)
            ins.then_inc(tok[0], inc)
        for s, v in final_waits:
            e.wait_ge(s, v)


class _Stop(Exception):
    pass


def build_nc(T, NE, CAP, debug=False, upto="C", cutv=0):
    assert T % 512 == 0 and CAP % 128 == 0 and CAP <= 512
    NT = T // 128
    NST = CAP // 128
    NB = T // 512
    nc = bass.Bass("TRN2", target_bir_lowering=False)

    def din(name, shape, dt=F32):
        return nc.dram_tensor(name, shape, dt, kind="ExternalInput").ap()

    xin = din("xin", [T + HALO, D])
    w_in = din("w_in", [D, DIN])
    w_a = din("w_a", [D, D])
    w_o = din("w_o", [D, D])
    pool_w = din("pool_w", [1024, 512])
    router_w = din("router_w", [D, NE])
    w_gu = din("w_gu", [NE * D, 2 * D])
    w_dn = din("w_dn", [NE * D, D])
    b_dn = din("b_dn", [NE, D])
    NF = 8 * 16 + 2 * NE * 16
    fvec = din("fvec", [128, NF])
    NR = 2 * D + NE
    rvec = din("rvec", [128, NR])
    out = nc.dram_tensor("out", [T, D], F32, kind="ExternalOutput").ap()
    import os as _os
    SCR = _os.environ.get("SCRKIND", "Internal")
    h1f_d = nc.dram_tensor("h1f_d", [T, D], F32, kind=SCR).ap()
    h1b_d = nc.dram_tensor("h1b_d", [T, D], BF16, kind=SCR).ap()
    contrib = nc.dram_tensor("contrib", [NE * CAP, D], F32, kind=SCR).ap()
    dbg = {}
    if debug:
        dbg["h1"] = nc.dram_tensor("dbg_h1", [T, D], F32, kind="ExternalOutput").ap()
        dbg["route"] = nc.dram_tensor("dbg_route", [128, NT * 8], F32, kind="ExternalOutput").ap()
        dbg["za"] = nc.dram_tensor("dbg_za", [128, KC * 512], BF16, kind="ExternalOutput").ap()
        dbg["pl"] = nc.dram_tensor("dbg_pl", [128, 8 * 512], BF16, kind="ExternalOutput").ap()
        dbg["mg"] = nc.dram_tensor("dbg_mg", [128, KC * 512], BF16, kind="ExternalOutput").ap()
        dbg["h0b"] = nc.dram_tensor("dbg_h0b", [128, KC * 512], BF16, kind="ExternalOutput").ap()

    FO = dict(lng=0, lnb=16, cw0=32, cw1=48, cw2=64, psc=80, l1g=96, l1b=112, bg=128, bl=128 + NE * 16)

    with ExitStack() as es:
        S = Sched(nc, es)

        def sb(name, shape, dt):
            return es.enter_context(nc.sbuf_tensor(name, shape, dt))

        fv = sb("fv", [128, NF], F32)
        ident = sb("ident", [128, 128], F32)
        ones_f = sb("ones_f", [128, 128], F32)
        ones_b = sb("ones_b", [128, 128], BF16)
        triu_b = sb("triu_b", [128, 128], BF16)
        iota_i = sb("iota_i", [128, 512], I32)
        iota_f = sb("iota_f", [128, 512], F32)
        pidx_i = sb("pidx_i", [128, 1], I32)
        pidx_f = sb("pidx_f", [128, 1], F32)
        rw = sb("rw", [128, KC, NE], F32)
        rb = sb("rb", [128, NE], F32)
        maskb = sb("maskb", [128, NT, NE], BF16)
        posm = sb("posm", [128, NT, NE], F32)
        w4 = sb("w4", [128, NT, 4], F32)
        rows = sb("rows", [128, NT, 4], U32)
        B_fv, B_const, B_rw = Buf("fv"), Buf("const"), Buf("rw")
        B_maskb = [Buf(f"maskb{i}") for i in range(NT)]
        B_posm = [Buf(f"posm{i}") for i in range(NT)]
        B_w4 = [Buf(f"w4{i}") for i in range(NT)]
        B_rows = [Buf(f"rows{i}") for i in range(NT)]

        S.dma("sp", lambda e: e.dma_start(out=fv[:], in_=fvec[:, :]), writes=[B_fv])
        S.dma("sp", lambda e: e.dma_start(out=rw[:], in_=router_w.rearrange("(k p) n -> p k n", p=128)), writes=[B_rw])
        S.dma("sp", lambda e: e.dma_start(out=rb[:], in_=rvec[:, 2 * D:2 * D + NE]), writes=[B_rw])
        S.op("pool", lambda e: e.iota(iota_i[:], pattern=[[1, 512]], base=0, channel_multiplier=0), writes=[B_const])
        S.op("pool", lambda e: e.iota(pidx_i[:], pattern=[[1, 1]], base=0, channel_multiplier=1), writes=[B_const])
        S.op("dve", lambda e: e.tensor_copy(out=iota_f[:], in_=iota_i[:]), reads=[B_const], writes=[B_const])
        S.op("dve", lambda e: e.tensor_copy(out=pidx_f[:], in_=pidx_i[:]), reads=[B_const], writes=[B_const])
        S.op("dve", lambda e: e.tensor_scalar(out=ident[:], in0=iota_f[:, 0:128], scalar1=pidx_f[:, 0:1], scalar2=None,
                                              op0=ALU.is_equal), reads=[B_const], writes=[B_const])
        S.op("dve", lambda e: e.tensor_scalar(out=triu_b[:], in0=iota_f[:, 0:128], scalar1=pidx_f[:, 0:1], scalar2=None,
                                              op0=ALU.is_gt), reads=[B_const], writes=[B_const])
        S.op("dve", lambda e: e.memset(ones_f[:], 1.0), writes=[B_const])
        S.op("dve", lambda e: e.memset(ones_b[:], 1.0), writes=[B_const])
        S.op("dve", lambda e: e.tensor_scalar(out=fv[:, FO["bl"]:FO["bl"] + NE * 16], in0=fv[:, FO["bl"]:FO["bl"] + NE * 16],
                                              scalar1=1.0, scalar2=None, op0=ALU.add), reads=[B_fv], writes=[B_fv])

        def cut(k):
            if cutv == k:
                raise _Stop()

        def fcol(key, k):
            c = FO[key] + k
            return fv[:, c:c + 1]

        NARENA = 46592
        arena_t = sb("arena", [128, NARENA], F32)
        arena_off = [0]

        def carve(shape, dt):
            n = 1
            for d_ in shape[1:]:
                n *= d_
            nbytes = n * (2 if dt == BF16 else 4)
            nf = (nbytes + 7) // 8 * 2
            off = arena_off[0]
            assert off + nf <= NARENA, ("arena overflow", off, nf)
            arena_off[0] = off + nf
            v = arena_t[:, off:off + nf]
            if dt != F32:
                v = v.bitcast(dt)
            v = v[:, 0:n]
            if len(shape) == 3:
                v = v.rearrange("p (a b) -> p a b", a=shape[1])
            return v

        def phase_barrier(bufs):
            toks = [(S.sem[k], S.cnt[k]) for k in ("pe", "act", "dve") if S.cnt[k] > 0]
            toks += [(S.dsem[q][i], S.dcnt[q][i]) for q in ("sp", "pool") for i in range(S.nds) if S.dcnt[q][i] > 0]
            for bb in bufs:
                for tk in toks:
                    bb.r[id(tk[0])] = tk

        psum = [es.enter_context(nc.psum_tensor(f"ps{i}", [128, 512], F32)) for i in range(8)]
        B_ps = [Buf(f"ps{i}") for i in range(8)]
        ps_rr = [0]
        ps_reserved = set()

        def next_ps():
            while ps_rr[0] in ps_reserved:
                ps_rr[0] = (ps_rr[0] + 1) % 8
            i = ps_rr[0]
            ps_rr[0] = (i + 1) % 8
            return psum[i], B_ps[i]

        ring = {"bufs": [], "B": [], "rr": 0}

        def make_ring(n, elems):
            ring["bufs"] = [carve([128, elems], BF16) for _ in range(n)]
            ring["B"] = [Buf(f"wr{i}") for i in range(n)]
            ring["rr"] = 0
            return ring["B"]

        def load_w(src_ap, kk=KC, ncols=WCOLS):
            i = ring["rr"]
            ring["rr"] = (i + 1) % len(ring["bufs"])
            view = ring["bufs"][i][:, 0:kk * ncols].rearrange("p (k n) -> p k n", k=kk)
            Bw = ring["B"][i]
            S.dma("pool", lambda e: e.dma_start(out=view, in_=src_ap.rearrange("(k p) n -> p k n", p=128)), writes=[Bw])
            return view, Bw

        def mm_group(ps_ap, B_out, pairs, reads):
            def fn(e):
                ins = None
                n = len(pairs)
                for i, (l, r) in enumerate(pairs):
                    ins = e.matmul(ps_ap, lhsT=l, rhs=r, start=(i == 0), stop=(i == n - 1))
                return ins
            return S.op("pe", fn, reads=reads, writes=[B_out])

        def tr_group(B_out, items, reads):
            def fn(e):
                ins = None
                for o, i_, idn in items:
                    ins = e.transpose(o, i_, idn)
                return ins
            return S.op("pe", fn, reads=reads, writes=[B_out])

        if True:
            def sbA(name, shape, dt):
                return carve(shape, dt)

            make_ring(NWR, KC * WCOLS)
            h0f = sbA("h0f", [128, KC, 512], F32)
            h0b = sbA("h0b", [128, KC, 512], BF16)
            za = sbA("za", [128, KC, 512], BF16)
            pl = sbA("pl", [128, 8, 512], BF16)
            mg = sbA("mg", [128, KC, 512], BF16)
            xst = [sbA(f"xst{i}", [128, D], F32) for i in range(2)]
            hbst = [sbA(f"hbst{i}", [128, D], BF16) for i in range(2)]
            cuh = sbA("cuh", [128, KC, 16], F32)
            vh = sbA("vh", [128, 8, 16], F32)
            NTMP = 8
            tmp = [sbA(f"tmp{i}", [128, 528], F32) for i in range(NTMP)]
            stat = sbA("stat", [128, 4, 6], F32)
            mv = sbA("mv", [128, 8], F32)
            rt = sbA("rt", [128, 512], F32)
            rti = sbA("rti", [128, 8], U32)
            lgall = sbA("lgall", [128, 4, NE], F32)
            B_lg = [Buf(f"lg{i}") for i in range(4)]
            B_h0f = [Buf(f"h0f{k}") for k in range(KC)]
            B_h0b = [Buf(f"h0b{k}") for k in range(KC)]
            B_za = [Buf(f"za{k}") for k in range(KC)]
            B_pl = [Buf(f"pl{k}") for k in range(8)]
            B_mg = [Buf(f"mg{k}") for k in range(KC)]
            B_xst = [Buf("xst0"), Buf("xst1")]
            B_hbst = [Buf("hbst0"), Buf("hbst1")]
            B_cuh = [Buf(f"cuh{k}") for k in range(KC)]
            B_vh = [Buf(f"vh{k}") for k in range(8)]
            B_tmp = [Buf(f"tmp{i}") for i in range(NTMP)]
            B_stat, B_mv, B_rt = Buf("stat"), Buf("mv"), Buf("rt")
            wpl_t = sbA("wpl", [128, 8, 512], BF16)
            B_wpl = Buf("wpl")
            S.dma("pool", lambda e: e.dma_start(out=wpl_t, in_=pool_w.rearrange("(k p) n -> p k n", p=128)), writes=[B_wpl])
            tmp_rr = [0]

            def next_tmp():
                i = tmp_rr[0]
                tmp_rr[0] = (i + 1) % NTMP
                return tmp[i], B_tmp[i]

            ev_rr = [0]

            def evac_copy(out_ap, in_ap, reads, writes):
                ev_rr[0] ^= 1
                if ev_rr[0]:
                    return S.op("act", lambda e: e.copy(out=out_ap, in_=in_ap), reads=reads, writes=writes)
                return S.op("dve", lambda e: e.tensor_copy(out=out_ap, in_=in_ap), reads=reads, writes=writes)

            def ln_rows(x_ap, npart, B_x):
                for c in range(4):
                    S.op("dve", lambda e, c=c: e.bn_stats(out=stat[0:npart, c, :], in_=x_ap[:, c * 512:(c + 1) * 512]),
                         reads=[B_x], writes=[B_stat] if c == 0 else [B_stat])
                S.op("dve", lambda e: e.bn_aggr(out=mv[0:npart, 0:2], in_=stat[0:npart, :, :].rearrange("p a b -> p (a b)")),
                     reads=[B_stat], writes=[B_mv])
                S.op("dve", lambda e: e.tensor_scalar(out=mv[0:npart, 2:3], in0=mv[0:npart, 1:2], scalar1=LN_EPS, scalar2=None,
                                                      op0=ALU.add), reads=[B_mv], writes=[B_mv])
                S.op("act", lambda e: e.activation(out=mv[0:npart, 2:3], in_=mv[0:npart, 2:3], func=AF.Sqrt), reads=[B_mv], writes=[B_mv])
                S.op("dve", lambda e: e.reciprocal(out=mv[0:npart, 2:3], in_=mv[0:npart, 2:3]), reads=[B_mv], writes=[B_mv])
                S.op("dve", lambda e: e.tensor_scalar(out=mv[0:npart, 3:4], in0=mv[0:npart, 0:1], scalar1=mv[0:npart, 2:3], scalar2=-1.0,
                                                      op0=ALU.mult, op1=ALU.mult), reads=[B_mv], writes=[B_mv])
                S.op("act", lambda e: e.activation(out=x_ap, in_=x_ap, func=AF.Identity, bias=mv[0:npart, 3:4],
                                                   scale=mv[0:npart, 2:3]), reads=[B_mv, B_x], writes=[B_x])

            def mixer_block(n, r0, halo, tile0, prev_tail):
                nsub = max(1, n // 128)
                npart = min(n, 128)
                for sub in range(nsub):
                    xt, B_x = xst[sub % 2], B_xst[sub % 2]
                    S.dma("sp", lambda e, xt=xt, sub=sub: e.dma_start(out=xt[0:npart, :], in_=xin[r0 + sub * 128:r0 + sub * 128 + npart, :]),
                          writes=[B_x])
                    ln_rows(xt[0:npart, :], npart, B_x)
                    for q in range(4):
                        ps, B_p = next_ps()
                        items = [(ps[:, kk * npart:(kk + 1) * npart], xt[0:npart, (4 * q + kk) * 128:(4 * q + kk + 1) * 128],
                                  ident[0:npart, 0:npart]) for kk in range(4)]
                        tr_group(B_p, items, reads=[B_x, B_const])
                        evac_copy(h0f[:, 4 * q:4 * q + 4, sub * 128:sub * 128 + npart],
                                  ps[:, 0:4 * npart].rearrange("p (k t) -> p k t", k=4),
                                  reads=[B_p], writes=[B_h0f[4 * q + kk] for kk in range(4)])
                for k in range(KC):
                    S.op("act", lambda e, k=k: e.activation(out=h0b[:, k, 0:n], in_=h0f[:, k, 0:n], func=AF.Identity,
                                                            bias=fcol("lnb", k), scale=fcol("lng", k)),
                         reads=[B_h0f[k], B_fv], writes=[B_h0b[k]])
                    if not halo:
                        S.op("dve", lambda e, k=k: e.tensor_scalar(out=h0f[:, k, 0:n], in0=h0f[:, k, 0:n], scalar1=fcol("lng", k),
                                                                   scalar2=fcol("lnb", k), op0=ALU.mult, op1=ALU.add),
                             reads=[B_h0f[k], B_fv], writes=[B_h0f[k]])

                if prev_tail is not None:
                    prev_tail()
                cut(2 if halo else 4)

                def proj(wv, B_w, kk, reads_extra=()):
                    ps, B_p = next_ps()
                    pairs = [(wv[:, k, kk * 128:(kk + 1) * 128], h0b[:, k, 0:n]) for k in range(KC)]
                    mm_group(ps[:, 0:n], B_p, pairs, reads=[B_w] + B_h0b)
                    return ps, B_p

                for s in range(8):
                    if halo:
                        wb = None
                    else:
                        wb = load_w(w_in[:, 256 * s:256 * s + 256])
                    wc = load_w(w_in[:, 2048 + 256 * s:2048 + 256 * s + 256])
                    wu = load_w(w_in[:, 4096 + 256 * s:4096 + 256 * s + 256])
                    for kk in range(2):
                        i = 2 * s + kk
                        pc, B_pc = proj(wc[0], wc[1], kk)
                        pu, B_pu = proj(wu[0], wu[1], kk)
                        if halo:
                            ut, B_ut = next_tmp()
                            S.op("act", lambda e, ut=ut, pu=pu: e.copy(out=ut[:, 0:n], in_=pu[:, 0:n]), reads=[B_pu], writes=[B_ut])
                            S.op("dve", lambda e, ut=ut, pc=pc, i=i: e.tensor_tensor(out=cuh[:, i, :], in0=pc[:, 0:n], in1=ut[:, 0:n], op=ALU.mult),
                                 reads=[B_pc, B_ut], writes=[B_cuh[i]])
                            continue
                        pb, B_pb = proj(wb[0], wb[1], kk)
                        ut, B_ut = next_tmp()
                        cu, B_cu = next_tmp()
                        t1, B_t1 = next_tmp()
                        S.op("act", lambda e, ut=ut, pu=pu: e.copy(out=ut[:, 0:n], in_=pu[:, 0:n]), reads=[B_pu], writes=[B_ut])
                        S.op("act", lambda e, cu=cu, i=i: e.copy(out=cu[:, 0:16], in_=cuh[:, i, :]), reads=[B_cuh[i]], writes=[B_cu])
                        S.op("dve", lambda e, cu=cu, pc=pc, ut=ut: e.tensor_tensor(out=cu[:, 16:16 + n], in0=pc[:, 0:n], in1=ut[:, 0:n], op=ALU.mult),
                             reads=[B_pc, B_ut], writes=[B_cu])
                        S.op("act", lambda e, cu=cu, i=i: e.copy(out=cuh[:, i, :], in_=cu[:, n:n + 16]), reads=[B_cu], writes=[B_cuh[i]])
                        S.op("dve", lambda e, t1=t1, cu=cu, i=i: e.tensor_scalar(out=t1[:, 0:n], in0=cu[:, 16:16 + n], scalar1=fcol("cw2", i),
                                                                                 scalar2=None, op0=ALU.mult), reads=[B_cu, B_fv], writes=[B_t1])
                        S.op("dve", lambda e, t1=t1, cu=cu, i=i: e.scalar_tensor_tensor(out=t1[:, 0:n], in0=cu[:, 15:15 + n], scalar=fcol("cw1", i),
                                                                                        in1=t1[:, 0:n], op0=ALU.mult, op1=ALU.add),
                             reads=[B_cu, B_fv, B_t1], writes=[B_t1])
                        S.op("dve", lambda e, t1=t1, cu=cu, i=i: e.scalar_tensor_tensor(out=t1[:, 0:n], in0=cu[:, 14:14 + n], scalar=fcol("cw0", i),
                                                                                        in1=t1[:, 0:n], op0=ALU.mult, op1=ALU.add),
                             reads=[B_cu, B_fv, B_t1], writes=[B_t1])
                        S.op("dve", lambda e, t1=t1, pb=pb, i=i: e.tensor_tensor(out=za[:, i, 0:n], in0=pb[:, 0:n], in1=t1[:, 0:n], op=ALU.mult),
                             reads=[B_pb, B_t1], writes=[B_za[i]])

                if not halo:
                    cut(5)
                for g in range(4):
                    wv_ = load_w(w_in[:, 6144 + 256 * g:6144 + 256 * g + 256])
                    W = 2 << g
                    for kk in range(2):
                        c = 2 * g + kk
                        pv, B_pv = proj(wv_[0], wv_[1], kk)
                        if halo:
                            S.op("act", lambda e, pv=pv, c=c: e.copy(out=vh[:, c, :], in_=pv[:, 0:n]), reads=[B_pv], writes=[B_vh[c]])
                            continue
                        vs, B_vs = next_tmp()
                        S.op("act", lambda e, vs=vs, c=c: e.copy(out=vs[:, 0:16], in_=vh[:, c, :]), reads=[B_vh[c]], writes=[B_vs])
                        S.op("act", lambda e, vs=vs, pv=pv: e.copy(out=vs[:, 16:16 + n], in_=pv[:, 0:n]), reads=[B_pv], writes=[B_vs])
                        S.op("act", lambda e, vs=vs, c=c: e.copy(out=vh[:, c, :], in_=vs[:, n:n + 16]), reads=[B_vs], writes=[B_vh[c]])
                        cur, B_cur = vs, B_vs
                        sh = 1
                        while sh < W:
                            nx, B_nx = next_tmp()
                            S.op("dve", lambda e, nx=nx, cur=cur, sh=sh: e.tensor_tensor(out=nx[:, sh:16 + n], in0=cur[:, sh:16 + n],
                                                                                          in1=cur[:, 0:16 + n - sh], op=ALU.add),
                                 reads=[B_cur], writes=[B_nx])
                            cur, B_cur = nx, B_nx
                            sh *= 2
                        S.op("dve", lambda e, cur=cur, vs=vs, c=c, W=W: e.scalar_tensor_tensor(out=pl[:, c, 0:n], in0=cur[:, 16:16 + n], scalar=1.0 / W,
                                                                                                in1=vs[:, 16:16 + n], op0=ALU.mult, op1=ALU.subtract),
                             reads=[B_cur, B_vs], writes=[B_pl[c]])
                if halo:
                    return
                cut(6)

                wpl = (wpl_t, B_wpl)
                for jp in range(8):
                    wa = load_w(w_a[:, 256 * jp:256 * jp + 256])
                    wga = load_w(w_in[:, 7168 + 256 * jp:7168 + 256 * jp + 256])
                    wgb = load_w(w_in[:, 9216 + 256 * jp:9216 + 256 * jp + 256])
                    for kk in range(2):
                        j = 2 * jp + kk
                        g = j // 4
                        pya, B_pya = next_ps()
                        mm_group(pya[:, 0:n], B_pya, [(wa[0][:, i, kk * 128:(kk + 1) * 128], za[:, i, 0:n]) for i in range(KC)],
                                 reads=[wa[1]] + B_za)
                        pyb, B_pyb = next_ps()
                        mm_group(pyb[:, 0:n], B_pyb, [(wpl[0][:, 2 * g + c2, (j % 4) * 128:(j % 4 + 1) * 128], pl[:, 2 * g + c2, 0:n]) for c2 in range(2)],
                                 reads=[wpl[1], B_pl[2 * g], B_pl[2 * g + 1]])
                        pga, B_pga = proj(wga[0], wga[1], kk)
                        pgb, B_pgb = proj(wgb[0], wgb[1], kk)
                        sga, B_sga = next_tmp()
                        sgb, B_sgb = next_tmp()
                        S.op("act", lambda e, sga=sga, pga=pga: e.activation(out=sga[:, 0:n], in_=pga[:, 0:n], func=AF.Sigmoid), reads=[B_pga], writes=[B_sga])
                        S.op("act", lambda e, sgb=sgb, pgb=pgb: e.activation(out=sgb[:, 0:n], in_=pgb[:, 0:n], func=AF.Sigmoid), reads=[B_pgb], writes=[B_sgb])
                        S.op("dve", lambda e, sga=sga, pya=pya: e.tensor_tensor(out=sga[:, 0:n], in0=pya[:, 0:n], in1=sga[:, 0:n], op=ALU.mult),
                             reads=[B_pya, B_sga], writes=[B_sga])
                        S.op("dve", lambda e, sgb=sgb, pyb=pyb, j=j: e.scalar_tensor_tensor(out=sgb[:, 0:n], in0=pyb[:, 0:n], scalar=fcol("psc", j),
                                                                                            in1=sgb[:, 0:n], op0=ALU.mult, op1=ALU.mult),
                             reads=[B_pyb, B_sgb, B_fv], writes=[B_sgb])
                        S.op("dve", lambda e, sga=sga, sgb=sgb, j=j: e.tensor_tensor(out=mg[:, j, 0:n], in0=sga[:, 0:n], in1=sgb[:, 0:n], op=ALU.add),
                             reads=[B_sga, B_sgb], writes=[B_mg[j]])

                if debug and tile0 == 0:
                    S.dma("sp", lambda e: e.dma_start(out=dbg["za"].rearrange("p (k n) -> p k n", k=KC), in_=za), reads=B_za)
                    S.dma("sp", lambda e: e.dma_start(out=dbg["pl"].rearrange("p (k n) -> p k n", k=8), in_=pl), reads=B_pl)
                    S.dma("sp", lambda e: e.dma_start(out=dbg["mg"].rearrange("p (k n) -> p k n", k=KC), in_=mg), reads=B_mg)
                    S.dma("sp", lambda e: e.dma_start(out=dbg["h0b"].rearrange("p (k n) -> p k n", k=KC), in_=h0b), reads=B_h0b)
                cut(7)
                psum_s, B_pss = next_ps()
                psum_q, B_psq = next_ps()
                res_banks = [i for i in range(8) if psum[i] is psum_s or psum[i] is psum_q]
                ps_reserved.update(res_banks)

                def ln1_stats(j, sq, B_sq):
                    def fn_s(e):
                        return e.matmul(psum_s[:, 0:n], lhsT=ones_f[:], rhs=h0f[:, j, 0:n], start=(j == 0), stop=(j == KC - 1))

                    def fn_q(e):
                        return e.matmul(psum_q[:, 0:n], lhsT=ones_f[:], rhs=sq[:, 0:n], start=(j == 0), stop=(j == KC - 1))
                    S.op("pe", fn_s, reads=[B_h0f[j], B_const] + ([B_pss] if j else []), writes=[B_pss])
                    S.op("pe", fn_q, reads=[B_sq] + ([B_psq] if j else []), writes=[B_psq])

                pend = None
                for jp in range(8):
                    wo = load_w(w_o[:, 256 * jp:256 * jp + 256])
                    for kk in range(2):
                        j = 2 * jp + kk
                        pm, B_pm = next_ps()
                        mm_group(pm[:, 0:n], B_pm, [(wo[0][:, i, kk * 128:(kk + 1) * 128], mg[:, i, 0:n]) for i in range(KC)],
                                 reads=[wo[1]] + B_mg)
                        S.op("dve", lambda e, pm=pm, j=j: e.scalar_tensor_tensor(out=h0f[:, j, 0:n], in0=h0f[:, j, 0:n], scalar=ALPHA, in1=pm[:, 0:n],
                                                                                 op0=ALU.mult, op1=ALU.add), reads=[B_pm, B_h0f[j]], writes=[B_h0f[j]])
                        sq, B_sq = next_tmp()
                        S.op("act", lambda e, sq=sq, j=j: e.activation(out=sq[:, 0:n], in_=h0f[:, j, 0:n], func=AF.Square), reads=[B_h0f[j]], writes=[B_sq])
                        if pend is not None:
                            ln1_stats(*pend)
                        pend = (j, sq, B_sq)
                ln1_stats(*pend)
                for i_ in res_banks:
                    ps_reserved.discard(i_)
                cut(8)
                mean, B_mean = next_tmp()
                rstd, B_rstd = next_tmp()
                S.op("dve", lambda e: e.tensor_scalar(out=mean[:, 0:n], in0=psum_s[:, 0:n], scalar1=1.0 / D, scalar2=None, op0=ALU.mult),
                     reads=[B_pss], writes=[B_mean])
                S.op("dve", lambda e: e.tensor_tensor(out=rstd[:, 0:n], in0=mean[:, 0:n], in1=mean[:, 0:n], op=ALU.mult), reads=[B_mean], writes=[B_rstd])
                S.op("dve", lambda e: e.scalar_tensor_tensor(out=rstd[:, 0:n], in0=psum_q[:, 0:n], scalar=1.0 / D, in1=rstd[:, 0:n],
                                                             op0=ALU.mult, op1=ALU.subtract), reads=[B_psq, B_rstd], writes=[B_rstd])
                S.op("dve", lambda e: e.tensor_scalar(out=rstd[:, 0:n], in0=rstd[:, 0:n], scalar1=LN_EPS, scalar2=None, op0=ALU.add),
                     reads=[B_rstd], writes=[B_rstd])
                S.op("act", lambda e: e.activation(out=rstd[:, 0:n], in_=rstd[:, 0:n], func=AF.Sqrt), reads=[B_rstd], writes=[B_rstd])
                S.op("dve", lambda e: e.reciprocal(out=rstd[:, 0:n], in_=rstd[:, 0:n]), reads=[B_rstd], writes=[B_rstd])
                for j in range(KC):
                    S.op("dve", lambda e, j=j: e.tensor_tensor(out=h0f[:, j, 0:n], in0=h0f[:, j, 0:n], in1=mean[:, 0:n], op=ALU.subtract),
                         reads=[B_h0f[j], B_mean], writes=[B_h0f[j]])
                    S.op("dve", lambda e, j=j: e.tensor_tensor(out=h0f[:, j, 0:n], in0=h0f[:, j, 0:n], in1=rstd[:, 0:n], op=ALU.mult),
                         reads=[B_h0f[j], B_rstd], writes=[B_h0f[j]])
                    S.op("act", lambda e, j=j: e.activation(out=h0f[:, j, 0:n], in_=h0f[:, j, 0:n], func=AF.Identity, bias=fcol("l1b", j),
                                                            scale=fcol("l1g", j)), reads=[B_h0f[j], B_fv], writes=[B_h0f[j]])

                cut(9)
                for sub in range(nsub):
                    ti = tile0 + sub
                    xt, B_x = xst[sub % 2], B_xst[sub % 2]
                    hb, B_hb = hbst[sub % 2], B_hbst[sub % 2]
                    for q in range(4):
                        ps, B_p = next_ps()
                        items = [(ps[:, kk * 128:(kk + 1) * 128], h0f[:, 4 * q + kk, sub * 128:(sub + 1) * 128], ident[:]) for kk in range(4)]
                        tr_group(B_p, items, reads=[B_h0f[4 * q + kk] for kk in range(4)] + [B_const])
                        A5 = int(_os.environ.get("A5MODE", "9"))
                        if A5 >= 2:
                            S.op("act", lambda e, xt=xt, ps=ps, q=q: e.copy(out=xt[:, q * 512:(q + 1) * 512], in_=ps[:, :]), reads=[B_p], writes=[B_x])
                        if A5 >= 3:
                            S.op("dve", lambda e, hb=hb, xt=xt, q=q: e.tensor_copy(out=hb[:, q * 512:(q + 1) * 512], in_=xt[:, q * 512:(q + 1) * 512]),
                                 reads=[B_x], writes=[B_hb])
                    if A5 >= 4:
                        S.dma("sp", lambda e, xt=xt, ti=ti: e.dma_start(out=h1f_d[ti * 128:(ti + 1) * 128, :], in_=xt[:, :]), reads=[B_x])
                    if A5 >= 5:
                        S.dma("sp", lambda e, hb=hb, ti=ti: e.dma_start(out=h1b_d[ti * 128:(ti + 1) * 128, :], in_=hb[:, :]), reads=[B_hb])
                    if debug and A5 >= 6:
                        S.dma("sp", lambda e, xt=xt, ti=ti: e.dma_start(out=dbg["h1"][ti * 128:(ti + 1) * 128, :], in_=xt[:, :]), reads=[B_x])
                    cut(10)
                    pl_, B_plg = next_ps()
                    mm_group(pl_[:, 0:NE], B_plg, [(h0f[:, k, sub * 128:(sub + 1) * 128], rw[:, k, :]) for k in range(KC)],
                             reads=B_h0f + [B_rw])
                    S.op("dve", lambda e, pl_=pl_, sub=sub: e.tensor_tensor(out=lgall[:, sub, :], in0=pl_[:, 0:NE], in1=rb[:], op=ALU.add),
                         reads=[B_plg, B_rw], writes=[B_lg[sub]])

                def routing_tail():
                  for sub in range(nsub):
                    ti = tile0 + sub
                    lg = lgall[:, sub, :]
                    m8 = rt[:, 32:40]
                    i8f = rt[:, 40:48]
                    oh = rt[:, 64:64 + 4 * NE].rearrange("p (k e) -> p k e", k=4)
                    msk = rt[:, 192:192 + NE]
                    ex = rt[:, 224:228]
                    sm = rt[:, 228:229]
                    nm = rt[:, 229:230]
                    pos = rt[:, 256:256 + NE]
                    prod = rt[:, 288:288 + 4 * NE].rearrange("p (k e) -> p k e", k=4)
                    pk = rt[:, 416:420]
                    rowf = rt[:, 420:424]
                    R = [B_rt]
                    S.op("dve", lambda e, lg=lg: e.max(out=m8, in_=lg), reads=R + [B_lg[sub]], writes=R)
                    S.op("dve", lambda e, lg=lg: e.max_index(out=rti[:], in_max=m8, in_values=lg), reads=R + [B_lg[sub]], writes=R)
                    S.op("dve", lambda e: e.tensor_copy(out=i8f, in_=rti[:]), reads=R, writes=R)
                    for k in range(TOPK):
                        S.op("dve", lambda e, k=k: e.tensor_scalar(out=oh[:, k, :], in0=iota_f[:, 0:NE], scalar1=i8f[:, k:k + 1], scalar2=None,
                                                                   op0=ALU.is_equal), reads=R + [B_const], writes=R)
                    S.op("dve", lambda e: e.tensor_tensor(out=msk, in0=oh[:, 0, :], in1=oh[:, 1, :], op=ALU.add), reads=R, writes=R)
                    S.op("dve", lambda e: e.tensor_tensor(out=msk, in0=msk, in1=oh[:, 2, :], op=ALU.add), reads=R, writes=R)
                    S.op("dve", lambda e: e.tensor_tensor(out=msk, in0=msk, in1=oh[:, 3, :], op=ALU.add), reads=R, writes=R)
                    S.op("dve", lambda e, ti=ti: e.tensor_copy(out=maskb[:, ti, :], in_=msk), reads=R, writes=[B_maskb[ti]])
                    S.op("dve", lambda e: e.tensor_scalar(out=nm, in0=m8[:, 0:1], scalar1=-1.0, scalar2=None, op0=ALU.mult), reads=R, writes=R)
                    S.op("act", lambda e: e.activation(out=ex, in_=m8[:, 0:4], func=AF.Exp, bias=nm, scale=1.0), reads=R, writes=R)
                    S.op("dve", lambda e: e.reduce_sum(out=sm, in_=ex, axis=AX.X), reads=R, writes=R)
                    S.op("dve", lambda e: e.reciprocal(out=sm, in_=sm), reads=R, writes=R)
                    S.op("dve", lambda e, ti=ti: e.tensor_scalar(out=w4[:, ti, :], in0=ex, scalar1=sm, scalar2=None, op0=ALU.mult), reads=R, writes=[B_w4[ti]])
                    pp, B_pp = next_ps()
                    pairs = [(triu_b[:], maskb[:, ti, :])] + [(ones_b[:], maskb[:, t2, :]) for t2 in range(ti)]
                    mm_group(pp[:, 0:NE], B_pp, pairs, reads=[B_const] + B_maskb[0:ti + 1])
                    S.op("dve", lambda e, pp=pp: e.tensor_copy(out=pos, in_=pp[:, 0:NE]), reads=[B_pp], writes=R)
                    S.op("dve", lambda e, ti=ti: e.scalar_tensor_tensor(out=posm[:, ti, :], in0=pos, scalar=1.0, in1=msk, op0=ALU.add, op1=ALU.mult),
                         reads=R, writes=[B_posm[ti]])
                    S.op("dve", lambda e, ti=ti: e.tensor_scalar(out=posm[:, ti, :], in0=posm[:, ti, :], scalar1=-1.0, scalar2=None, op0=ALU.add),
                         reads=[B_posm[ti]], writes=[B_posm[ti]])
                    for k in range(TOPK):
                        S.op("dve", lambda e, k=k: e.tensor_tensor(out=prod[:, k, :], in0=oh[:, k, :], in1=pos, op=ALU.mult), reads=R, writes=R)
                    S.op("dve", lambda e: e.tensor_reduce(out=pk, in_=prod, axis=AX.X, op=ALU.add), reads=R, writes=R)
                    S.op("dve", lambda e: e.scalar_tensor_tensor(out=rowf, in0=i8f[:, 0:4], scalar=float(CAP), in1=pk, op0=ALU.mult, op1=ALU.add),
                         reads=R, writes=R)
                    S.op("dve", lambda e, ti=ti: e.tensor_copy(out=rows[:, ti, :], in_=rowf), reads=R, writes=[B_rows[ti]])
                    if debug:
                        S.dma("sp", lambda e, ti=ti: e.dma_start(out=dbg["route"][:, ti * 8:ti * 8 + 4], in_=rowf), reads=R)
                        S.dma("sp", lambda e, ti=ti: e.dma_start(out=dbg["route"][:, ti * 8 + 4:ti * 8 + 8], in_=w4[:, ti, :]), reads=[B_w4[ti]])
                return routing_tail

            try:
                cut(1)
                mixer_block(HALO, 0, True, 0, None)
                cut(3)
                tail = None
                for b in range(NB):
                    tail = mixer_block(512, HALO + 512 * b, False, 4 * b, tail)
                tail()
            except _Stop:
                upto = "A"

        if upto in ("B", "C"):
            arena_off[0] = 0

            def sbB(name, shape, dt):
                return carve(shape, dt)

            B_ringB = make_ring(4, KC * 512)
            xs = sbB("xs", [128, KC, CAP], BF16)
            act = sbB("act", [128, KC, CAP], BF16)
            sel = sbB("sel", [128, NT, CAP], BF16)
            ost = sbB("ost", [128, NST, D], F32)
            bdn = [sbB(f"bdn{i}", [128, D], BF16) for i in range(2)]
            xg = [sbB(f"xg{i}", [128, NST, D], BF16) for i in range(2)]
            idxu = [sbB(f"idxu{i}", [128, 4], U32) for i in range(2)]
            idxf = sbB("idxf", [128, 16], F32)
            tokcol = sbB("tokcol", [128, NT, 2], BF16)
            ident_b = sbB("ident_b", [128, 128], BF16)
            NTB = 8
            tb = [sbB(f"tb{i}", [128, CAP], F32) for i in range(NTB)]
            B_xs = [Buf(f"xs{k}") for k in range(KC)]
            B_act = [Buf(f"act{k}") for k in range(KC)]
            B_sel = [Buf(f"sel{i}") for i in range(NT)]
            B_ost = [Buf(f"ost{i}") for i in range(NST)]
            B_bdn = [Buf("bdn0"), Buf("bdn1")]
            B_xg = [[Buf(f"xg{i}_{st}") for st in range(NST)] for i in range(2)]
            B_idxu = [Buf("idxu0"), Buf("idxu1")]
            B_idxf, B_tok = Buf("idxf"), Buf("tokcol")
            B_tb = [Buf(f"tb{i}") for i in range(NTB)]
            tb_rr = [0]

            def next_tb():
                i = tb_rr[0]
                tb_rr[0] = (i + 1) % NTB
                return tb[i], B_tb[i]

            phase_barrier(B_ringB + B_xs + B_act + B_sel + B_ost + B_bdn + B_xg[0] + B_xg[1] + B_idxu + [B_idxf, B_tok] + B_tb)

            for ti in range(NT):
                S.op("dve", lambda e, ti=ti: e.tensor_copy(out=tokcol[:, ti, 0:1], in_=pidx_f[:, 0:1]), reads=[B_const], writes=[B_tok])
                S.op("dve", lambda e, ti=ti: e.memset(tokcol[:, ti, 1:2], float(ti)), writes=[B_tok])
            S.op("dve", lambda e: e.tensor_copy(out=ident_b, in_=ident[:]), reads=[B_const], writes=[B_tok])

            def prep_sel(ex_):
                for ti in range(NT):
                    S.op("dve", lambda e, ti=ti, ex_=ex_: e.tensor_scalar(out=sel[:, ti, :], in0=iota_f[:, 0:CAP], scalar1=posm[:, ti, ex_:ex_ + 1],
                                                                          scalar2=None, op0=ALU.is_equal),
                         reads=[B_posm[ti], B_const], writes=[B_sel[ti]])

            def prep_idx(ex_):
                bi = ex_ % 2
                for st in range(NST):
                    pi, B_pi = next_ps()
                    mm_group(pi[:, 0:2], B_pi, [(sel[:, ti, st * 128:(st + 1) * 128], tokcol[:, ti, :]) for ti in range(NT)],
                             reads=B_sel + [B_tok])
                    S.op("dve", lambda e, pi=pi, st=st: e.tensor_copy(out=idxf[:, 4 + 2 * st:6 + 2 * st], in_=pi[:, 0:2]), reads=[B_pi], writes=[B_idxf])
                    S.op("dve", lambda e, st=st: e.scalar_tensor_tensor(out=idxf[:, st:st + 1], in0=idxf[:, 5 + 2 * st:6 + 2 * st], scalar=128.0,
                                                                        in1=idxf[:, 4 + 2 * st:5 + 2 * st], op0=ALU.mult, op1=ALU.add),
                         reads=[B_idxf], writes=[B_idxf])
                S.op("dve", lambda e, bi=bi: e.tensor_copy(out=idxu[bi][:, 0:NST], in_=idxf[:, 0:NST]), reads=[B_idxf], writes=[B_idxu[bi]])

            def prep_gather(ex_):
                bi = ex_ % 2
                for st in range(NST):
                    S.dma("pool", lambda e, bi=bi, st=st: e.indirect_dma_start(
                        out=xg[bi][:, st, :], out_offset=None, in_=h1b_d[:, :],
                        in_offset=bass.IndirectOffsetOnAxis(ap=idxu[bi][:, st:st + 1], axis=0)),
                        reads=[B_idxu[bi]], writes=[B_xg[bi][st]])

            prep_sel(0)
            prep_idx(0)
            prep_gather(0)
            for ex_ in range(NE):
                r0 = ex_ * D
                bi = ex_ % 2
                bd, B_bd = bdn[ex_ % 2], B_bdn[ex_ % 2]
                S.dma("pool", lambda e, bd=bd, ex_=ex_: e.dma_start(out=bd[0:1, :], in_=b_dn[ex_:ex_ + 1, :]), writes=[B_bd])
                if ex_ + 1 < NE:
                    prep_sel(ex_ + 1)
                for m in range(KC):
                    ps, B_p = next_ps()
                    psb = ps[:, 0:CAP // 2].bitcast(BF16)
                    items = [(psb[:, st * 128:(st + 1) * 128], xg[bi][:, st, m * 128:(m + 1) * 128], ident_b) for st in range(NST)]
                    tr_group(B_p, items, reads=B_xg[bi] + [B_tok])
                    S.op("act", lambda e, psb=psb, m=m: e.copy(out=xs[:, m, :], in_=psb), reads=[B_p], writes=[B_xs[m]])
                for q in range(4):
                    if q == 1 and ex_ + 1 < NE:
                        prep_idx(ex_ + 1)
                    if q == 2 and ex_ + 1 < NE:
                        prep_gather(ex_ + 1)
                    wg = load_w(w_gu[r0:r0 + D, 512 * q:512 * q + 512], ncols=512)
                    wl = load_w(w_gu[r0:r0 + D, D + 512 * q:D + 512 * q + 512], ncols=512)
                    for kk in range(4):
                        j = 4 * q + kk
                        pg, B_pg = next_ps()
                        mm_group(pg[:, 0:CAP], B_pg, [(wg[0][:, k, kk * 128:(kk + 1) * 128], xs[:, k, :]) for k in range(KC)], reads=[wg[1]] + B_xs)
                        pli, B_pli = next_ps()
                        mm_group(pli[:, 0:CAP], B_pli, [(wl[0][:, k, kk * 128:(kk + 1) * 128], xs[:, k, :]) for k in range(KC)], reads=[wl[1]] + B_xs)
                        gt, B_gt = next_tb()
                        sg, B_sg = next_tb()
                        l1, B_l1 = next_tb()
                        cb = FO["bg"] + ex_ * 16 + j
                        cl = FO["bl"] + ex_ * 16 + j
                        S.op("dve", lambda e, gt=gt, pg=pg, cb=cb: e.tensor_scalar(out=gt[:], in0=pg[:, 0:CAP], scalar1=fv[:, cb:cb + 1], scalar2=LIMIT,
                                                                                  op0=ALU.add, op1=ALU.min), reads=[B_pg, B_fv], writes=[B_gt])
                        S.op("act", lambda e, sg=sg, gt=gt: e.activation(out=sg[:], in_=gt[:], func=AF.Sigmoid, scale=SW_ALPHA), reads=[B_gt], writes=[B_sg])
                        S.op("dve", lambda e, l1=l1, pli=pli, cl=cl: e.tensor_scalar(out=l1[:], in0=pli[:, 0:CAP], scalar1=fv[:, cl:cl + 1], scalar2=LIMIT + 1.0,
                                                                                    op0=ALU.add, op1=ALU.min), reads=[B_pli, B_fv], writes=[B_l1])
                        S.op("dve", lambda e, gt=gt, sg=sg: e.tensor_tensor(out=gt[:], in0=gt[:], in1=sg[:], op=ALU.mult), reads=[B_gt, B_sg], writes=[B_gt])
                        S.op("dve", lambda e, l1=l1, gt=gt, j=j: e.scalar_tensor_tensor(out=act[:, j, :], in0=l1[:], scalar=1.0 - LIMIT, in1=gt[:],
                                                                                        op0=ALU.max, op1=ALU.mult), reads=[B_l1, B_gt], writes=[B_act[j]])
                for nb in range(4):
                    wd = load_w(w_dn[r0:r0 + D, 512 * nb:512 * nb + 512], ncols=512)
                    for st in range(NST):
                        po, B_po = next_ps()
                        pairs = [(act[:, j, st * 128:(st + 1) * 128], wd[0][:, j, :]) for j in range(KC)]
                        pairs.append((ones_b[0:1, :], bd[0:1, 512 * nb:512 * nb + 512]))
                        mm_group(po[:, 0:512], B_po, pairs, reads=[wd[1], B_bd, B_const] + B_act)
                        if (nb + st) % 2:
                            S.op("act", lambda e, po=po, st=st, nb=nb: e.copy(out=ost[:, st, 512 * nb:512 * nb + 512], in_=po[:, 0:512]),
                                 reads=[B_po], writes=[B_ost[st]])
                        else:
                            S.op("dve", lambda e, po=po, st=st, nb=nb: e.tensor_copy(out=ost[:, st, 512 * nb:512 * nb + 512], in_=po[:, 0:512]),
                                 reads=[B_po], writes=[B_ost[st]])
                for st in range(NST):
                    S.dma("sp", lambda e, st=st, ex_=ex_: e.dma_start(out=contrib[ex_ * CAP + st * 128:ex_ * CAP + (st + 1) * 128, :], in_=ost[:, st, :]),
                          reads=[B_ost[st]], writes=[])

        if upto == "C":
            arena_off[0] = 0

            def sbC(name, shape, dt):
                return carve(shape, dt)

            NG = 12
            G = [sbC(f"G{i}", [128, D], F32) for i in range(NG)]
            hh = [sbC(f"hh{i}", [128, D], F32) for i in range(2)]
            lg2 = sbC("lg2", [128, 2 * D], F32)
            stat2 = sbC("stat2", [128, 4, 6], F32)
            mv2 = sbC("mv2", [128, 8], F32)
            B_G = [Buf(f"G{i}") for i in range(NG)]
            B_hh = [Buf("hh0"), Buf("hh1")]
            B_lg2, B_stat2, B_mv2 = Buf("lg2"), Buf("stat2"), Buf("mv2")
            phase_barrier(B_G + B_hh + [B_lg2, B_stat2, B_mv2])
            S.dma("sp", lambda e: e.dma_start(out=lg2[:], in_=rvec[:, 0:2 * D]), writes=[B_lg2])
            final = []
            for ti in range(NT):
                h, B_h = hh[ti % 2], B_hh[ti % 2]
                S.dma("sp", lambda e, h=h, ti=ti: e.dma_start(out=h[:], in_=h1f_d[ti * 128:(ti + 1) * 128, :]), writes=[B_h])
                S.op("act", lambda e, h=h: e.mul(out=h[:], in_=h[:], mul=ALPHA), reads=[B_h], writes=[B_h])
                for k in range(TOPK):
                    gi = (ti * TOPK + k) % NG
                    S.dma("pool", lambda e, gi=gi, ti=ti, k=k: e.indirect_dma_start(
                        out=G[gi][:], out_offset=None, in_=contrib[:, :],
                        in_offset=bass.IndirectOffsetOnAxis(ap=rows[:, ti, k:k + 1], axis=0)),
                        reads=[B_rows[ti]], writes=[B_G[gi]])
                    S.op("dve", lambda e, gi=gi, h=h, ti=ti, k=k: e.scalar_tensor_tensor(out=h[:], in0=G[gi][:], scalar=w4[:, ti, k:k + 1], in1=h[:],
                                                                                         op0=ALU.mult, op1=ALU.add),
                         reads=[B_G[gi], B_h, B_w4[ti]], writes=[B_h])
                for c in range(4):
                    S.op("dve", lambda e, c=c, h=h: e.bn_stats(out=stat2[:, c, :], in_=h[:, c * 512:(c + 1) * 512]), reads=[B_h], writes=[B_stat2])
                S.op("dve", lambda e: e.bn_aggr(out=mv2[:, 0:2], in_=stat2[:, :, :].rearrange("p a b -> p (a b)")), reads=[B_stat2], writes=[B_mv2])
                S.op("dve", lambda e: e.tensor_scalar(out=mv2[:, 2:3], in0=mv2[:, 1:2], scalar1=LN_EPS, scalar2=None, op0=ALU.add),
                     reads=[B_mv2], writes=[B_mv2])
                S.op("act", lambda e: e.activation(out=mv2[:, 2:3], in_=mv2[:, 2:3], func=AF.Sqrt), reads=[B_mv2], writes=[B_mv2])
                S.op("dve", lambda e: e.reciprocal(out=mv2[:, 2:3], in_=mv2[:, 2:3]), reads=[B_mv2], writes=[B_mv2])
                S.op("dve", lambda e: e.tensor_scalar(out=mv2[:, 3:4], in0=mv2[:, 0:1], scalar1=mv2[:, 2:3], scalar2=-1.0, op0=ALU.mult, op1=ALU.mult),
                     reads=[B_mv2], writes=[B_mv2])
                S.op("act", lambda e, h=h: e.activation(out=h[:], in_=h[:], func=AF.Identity, bias=mv2[:, 3:4], scale=mv2[:, 2:3]),
                     reads=[B_mv2, B_h], writes=[B_h])
                S.op("dve", lambda e, h=h: e.tensor_tensor(out=h[:], in0=h[:], in1=lg2[:, 0:D], op=ALU.mult), reads=[B_h, B_lg2], writes=[B_h])
                S.op("dve", lambda e, h=h: e.tensor_tensor(out=h[:], in0=h[:], in1=lg2[:, D:2 * D], op=ALU.add), reads=[B_h, B_lg2], writes=[B_h])
                final.append(S.dma("sp", lambda e, h=h, ti=ti: e.dma_start(out=out[ti * 128:(ti + 1) * 128, :], in_=h[:]), reads=[B_h]))

        all_dma = [(S.dsem[q][i], S.dcnt[q][i]) for q in ("sp", "pool") for i in range(S.nds) if S.dcnt[q][i] > 0]
        with nc.Block() as block:
            @block.tensor
            def _(e):
                S.emit("pe", e)

            @block.scalar
            def _(e):
                S.emit("act", e)

            @block.vector
            def _(e):
                S.emit("dve", e)

            @block.gpsimd
            def _(e):
                S.emit("pool", e)

            @block.sync
            def _(e):
                S.emit("sp", e, final_waits=all_dma)
    return nc


def _fm(v):
    v = np.asarray(v, np.float32)
    return np.ascontiguousarray(v.reshape(-1, 128).T)


def make_core_inputs(c, T, NE, x, meta_tokens, ln_in_g, ln_in_b, w_in, conv_w, w_a_out, pool_w, pool_scale, w_o,
                     ln1_g, ln1_b, router_w, router_b, w_gate_up, b_gate_up, w_down, b_down, ln2_g, ln2_b, shared):
    halves = x.shape[1] // T
    b, h = divmod(c, halves)
    if h == 0:
        xin = np.concatenate([meta_tokens.astype(np.float32), x[b, 0:T]], axis=0)
    else:
        xin = x[b, h * T - HALO:(h + 1) * T]
    d = dict(shared)
    d["xin"] = np.ascontiguousarray(xin, dtype=np.float32)
    return d


def make_shared(NE, ln_in_g, ln_in_b, w_in, conv_w, w_a_out, pool_w, pool_scale, w_o, ln1_g, ln1_b, router_w, router_b,
                w_gate_up, b_gate_up, w_down, b_down, ln2_g, ln2_b):
    cols = [_fm(ln_in_g), _fm(ln_in_b), _fm(conv_w[0, 0]), _fm(conv_w[0, 1]), _fm(conv_w[0, 2]), _fm(pool_scale[0]),
            _fm(ln1_g[0]), _fm(ln1_b[0])]
    bgu = np.asarray(b_gate_up[0], np.float32)
    bg = np.concatenate([_fm(bgu[e, :D]) for e in range(NE)], axis=1)
    bl = np.concatenate([_fm(bgu[e, D:]) for e in range(NE)], axis=1)
    fvec = np.ascontiguousarray(np.concatenate(cols + [bg, bl], axis=1), dtype=np.float32)
    rrow = np.concatenate([np.asarray(ln2_g[0], np.float32), np.asarray(ln2_b[0], np.float32), np.asarray(router_b[0], np.float32)])
    rvec = np.ascontiguousarray(np.broadcast_to(rrow[None, :], (128, rrow.shape[0])), dtype=np.float32)
    return {
        "w_in": np.ascontiguousarray(w_in[0], dtype=np.float32),
        "w_a": np.ascontiguousarray(w_a_out[0], dtype=np.float32),
        "w_o": np.ascontiguousarray(w_o[0], dtype=np.float32),
        "pool_w": np.ascontiguousarray(np.asarray(pool_w[0], np.float32).reshape(1024, 512)),
        "router_w": np.ascontiguousarray(router_w[0], dtype=np.float32),
        "w_gu": np.ascontiguousarray(np.asarray(w_gate_up[0], np.float32).reshape(NE * D, 2 * D)),
        "w_dn": np.ascontiguousarray(np.asarray(w_down[0], np.float32).reshape(NE * D, D)),
        "b_dn": np.ascontiguousarray(b_down[0], dtype=np.float32),
        "fvec": fvec,
        "rvec": rvec,
    }


def kernel(**inputs):
    inputs = {k: np.asarray(v) for k, v in inputs.items()}
    x = inputs["x"]
    B, SEQ, _ = x.shape
    NE = inputs["router_w"].shape[-1]
    n_cores = 8
    T = B * SEQ // n_cores
    CAP = 384
    names = ["ln_in_g", "ln_in_b", "w_in", "conv_w", "w_a_out", "pool_w", "pool_scale", "w_o", "ln1_g", "ln1_b", "router_w",
             "router_b", "w_gate_up", "b_gate_up", "w_down", "b_down", "ln2_g", "ln2_b"]
    shared = make_shared(NE, *[inputs[n] for n in names])
    in_maps = [make_core_inputs(c, T, NE, x, inputs["meta_tokens"], *[inputs[n] for n in names], shared) for c in range(n_cores)]
    nc = build_nc(T, NE, CAP)
    res = run_bass_kernel_spmd(nc, in_maps, core_ids=list(range(n_cores)))
    halves = SEQ // T
    outp = np.empty((B, SEQ, D), np.float32)
    for c in range(n_cores):
        b, h = divmod(c, halves)
        outp[b, h * T:(h + 1) * T] = res.results[c]["out"]
    return outp
```

```python
import numpy as np
from contextlib import ExitStack
import concourse.bass as bass
import concourse.mybir as mybir
from concourse.bass_utils import run_bass_kernel_spmd

F32 = mybir.dt.float32
BF16 = mybir.dt.bfloat16
U32 = mybir.dt.uint32
I32 = mybir.dt.int32
AF = mybir.ActivationFunctionType
ALU = mybir.AluOpType
AX = mybir.AxisListType

D = 2048
KC = 16
DIN = 11264
NMETA = 16
HALO = 16
TOPK = 4
LN_EPS = 1e-5
ALPHA = 2.0 ** 0.25
LIMIT = 7.0
SW_ALPHA = 1.702
WCOLS = 256
NWR = 5


class Buf:
    __slots__ = ("name", "w", "r")

    def __init__(self, name):
        self.name = name
        self.w = None
        self.r = {}


class Sched:
    def __init__(self, nc, es):
        self.nc = nc
        self.es = es
        self.ops = {k: [] for k in ("pe", "act", "dve", "pool", "sp")}
        self.sem = {k: es.enter_context(nc.semaphore("s_" + k)) for k in ("pe", "act", "dve", "poolc")}
        self.cnt = {k: 0 for k in self.sem}
        self.nds = 8
        self.dsem = {q: [es.enter_context(nc.semaphore(f"d_{q}{i}")) for i in range(self.nds)] for q in ("sp", "pool")}
        self.dcnt = {q: [0] * self.nds for q in ("sp", "pool")}
        self.dnext = {q: 0 for q in ("sp", "pool")}

    def _deps(self, reads, writes):
        deps = []
        for b in reads:
            if b.w is not None:
                deps.append(b.w)
        for b in writes:
            if b.w is not None:
                deps.append(b.w)
            deps.extend(b.r.values())
        return deps

    def _commit(self, tok, reads, writes):
        for b in writes:
            b.w = tok
            b.r = {}
        for b in reads:
            if b in writes:
                continue
            s, v = tok
            if b.r.get(id(s), (None, 0))[1] < v:
                b.r[id(s)] = (s, v)

    def op(self, eng, fn, reads=(), writes=()):
        skey = "poolc" if eng == "pool" else eng
        deps = self._deps(reads, writes)
        self.cnt[skey] += 1
        tok = (self.sem[skey], self.cnt[skey])
        self.ops[eng].append((fn, deps, tok, 1))
        self._commit(tok, reads, writes)
        return tok

    def dma(self, q, fn, reads=(), writes=()):
        deps = self._deps(reads, writes)
        i = self.dnext[q]
        self.dnext[q] = (i + 1) % self.nds
        sem = self.dsem[q][i]
        if self.dcnt[q][i] > 0:
            deps.append((sem, self.dcnt[q][i]))
        self.dcnt[q][i] += 16
        tok = (sem, self.dcnt[q][i])
        self.ops[q].append((fn, deps, tok, 16))
        self._commit(tok, reads, writes)
        return tok

    def emit(self, eng, e, final_waits=()):
        waited = {}
        for fn, deps, tok, inc in self.ops[eng]:
            for s, v in deps:
                k = id(s)
                if waited.get(k, 0) < v:
                    e.wait_ge(s, v)
                    waited[k] = v
            ins = fn(e)
            ins.then_inc(tok[0], inc)
        for s, v in final_waits:
            e.wait_ge(s, v)


class _Stop(Exception):
    pass


def build_nc(T, NE, CAP, debug=False, upto="C", cutv=0):
    assert T % 512 == 0 and CAP % 128 == 0 and CAP <= 512
    NT = T // 128
    NST = CAP // 128
    NB = T // 512
    nc = bass.Bass("TRN2", target_bir_lowering=False)

    def din(name, shape, dt=F32):
        return nc.dram_tensor(name, shape, dt, kind="ExternalInput").ap()

    xin = din("xin", [T + HALO, D])
    w_in = din("w_in", [D, DIN])
    w_a = din("w_a", [D, D])
    w_o = din("w_o", [D, D])
    pool_w = din("pool_w", [1024, 512])
    router_w = din("router_w", [D, NE])
    w_gu = din("w_gu", [NE * D, 2 * D])
    w_dn = din("w_dn", [NE * D, D])
    b_dn = din("b_dn", [NE, D])
    NF = 8 * 16 + 2 * NE * 16
    fvec = din("fvec", [128, NF])
    NR = 2 * D + NE
    rvec = din("rvec", [128, NR])
    out = nc.dram_tensor("out", [T, D], F32, kind="ExternalOutput").ap()
    import os as _os
    SCR = _os.environ.get("SCRKIND", "Internal")
    h1f_d = nc.dram_tensor("h1f_d", [T, D], F32, kind=SCR).ap()
    h1b_d = nc.dram_tensor("h1b_d", [T, D], BF16, kind=SCR).ap()
    contrib = nc.dram_tensor("contrib", [NE * CAP, D], F32, kind=SCR).ap()
    dbg = {}
    if debug:
        dbg["h1"] = nc.dram_tensor("dbg_h1", [T, D], F32, kind="ExternalOutput").ap()
        dbg["route"] = nc.dram_tensor("dbg_route", [128, NT * 8], F32, kind="ExternalOutput").ap()
        dbg["za"] = nc.dram_tensor("dbg_za", [128, KC * 512], BF16, kind="ExternalOutput").ap()
        dbg["pl"] = nc.dram_tensor("dbg_pl", [128, 8 * 512], BF16, kind="ExternalOutput").ap()
        dbg["mg"] = nc.dram_tensor("dbg_mg", [128, KC * 512], BF16, kind="ExternalOutput").ap()
        dbg["h0b"] = nc.dram_tensor("dbg_h0b", [128, KC * 512], BF16, kind="ExternalOutput").ap()

    FO = dict(lng=0, lnb=16, cw0=32, cw1=48, cw2=64, psc=80, l1g=96, l1b=112, bg=128, bl=128 + NE * 16)

    with ExitStack() as es:
        S = Sched(nc, es)

        def sb(name, shape, dt):
            return es.enter_context(nc.sbuf_tensor(name, shape, dt))

        fv = sb("fv", [128, NF], F32)
        ident = sb("ident", [128, 128], F32)
        ones_f = sb("ones_f", [128, 128], F32)
        ones_b = sb("ones_b", [128, 128], BF16)
        triu_b = sb("triu_b", [128, 128], BF16)
        iota_i = sb("iota_i", [128, 512], I32)
        iota_f = sb("iota_f", [128, 512], F32)
        pidx_i = sb("pidx_i", [128, 1], I32)
        pidx_f = sb("pidx_f", [128, 1], F32)
        rw = sb("rw", [128, KC, NE], F32)
        rb = sb("rb", [128, NE], F32)
        maskb = sb("maskb", [128, NT, NE], BF16)
        posm = sb("posm", [128, NT, NE], F32)
        w4 = sb("w4", [128, NT, 4], F32)
        rows = sb("rows", [128, NT, 4], U32)
        B_fv, B_const, B_rw = Buf("fv"), Buf("const"), Buf("rw")
        B_maskb = [Buf(f"maskb{i}") for i in range(NT)]
        B_posm = [Buf(f"posm{i}") for i in range(NT)]
        B_w4 = [Buf(f"w4{i}") for i in range(NT)]
        B_rows = [Buf(f"rows{i}") for i in range(NT)]

        S.dma("sp", lambda e: e.dma_start(out=fv[:], in_=fvec[:, :]), writes=[B_fv])
        S.dma("sp", lambda e: e.dma_start(out=rw[:], in_=router_w.rearrange("(k p) n -> p k n", p=128)), writes=[B_rw])
        S.dma("sp", lambda e: e.dma_start(out=rb[:], in_=rvec[:, 2 * D:2 * D + NE]), writes=[B_rw])
        S.op("pool", lambda e: e.iota(iota_i[:], pattern=[[1, 512]], base=0, channel_multiplier=0), writes=[B_const])
        S.op("pool", lambda e: e.iota(pidx_i[:], pattern=[[1, 1]], base=0, channel_multiplier=1), writes=[B_const])
        S.op("dve", lambda e: e.tensor_copy(out=iota_f[:], in_=iota_i[:]), reads=[B_const], writes=[B_const])
        S.op("dve", lambda e: e.tensor_copy(out=pidx_f[:], in_=pidx_i[:]), reads=[B_const], writes=[B_const])
        S.op("dve", lambda e: e.tensor_scalar(out=ident[:], in0=iota_f[:, 0:128], scalar1=pidx_f[:, 0:1], scalar2=None,
                                              op0=ALU.is_equal), reads=[B_const], writes=[B_const])
        S.op("dve", lambda e: e.tensor_scalar(out=triu_b[:], in0=iota_f[:, 0:128], scalar1=pidx_f[:, 0:1], scalar2=None,
                                              op0=ALU.is_gt), reads=[B_const], writes=[B_const])
        S.op("dve", lambda e: e.memset(ones_f[:], 1.0), writes=[B_const])
        S.op("dve", lambda e: e.memset(ones_b[:], 1.0), writes=[B_const])
        S.op("dve", lambda e: e.tensor_scalar(out=fv[:, FO["bl"]:FO["bl"] + NE * 16], in0=fv[:, FO["bl"]:FO["bl"] + NE * 16],
                                              scalar1=1.0, scalar2=None, op0=ALU.add), reads=[B_fv], writes=[B_fv])

        def cut(k):
            if cutv == k:
                raise _Stop()

        def fcol(key, k):
            c = FO[key] + k
            return fv[:, c:c + 1]

        NARENA = 47104
        arena_t = sb("arena", [128, NARENA], F32)
        arena_off = [0]

        def carve(shape, dt):
            n = 1
            for d_ in shape[1:]:
                n *= d_
            nbytes = n * (2 if dt == BF16 else 4)
            nf = (nbytes + 7) // 8 * 2
            off = arena_off[0]
            assert off + nf <= NARENA, ("arena overflow", off, nf)
            arena_off[0] = off + nf
            v = arena_t[:, off:off + nf]
            if dt != F32:
                v = v.bitcast(dt)
            v = v[:, 0:n]
            if len(shape) == 3:
                v = v.rearrange("p (a b) -> p a b", a=shape[1])
            return v

        def phase_barrier(bufs):
            toks = [(S.sem[k], S.cnt[k]) for k in ("pe", "act", "dve") if S.cnt[k] > 0]
            toks += [(S.dsem[q][i], S.dcnt[q][i]) for q in ("sp", "pool") for i in range(S.nds) if S.dcnt[q][i] > 0]
            for bb in bufs:
                for tk in toks:
                    bb.r[id(tk[0])] = tk

        psum = [es.enter_context(nc.psum_tensor(f"ps{i}", [128, 512], F32)) for i in range(8)]
        B_ps = [Buf(f"ps{i}") for i in range(8)]
        ps_rr = [0]
        ps_reserved = set()

        def next_ps():
            while ps_rr[0] in ps_reserved:
                ps_rr[0] = (ps_rr[0] + 1) % 8
            i = ps_rr[0]
            ps_rr[0] = (i + 1) % 8
            return psum[i], B_ps[i]

        ring = {"bufs": [], "B": [], "rr": 0}

        def make_ring(n, elems):
            ring["bufs"] = [carve([128, elems], BF16) for _ in range(n)]
            ring["B"] = [Buf(f"wr{i}") for i in range(n)]
            ring["rr"] = 0
            return ring["B"]

        def load_w(src_ap, kk=KC, ncols=WCOLS):
            i = ring["rr"]
            ring["rr"] = (i + 1) % len(ring["bufs"])
            view = ring["bufs"][i][:, 0:kk * ncols].rearrange("p (k n) -> p k n", k=kk)
            Bw = ring["B"][i]
            S.dma("pool", lambda e: e.dma_start(out=view, in_=src_ap.rearrange("(k p) n -> p k n", p=128)), writes=[Bw])
            return view, Bw

        def mm_group(ps_ap, B_out, pairs, reads):
            def fn(e):
                ins = None
                n = len(pairs)
                for i, (l, r) in enumerate(pairs):
                    ins = e.matmul(ps_ap, lhsT=l, rhs=r, start=(i == 0), stop=(i == n - 1))
                return ins
            return S.op("pe", fn, reads=reads, writes=[B_out])

        def tr_group(B_out, items, reads):
            def fn(e):
                ins = None
                for o, i_, idn in items:
                    ins = e.transpose(o, i_, idn)
                return ins
            return S.op("pe", fn, reads=reads, writes=[B_out])

        if True:
            def sbA(name, shape, dt):
                return carve(shape, dt)

            make_ring(NWR, KC * WCOLS)
            h0f = sbA("h0f", [128, KC, 512], F32)
            h0b = sbA("h0b", [128, KC, 512], BF16)
            za = sbA("za", [128, KC, 512], BF16)
            pl = sbA("pl", [128, 8, 512], BF16)
            mg = sbA("mg", [128, KC, 512], BF16)
            xst = [sbA(f"xst{i}", [128, D], F32) for i in range(2)]
            hbst = [sbA(f"hbst{i}", [128, D], BF16) for i in range(2)]
            cuh = sbA("cuh", [128, KC, 16], F32)
            vh = sbA("vh", [128, 8, 16], F32)
            NTMP = 8
            tmp = [sbA(f"tmp{i}", [128, 528], F32) for i in range(NTMP)]
            stat = sbA("stat", [128, 4, 6], F32)
            mv = sbA("mv", [128, 8], F32)
            rt = sbA("rt", [128, 512], F32)
            rti = sbA("rti", [128, 8], U32)
            lgall = sbA("lgall", [128, 4, NE], F32)
            B_lg = [Buf(f"lg{i}") for i in range(4)]
            B_h0f = [Buf(f"h0f{k}") for k in range(KC)]
            B_h0b = [Buf(f"h0b{k}") for k in range(KC)]
            B_za = [Buf(f"za{k}") for k in range(KC)]
            B_pl = [Buf(f"pl{k}") for k in range(8)]
            B_mg = [Buf(f"mg{k}") for k in range(KC)]
            B_xst = [Buf("xst0"), Buf("xst1")]
            B_hbst = [Buf("hbst0"), Buf("hbst1")]
            B_cuh = [Buf(f"cuh{k}") for k in range(KC)]
            B_vh = [Buf(f"vh{k}") for k in range(8)]
            B_tmp = [Buf(f"tmp{i}") for i in range(NTMP)]
            B_stat, B_mv, B_rt = Buf("stat"), Buf("mv"), Buf("rt")
            wpl_t = sbA("wpl", [128, 8, 512], BF16)
            B_wpl = Buf("wpl")
            S.dma("pool", lambda e: e.dma_start(out=wpl_t, in_=pool_w.rearrange("(k p) n -> p k n", p=128)), writes=[B_wpl])
            tmp_rr = [0]

            def next_tmp():
                i = tmp_rr[0]
                tmp_rr[0] = (i + 1) % NTMP
                return tmp[i], B_tmp[i]

            ev_rr = [0]

            def evac_copy(out_ap, in_ap, reads, writes):
                ev_rr[0] ^= 1
                if ev_rr[0]:
                    return S.op("act", lambda e: e.copy(out=out_ap, in_=in_ap), reads=reads, writes=writes)
                return S.op("dve", lambda e: e.tensor_copy(out=out_ap, in_=in_ap), reads=reads, writes=writes)

            def ln_rows(x_ap, npart, B_x):
                for c in range(4):
                    S.op("dve", lambda e, c=c: e.bn_stats(out=stat[0:npart, c, :], in_=x_ap[:, c * 512:(c + 1) * 512]),
                         reads=[B_x], writes=[B_stat] if c == 0 else [B_stat])
                S.op("dve", lambda e: e.bn_aggr(out=mv[0:npart, 0:2], in_=stat[0:npart, :, :].rearrange("p a b -> p (a b)")),
                     reads=[B_stat], writes=[B_mv])
                S.op("dve", lambda e: e.tensor_scalar(out=mv[0:npart, 2:3], in0=mv[0:npart, 1:2], scalar1=LN_EPS, scalar2=None,
                                                      op0=ALU.add), reads=[B_mv], writes=[B_mv])
                S.op("act", lambda e: e.activation(out=mv[0:npart, 2:3], in_=mv[0:npart, 2:3], func=AF.Sqrt), reads=[B_mv], writes=[B_mv])
                S.op("dve", lambda e: e.reciprocal(out=mv[0:npart, 2:3], in_=mv[0:npart, 2:3]), reads=[B_mv], writes=[B_mv])
                S.op("dve", lambda e: e.tensor_scalar(out=mv[0:npart, 3:4], in0=mv[0:npart, 0:1], scalar1=mv[0:npart, 2:3], scalar2=-1.0,
                                                      op0=ALU.mult, op1=ALU.mult), reads=[B_mv], writes=[B_mv])
                S.op("act", lambda e: e.activation(out=x_ap, in_=x_ap, func=AF.Identity, bias=mv[0:npart, 3:4],
                                                   scale=mv[0:npart, 2:3]), reads=[B_mv, B_x], writes=[B_x])

            hhf = sbA("hhf", [128, KC, 16], F32)
            hhb = sbA("hhb", [128, KC, 16], BF16)
            B_hhf, B_hhb = Buf("hhf"), Buf("hhb")

            def load_ln_T(n, r0, dst_f, dst_b, Bf, Bb, keep_f32):
                nsub = max(1, n // 128)
                npart = min(n, 128)
                for sub in range(nsub):
                    xt, B_x = xst[sub % 2], B_xst[sub % 2]
                    S.dma("sp", lambda e, xt=xt, sub=sub: e.dma_start(out=xt[0:npart, :], in_=xin[r0 + sub * 128:r0 + sub * 128 + npart, :]),
                          writes=[B_x])
                    ln_rows(xt[0:npart, :], npart, B_x)
                    for q in range(4):
                        ps, B_p = next_ps()
                        items = [(ps[:, kk * npart:(kk + 1) * npart], xt[0:npart, (4 * q + kk) * 128:(4 * q + kk + 1) * 128],
                                  ident[0:npart, 0:npart]) for kk in range(4)]
                        tr_group(B_p, items, reads=[B_x, B_const])
                        evac_copy(dst_f[:, 4 * q:4 * q + 4, sub * 128:sub * 128 + npart],
                                  ps[:, 0:4 * npart].rearrange("p (k t) -> p k t", k=4),
                                  reads=[B_p], writes=[Bf[4 * q + kk] for kk in range(4)])
                for k in range(KC):
                    S.op("act", lambda e, k=k: e.activation(out=dst_b[:, k, 0:n], in_=dst_f[:, k, 0:n], func=AF.Identity,
                                                            bias=fcol("lnb", k), scale=fcol("lng", k)),
                         reads=[Bf[k], B_fv], writes=[Bb[k]])
                    if keep_f32:
                        S.op("dve", lambda e, k=k: e.tensor_scalar(out=dst_f[:, k, 0:n], in0=dst_f[:, k, 0:n], scalar1=fcol("lng", k),
                                                                   scalar2=fcol("lnb", k), op0=ALU.mult, op1=ALU.add),
                             reads=[Bf[k], B_fv], writes=[Bf[k]])

            def mixer_block(n, r0, halo, tile0, prev_tail, fold=False):
                nsub = max(1, n // 128)
                npart = min(n, 128)
                if fold:
                    load_ln_T(HALO, 0, hhf, hhb, [B_hhf] * KC, [B_hhb] * KC, False)
                load_ln_T(n, r0, h0f, h0b, B_h0f, B_h0b, not halo)

                cut(2 if halo else 4)

                def proj(wv, B_w, kk, reads_extra=()):
                    ps, B_p = next_ps()
                    pairs = [(wv[:, k, kk * 128:(kk + 1) * 128], h0b[:, k, 0:n]) for k in range(KC)]
                    mm_group(ps[:, 0:n], B_p, pairs, reads=[B_w] + B_h0b)
                    return ps, B_p

                def proj_h(wv, B_w, kk):
                    ps, B_p = next_ps()
                    pairs = [(wv[:, k, kk * 128:(kk + 1) * 128], hhb[:, k, :]) for k in range(KC)]
                    mm_group(ps[:, 0:HALO], B_p, pairs, reads=[B_w, B_hhb])
                    return ps, B_p

                for s in range(8):
                    if prev_tail is not None and 1 <= s <= len(prev_tail):
                        prev_tail[s - 1]()
                    if halo:
                        wb = None
                    else:
                        wb = load_w(w_in[:, 256 * s:256 * s + 256])
                    wc = load_w(w_in[:, 2048 + 256 * s:2048 + 256 * s + 256])
                    wu = load_w(w_in[:, 4096 + 256 * s:4096 + 256 * s + 256])
                    for kk in range(2):
                        i = 2 * s + kk
                        pc, B_pc = proj(wc[0], wc[1], kk)
                        pu, B_pu = proj(wu[0], wu[1], kk)
                        if halo:
                            ut, B_ut = next_tmp()
                            S.op("act", lambda e, ut=ut, pu=pu: e.copy(out=ut[:, 0:n], in_=pu[:, 0:n]), reads=[B_pu], writes=[B_ut])
                            S.op("dve", lambda e, ut=ut, pc=pc, i=i: e.tensor_tensor(out=cuh[:, i, :], in0=pc[:, 0:n], in1=ut[:, 0:n], op=ALU.mult),
                                 reads=[B_pc, B_ut], writes=[B_cuh[i]])
                            continue
                        if fold:
                            pcH, B_pcH = proj_h(wc[0], wc[1], kk)
                            puH, B_puH = proj_h(wu[0], wu[1], kk)
                            utH, B_utH = next_tmp()
                            S.op("act", lambda e, utH=utH, puH=puH: e.copy(out=utH[:, 0:HALO], in_=puH[:, 0:HALO]), reads=[B_puH], writes=[B_utH])
                            S.op("dve", lambda e, utH=utH, pcH=pcH, i=i: e.tensor_tensor(out=cuh[:, i, :], in0=pcH[:, 0:HALO], in1=utH[:, 0:HALO], op=ALU.mult),
                                 reads=[B_pcH, B_utH], writes=[B_cuh[i]])
                        pb, B_pb = proj(wb[0], wb[1], kk)
                        ut, B_ut = next_tmp()
                        cu, B_cu = next_tmp()
                        t1, B_t1 = next_tmp()
                        S.op("act", lambda e, ut=ut, pu=pu: e.copy(out=ut[:, 0:n], in_=pu[:, 0:n]), reads=[B_pu], writes=[B_ut])
                        S.op("act", lambda e, cu=cu, i=i: e.copy(out=cu[:, 0:16], in_=cuh[:, i, :]), reads=[B_cuh[i]], writes=[B_cu])
                        S.op("dve", lambda e, cu=cu, pc=pc, ut=ut: e.tensor_tensor(out=cu[:, 16:16 + n], in0=pc[:, 0:n], in1=ut[:, 0:n], op=ALU.mult),
                             reads=[B_pc, B_ut], writes=[B_cu])
                        S.op("act", lambda e, cu=cu, i=i: e.copy(out=cuh[:, i, :], in_=cu[:, n:n + 16]), reads=[B_cu], writes=[B_cuh[i]])
                        S.op("dve", lambda e, t1=t1, cu=cu, i=i: e.tensor_scalar(out=t1[:, 0:n], in0=cu[:, 16:16 + n], scalar1=fcol("cw2", i),
                                                                                 scalar2=None, op0=ALU.mult), reads=[B_cu, B_fv], writes=[B_t1])
                        S.op("dve", lambda e, t1=t1, cu=cu, i=i: e.scalar_tensor_tensor(out=t1[:, 0:n], in0=cu[:, 15:15 + n], scalar=fcol("cw1", i),
                                                                                        in1=t1[:, 0:n], op0=ALU.mult, op1=ALU.add),
                             reads=[B_cu, B_fv, B_t1], writes=[B_t1])
                        S.op("dve", lambda e, t1=t1, cu=cu, i=i: e.scalar_tensor_tensor(out=t1[:, 0:n], in0=cu[:, 14:14 + n], scalar=fcol("cw0", i),
                                                                                        in1=t1[:, 0:n], op0=ALU.mult, op1=ALU.add),
                             reads=[B_cu, B_fv, B_t1], writes=[B_t1])
                        S.op("dve", lambda e, t1=t1, pb=pb, i=i: e.tensor_tensor(out=za[:, i, 0:n], in0=pb[:, 0:n], in1=t1[:, 0:n], op=ALU.mult),
                             reads=[B_pb, B_t1], writes=[B_za[i]])

                if not halo:
                    cut(5)
                for g in range(4):
                    wv_ = load_w(w_in[:, 6144 + 256 * g:6144 + 256 * g + 256])
                    W = 2 << g
                    for kk in range(2):
                        c = 2 * g + kk
                        pv, B_pv = proj(wv_[0], wv_[1], kk)
                        if halo:
                            S.op("act", lambda e, pv=pv, c=c: e.copy(out=vh[:, c, :], in_=pv[:, 0:n]), reads=[B_pv], writes=[B_vh[c]])
                            continue
                        if fold:
                            pvH, B_pvH = proj_h(wv_[0], wv_[1], kk)
                            S.op("act", lambda e, pvH=pvH, c=c: e.copy(out=vh[:, c, :], in_=pvH[:, 0:HALO]), reads=[B_pvH], writes=[B_vh[c]])
                        vs, B_vs = next_tmp()
                        S.op("act", lambda e, vs=vs, c=c: e.copy(out=vs[:, 0:16], in_=vh[:, c, :]), reads=[B_vh[c]], writes=[B_vs])
                        S.op("act", lambda e, vs=vs, pv=pv: e.copy(out=vs[:, 16:16 + n], in_=pv[:, 0:n]), reads=[B_pv], writes=[B_vs])
                        S.op("act", lambda e, vs=vs, c=c: e.copy(out=vh[:, c, :], in_=vs[:, n:n + 16]), reads=[B_vs], writes=[B_vh[c]])
                        cur, B_cur = vs, B_vs
                        sh = 1
                        while sh < W:
                            nx, B_nx = next_tmp()
                            S.op("dve", lambda e, nx=nx, cur=cur, sh=sh: e.tensor_tensor(out=nx[:, sh:16 + n], in0=cur[:, sh:16 + n],
                                                                                          in1=cur[:, 0:16 + n - sh], op=ALU.add),
                                 reads=[B_cur], writes=[B_nx])
                            cur, B_cur = nx, B_nx
                            sh *= 2
                        S.op("dve", lambda e, cur=cur, vs=vs, c=c, W=W: e.scalar_tensor_tensor(out=pl[:, c, 0:n], in0=cur[:, 16:16 + n], scalar=1.0 / W,
                                                                                                in1=vs[:, 16:16 + n], op0=ALU.mult, op1=ALU.subtract),
                             reads=[B_cur, B_vs], writes=[B_pl[c]])
                if halo:
                    return
                cut(6)

                wpl = (wpl_t, B_wpl)
                for jp in range(8):
                    wa = load_w(w_a[:, 256 * jp:256 * jp + 256])
                    wga = load_w(w_in[:, 7168 + 256 * jp:7168 + 256 * jp + 256])
                    wgb = load_w(w_in[:, 9216 + 256 * jp:9216 + 256 * jp + 256])
                    for kk in range(2):
                        j = 2 * jp + kk
                        g = j // 4
                        pya, B_pya = next_ps()
                        mm_group(pya[:, 0:n], B_pya, [(wa[0][:, i, kk * 128:(kk + 1) * 128], za[:, i, 0:n]) for i in range(KC)],
                                 reads=[wa[1]] + B_za)
                        pyb, B_pyb = next_ps()
                        mm_group(pyb[:, 0:n], B_pyb, [(wpl[0][:, 2 * g + c2, (j % 4) * 128:(j % 4 + 1) * 128], pl[:, 2 * g + c2, 0:n]) for c2 in range(2)],
                                 reads=[wpl[1], B_pl[2 * g], B_pl[2 * g + 1]])
                        pga, B_pga = proj(wga[0], wga[1], kk)
                        pgb, B_pgb = proj(wgb[0], wgb[1], kk)
                        sga, B_sga = next_tmp()
                        sgb, B_sgb = next_tmp()
                        S.op("act", lambda e, sga=sga, pga=pga: e.activation(out=sga[:, 0:n], in_=pga[:, 0:n], func=AF.Sigmoid), reads=[B_pga], writes=[B_sga])
                        S.op("act", lambda e, sgb=sgb, pgb=pgb: e.activation(out=sgb[:, 0:n], in_=pgb[:, 0:n], func=AF.Sigmoid), reads=[B_pgb], writes=[B_sgb])
                        S.op("dve", lambda e, sga=sga, pya=pya: e.tensor_tensor(out=sga[:, 0:n], in0=pya[:, 0:n], in1=sga[:, 0:n], op=ALU.mult),
                             reads=[B_pya, B_sga], writes=[B_sga])
                        S.op("dve", lambda e, sgb=sgb, pyb=pyb, j=j: e.scalar_tensor_tensor(out=sgb[:, 0:n], in0=pyb[:, 0:n], scalar=fcol("psc", j),
                                                                                            in1=sgb[:, 0:n], op0=ALU.mult, op1=ALU.mult),
                             reads=[B_pyb, B_sgb, B_fv], writes=[B_sgb])
                        S.op("dve", lambda e, sga=sga, sgb=sgb, j=j: e.tensor_tensor(out=mg[:, j, 0:n], in0=sga[:, 0:n], in1=sgb[:, 0:n], op=ALU.add),
                             reads=[B_sga, B_sgb], writes=[B_mg[j]])

                if debug and tile0 == 0:
                    S.dma("sp", lambda e: e.dma_start(out=dbg["za"].rearrange("p (k n) -> p k n", k=KC), in_=za), reads=B_za)
                    S.dma("sp", lambda e: e.dma_start(out=dbg["pl"].rearrange("p (k n) -> p k n", k=8), in_=pl), reads=B_pl)
                    S.dma("sp", lambda e: e.dma_start(out=dbg["mg"].rearrange("p (k n) -> p k n", k=KC), in_=mg), reads=B_mg)
                    S.dma("sp", lambda e: e.dma_start(out=dbg["h0b"].rearrange("p (k n) -> p k n", k=KC), in_=h0b), reads=B_h0b)
                cut(7)
                psum_s, B_pss = next_ps()
                psum_q, B_psq = next_ps()
                res_banks = [i for i in range(8) if psum[i] is psum_s or psum[i] is psum_q]
                ps_reserved.update(res_banks)

                def ln1_stats(j, sq, B_sq):
                    def fn_s(e):
                        return e.matmul(psum_s[:, 0:n], lhsT=ones_f[:], rhs=h0f[:, j, 0:n], start=(j == 0), stop=(j == KC - 1))

                    def fn_q(e):
                        return e.matmul(psum_q[:, 0:n], lhsT=ones_f[:], rhs=sq[:, 0:n], start=(j == 0), stop=(j == KC - 1))
                    S.op("pe", fn_s, reads=[B_h0f[j], B_const] + ([B_pss] if j else []), writes=[B_pss])
                    S.op("pe", fn_q, reads=[B_sq] + ([B_psq] if j else []), writes=[B_psq])

                pend = None
                for jp in range(8):
                    wo = load_w(w_o[:, 256 * jp:256 * jp + 256])
                    for kk in range(2):
                        j = 2 * jp + kk
                        pm, B_pm = next_ps()
                        mm_group(pm[:, 0:n], B_pm, [(wo[0][:, i, kk * 128:(kk + 1) * 128], mg[:, i, 0:n]) for i in range(KC)],
                                 reads=[wo[1]] + B_mg)
                        S.op("dve", lambda e, pm=pm, j=j: e.scalar_tensor_tensor(out=h0f[:, j, 0:n], in0=h0f[:, j, 0:n], scalar=ALPHA, in1=pm[:, 0:n],
                                                                                 op0=ALU.mult, op1=ALU.add), reads=[B_pm, B_h0f[j]], writes=[B_h0f[j]])
                        sq, B_sq = next_tmp()
                        S.op("act", lambda e, sq=sq, j=j: e.activation(out=sq[:, 0:n], in_=h0f[:, j, 0:n], func=AF.Square), reads=[B_h0f[j]], writes=[B_sq])
                        if pend is not None:
                            ln1_stats(*pend)
                        pend = (j, sq, B_sq)
                ln1_stats(*pend)
                for i_ in res_banks:
                    ps_reserved.discard(i_)
                cut(8)
                mean, B_mean = next_tmp()
                rstd, B_rstd = next_tmp()
                S.op("dve", lambda e: e.tensor_scalar(out=mean[:, 0:n], in0=psum_s[:, 0:n], scalar1=1.0 / D, scalar2=None, op0=ALU.mult),
                     reads=[B_pss], writes=[B_mean])
                S.op("dve", lambda e: e.tensor_tensor(out=rstd[:, 0:n], in0=mean[:, 0:n], in1=mean[:, 0:n], op=ALU.mult), reads=[B_mean], writes=[B_rstd])
                S.op("dve", lambda e: e.scalar_tensor_tensor(out=rstd[:, 0:n], in0=psum_q[:, 0:n], scalar=1.0 / D, in1=rstd[:, 0:n],
                                                             op0=ALU.mult, op1=ALU.subtract), reads=[B_psq, B_rstd], writes=[B_rstd])
                S.op("dve", lambda e: e.tensor_scalar(out=rstd[:, 0:n], in0=rstd[:, 0:n], scalar1=LN_EPS, scalar2=None, op0=ALU.add),
                     reads=[B_rstd], writes=[B_rstd])
                S.op("act", lambda e: e.activation(out=rstd[:, 0:n], in_=rstd[:, 0:n], func=AF.Sqrt), reads=[B_rstd], writes=[B_rstd])
                S.op("dve", lambda e: e.reciprocal(out=rstd[:, 0:n], in_=rstd[:, 0:n]), reads=[B_rstd], writes=[B_rstd])
                for j in range(KC):
                    S.op("dve", lambda e, j=j: e.tensor_tensor(out=h0f[:, j, 0:n], in0=h0f[:, j, 0:n], in1=mean[:, 0:n], op=ALU.subtract),
                         reads=[B_h0f[j], B_mean], writes=[B_h0f[j]])
                    S.op("dve", lambda e, j=j: e.tensor_tensor(out=h0f[:, j, 0:n], in0=h0f[:, j, 0:n], in1=rstd[:, 0:n], op=ALU.mult),
                         reads=[B_h0f[j], B_rstd], writes=[B_h0f[j]])
                    S.op("act", lambda e, j=j: e.activation(out=h0f[:, j, 0:n], in_=h0f[:, j, 0:n], func=AF.Identity, bias=fcol("l1b", j),
                                                            scale=fcol("l1g", j)), reads=[B_h0f[j], B_fv], writes=[B_h0f[j]])

                cut(9)
                for sub in range(nsub):
                    ti = tile0 + sub
                    xt, B_x = xst[sub % 2], B_xst[sub % 2]
                    hb, B_hb = hbst[sub % 2], B_hbst[sub % 2]
                    for q in range(4):
                        ps, B_p = next_ps()
                        items = [(ps[:, kk * 128:(kk + 1) * 128], h0f[:, 4 * q + kk, sub * 128:(sub + 1) * 128], ident[:]) for kk in range(4)]
                        tr_group(B_p, items, reads=[B_h0f[4 * q + kk] for kk in range(4)] + [B_const])
                        A5 = int(_os.environ.get("A5MODE", "9"))
                        if A5 >= 2:
                            S.op("act", lambda e, xt=xt, ps=ps, q=q: e.copy(out=xt[:, q * 512:(q + 1) * 512], in_=ps[:, :]), reads=[B_p], writes=[B_x])
                        if A5 >= 3:
                            S.op("dve", lambda e, hb=hb, xt=xt, q=q: e.tensor_copy(out=hb[:, q * 512:(q + 1) * 512], in_=xt[:, q * 512:(q + 1) * 512]),
                                 reads=[B_x], writes=[B_hb])
                    if A5 >= 4:
                        S.dma("sp", lambda e, xt=xt, ti=ti: e.dma_start(out=h1f_d[ti * 128:(ti + 1) * 128, :], in_=xt[:, :]), reads=[B_x])
                    if A5 >= 5:
                        S.dma("sp", lambda e, hb=hb, ti=ti: e.dma_start(out=h1b_d[ti * 128:(ti + 1) * 128, :], in_=hb[:, :]), reads=[B_hb])
                    if debug and A5 >= 6:
                        S.dma("sp", lambda e, xt=xt, ti=ti: e.dma_start(out=dbg["h1"][ti * 128:(ti + 1) * 128, :], in_=xt[:, :]), reads=[B_x])
                    cut(10)
                    pl_, B_plg = next_ps()
                    mm_group(pl_[:, 0:NE], B_plg, [(h0f[:, k, sub * 128:(sub + 1) * 128], rw[:, k, :]) for k in range(KC)],
                             reads=B_h0f + [B_rw])
                    S.op("dve", lambda e, pl_=pl_, sub=sub: e.tensor_tensor(out=lgall[:, sub, :], in0=pl_[:, 0:NE], in1=rb[:], op=ALU.add),
                         reads=[B_plg, B_rw], writes=[B_lg[sub]])

                def routing_tail(sub):
                  if True:
                    ti = tile0 + sub
                    lg = lgall[:, sub, :]
                    m8 = rt[:, 32:40]
                    i8f = rt[:, 40:48]
                    oh = rt[:, 64:64 + 4 * NE].rearrange("p (k e) -> p k e", k=4)
                    msk = rt[:, 192:192 + NE]
                    ex = rt[:, 224:228]
                    sm = rt[:, 228:229]
                    nm = rt[:, 229:230]
                    pos = rt[:, 256:256 + NE]
                    prod = rt[:, 288:288 + 4 * NE].rearrange("p (k e) -> p k e", k=4)
                    pk = rt[:, 416:420]
                    rowf = rt[:, 420:424]
                    R = [B_rt]
                    S.op("dve", lambda e, lg=lg: e.max(out=m8, in_=lg), reads=R + [B_lg[sub]], writes=R)
                    S.op("dve", lambda e, lg=lg: e.max_index(out=rti[:], in_max=m8, in_values=lg), reads=R + [B_lg[sub]], writes=R)
                    S.op("dve", lambda e: e.tensor_copy(out=i8f, in_=rti[:]), reads=R, writes=R)
                    for k in range(TOPK):
                        S.op("dve", lambda e, k=k: e.tensor_scalar(out=oh[:, k, :], in0=iota_f[:, 0:NE], scalar1=i8f[:, k:k + 1], scalar2=None,
                                                                   op0=ALU.is_equal), reads=R + [B_const], writes=R)
                    S.op("dve", lambda e: e.tensor_tensor(out=msk, in0=oh[:, 0, :], in1=oh[:, 1, :], op=ALU.add), reads=R, writes=R)
                    S.op("dve", lambda e: e.tensor_tensor(out=msk, in0=msk, in1=oh[:, 2, :], op=ALU.add), reads=R, writes=R)
                    S.op("dve", lambda e: e.tensor_tensor(out=msk, in0=msk, in1=oh[:, 3, :], op=ALU.add), reads=R, writes=R)
                    S.op("dve", lambda e, ti=ti: e.tensor_copy(out=maskb[:, ti, :], in_=msk), reads=R, writes=[B_maskb[ti]])
                    S.op("dve", lambda e: e.tensor_scalar(out=nm, in0=m8[:, 0:1], scalar1=-1.0, scalar2=None, op0=ALU.mult), reads=R, writes=R)
                    S.op("act", lambda e: e.activation(out=ex, in_=m8[:, 0:4], func=AF.Exp, bias=nm, scale=1.0), reads=R, writes=R)
                    S.op("dve", lambda e: e.reduce_sum(out=sm, in_=ex, axis=AX.X), reads=R, writes=R)
                    S.op("dve", lambda e: e.reciprocal(out=sm, in_=sm), reads=R, writes=R)
                    S.op("dve", lambda e, ti=ti: e.tensor_scalar(out=w4[:, ti, :], in0=ex, scalar1=sm, scalar2=None, op0=ALU.mult), reads=R, writes=[B_w4[ti]])
                    pp, B_pp = next_ps()
                    pairs = [(triu_b[:], maskb[:, ti, :])] + [(ones_b[:], maskb[:, t2, :]) for t2 in range(ti)]
                    mm_group(pp[:, 0:NE], B_pp, pairs, reads=[B_const] + B_maskb[0:ti + 1])
                    S.op("dve", lambda e, pp=pp: e.tensor_copy(out=pos, in_=pp[:, 0:NE]), reads=[B_pp], writes=R)
                    S.op("dve", lambda e, ti=ti: e.scalar_tensor_tensor(out=posm[:, ti, :], in0=pos, scalar=1.0, in1=msk, op0=ALU.add, op1=ALU.mult),
                         reads=R, writes=[B_posm[ti]])
                    S.op("dve", lambda e, ti=ti: e.tensor_scalar(out=posm[:, ti, :], in0=posm[:, ti, :], scalar1=-1.0, scalar2=None, op0=ALU.add),
                         reads=[B_posm[ti]], writes=[B_posm[ti]])
                    for k in range(TOPK):
                        S.op("dve", lambda e, k=k: e.tensor_tensor(out=prod[:, k, :], in0=oh[:, k, :], in1=pos, op=ALU.mult), reads=R, writes=R)
                    S.op("dve", lambda e: e.tensor_reduce(out=pk, in_=prod, axis=AX.X, op=ALU.add), reads=R, writes=R)
                    S.op("dve", lambda e: e.scalar_tensor_tensor(out=rowf, in0=i8f[:, 0:4], scalar=float(CAP), in1=pk, op0=ALU.mult, op1=ALU.add),
                         reads=R, writes=R)
                    S.op("dve", lambda e, ti=ti: e.tensor_copy(out=rows[:, ti, :], in_=rowf), reads=R, writes=[B_rows[ti]])
                    if debug:
                        S.dma("sp", lambda e, ti=ti: e.dma_start(out=dbg["route"][:, ti * 8:ti * 8 + 4], in_=rowf), reads=R)
                        S.dma("sp", lambda e, ti=ti: e.dma_start(out=dbg["route"][:, ti * 8 + 4:ti * 8 + 8], in_=w4[:, ti, :]), reads=[B_w4[ti]])
                return [(lambda sub=sub: routing_tail(sub)) for sub in range(nsub)]

            try:
                cut(1)
                tail = None
                for b in range(NB):
                    tail = mixer_block(512, HALO + 512 * b, False, 4 * b, tail, fold=(b == 0))
                for t_ in tail:
                    t_()
            except _Stop:
                upto = "A"

        if upto in ("B", "C"):
            arena_off[0] = 0

            def sbB(name, shape, dt):
                return carve(shape, dt)

            B_ringB = make_ring(4, KC * 512)
            xs = sbB("xs", [128, KC, CAP], BF16)
            act = sbB("act", [128, KC, CAP], BF16)
            sel = sbB("sel", [128, NT, CAP], BF16)
            ost = sbB("ost", [128, NST, D], F32)
            bdn = [sbB(f"bdn{i}", [128, D], BF16) for i in range(2)]
            xg = [sbB(f"xg{i}", [128, NST, D], BF16) for i in range(2)]
            idxu = [sbB(f"idxu{i}", [128, 4], U32) for i in range(2)]
            idxf = sbB("idxf", [128, 16], F32)
            tokcol = sbB("tokcol", [128, NT, 2], BF16)
            ident_b = sbB("ident_b", [128, 128], BF16)
            NTB = 8
            tb = [sbB(f"tb{i}", [128, CAP], F32) for i in range(NTB)]
            B_xs = [Buf(f"xs{k}") for k in range(KC)]
            B_act = [Buf(f"act{k}") for k in range(KC)]
            B_sel = [Buf(f"sel{i}") for i in range(NT)]
            B_ost = [Buf(f"ost{i}") for i in range(NST)]
            B_bdn = [Buf("bdn0"), Buf("bdn1")]
            B_xg = [[Buf(f"xg{i}_{st}") for st in range(NST)] for i in range(2)]
            B_idxu = [Buf("idxu0"), Buf("idxu1")]
            B_idxf, B_tok = Buf("idxf"), Buf("tokcol")
            B_tb = [Buf(f"tb{i}") for i in range(NTB)]
            tb_rr = [0]

            def next_tb():
                i = tb_rr[0]
                tb_rr[0] = (i + 1) % NTB
                return tb[i], B_tb[i]

            phase_barrier(B_ringB + B_xs + B_act + B_sel + B_ost + B_bdn + B_xg[0] + B_xg[1] + B_idxu + [B_idxf, B_tok] + B_tb)

            for ti in range(NT):
                S.op("dve", lambda e, ti=ti: e.tensor_copy(out=tokcol[:, ti, 0:1], in_=pidx_f[:, 0:1]), reads=[B_const], writes=[B_tok])
                S.op("dve", lambda e, ti=ti: e.memset(tokcol[:, ti, 1:2], float(ti)), writes=[B_tok])
            S.op("dve", lambda e: e.tensor_copy(out=ident_b, in_=ident[:]), reads=[B_const], writes=[B_tok])

            def prep_sel(ex_):
                for ti in range(NT):
                    S.op("dve", lambda e, ti=ti, ex_=ex_: e.tensor_scalar(out=sel[:, ti, :], in0=iota_f[:, 0:CAP], scalar1=posm[:, ti, ex_:ex_ + 1],
                                                                          scalar2=None, op0=ALU.is_equal),
                         reads=[B_posm[ti], B_const], writes=[B_sel[ti]])

            def prep_idx(ex_):
                bi = ex_ % 2
                for st in range(NST):
                    pi, B_pi = next_ps()
                    mm_group(pi[:, 0:2], B_pi, [(sel[:, ti, st * 128:(st + 1) * 128], tokcol[:, ti, :]) for ti in range(NT)],
                             reads=B_sel + [B_tok])
                    S.op("dve", lambda e, pi=pi, st=st: e.tensor_copy(out=idxf[:, 4 + 2 * st:6 + 2 * st], in_=pi[:, 0:2]), reads=[B_pi], writes=[B_idxf])
                    S.op("dve", lambda e, st=st: e.scalar_tensor_tensor(out=idxf[:, st:st + 1], in0=idxf[:, 5 + 2 * st:6 + 2 * st], scalar=128.0,
                                                                        in1=idxf[:, 4 + 2 * st:5 + 2 * st], op0=ALU.mult, op1=ALU.add),
                         reads=[B_idxf], writes=[B_idxf])
                S.op("dve", lambda e, bi=bi: e.tensor_copy(out=idxu[bi][:, 0:NST], in_=idxf[:, 0:NST]), reads=[B_idxf], writes=[B_idxu[bi]])

            def prep_gather(ex_):
                bi = ex_ % 2
                for st in range(NST):
                    S.dma("pool", lambda e, bi=bi, st=st: e.indirect_dma_start(
                        out=xg[bi][:, st, :], out_offset=None, in_=h1b_d[:, :],
                        in_offset=bass.IndirectOffsetOnAxis(ap=idxu[bi][:, st:st + 1], axis=0)),
                        reads=[B_idxu[bi]], writes=[B_xg[bi][st]])

            prep_sel(0)
            prep_idx(0)
            prep_gather(0)
            for ex_ in range(NE):
                r0 = ex_ * D
                bi = ex_ % 2
                bd, B_bd = bdn[ex_ % 2], B_bdn[ex_ % 2]
                S.dma("pool", lambda e, bd=bd, ex_=ex_: e.dma_start(out=bd[0:1, :], in_=b_dn[ex_:ex_ + 1, :]), writes=[B_bd])
                if ex_ + 1 < NE:
                    prep_sel(ex_ + 1)
                for m in range(KC):
                    ps, B_p = next_ps()
                    psb = ps[:, 0:CAP // 2].bitcast(BF16)
                    items = [(psb[:, st * 128:(st + 1) * 128], xg[bi][:, st, m * 128:(m + 1) * 128], ident_b) for st in range(NST)]
                    tr_group(B_p, items, reads=B_xg[bi] + [B_tok])
                    S.op("act", lambda e, psb=psb, m=m: e.copy(out=xs[:, m, :], in_=psb), reads=[B_p], writes=[B_xs[m]])
                for q in range(4):
                    if q == 1 and ex_ + 1 < NE:
                        prep_idx(ex_ + 1)
                    if q == 2 and ex_ + 1 < NE:
                        prep_gather(ex_ + 1)
                    wg = load_w(w_gu[r0:r0 + D, 512 * q:512 * q + 512], ncols=512)
                    wl = load_w(w_gu[r0:r0 + D, D + 512 * q:D + 512 * q + 512], ncols=512)
                    for kk in range(4):
                        j = 4 * q + kk
                        pg, B_pg = next_ps()
                        mm_group(pg[:, 0:CAP], B_pg, [(wg[0][:, k, kk * 128:(kk + 1) * 128], xs[:, k, :]) for k in range(KC)], reads=[wg[1]] + B_xs)
                        pli, B_pli = next_ps()
                        mm_group(pli[:, 0:CAP], B_pli, [(wl[0][:, k, kk * 128:(kk + 1) * 128], xs[:, k, :]) for k in range(KC)], reads=[wl[1]] + B_xs)
                        gt, B_gt = next_tb()
                        sg, B_sg = next_tb()
                        l1, B_l1 = next_tb()
                        cb = FO["bg"] + ex_ * 16 + j
                        cl = FO["bl"] + ex_ * 16 + j
                        S.op("dve", lambda e, gt=gt, pg=pg, cb=cb: e.tensor_scalar(out=gt[:], in0=pg[:, 0:CAP], scalar1=fv[:, cb:cb + 1], scalar2=LIMIT,
                                                                                  op0=ALU.add, op1=ALU.min), reads=[B_pg, B_fv], writes=[B_gt])
                        S.op("act", lambda e, sg=sg, gt=gt: e.activation(out=sg[:], in_=gt[:], func=AF.Sigmoid, scale=SW_ALPHA), reads=[B_gt], writes=[B_sg])
                        S.op("dve", lambda e, l1=l1, pli=pli, cl=cl: e.tensor_scalar(out=l1[:], in0=pli[:, 0:CAP], scalar1=fv[:, cl:cl + 1], scalar2=LIMIT + 1.0,
                                                                                    op0=ALU.add, op1=ALU.min), reads=[B_pli, B_fv], writes=[B_l1])
                        S.op("dve", lambda e, gt=gt, sg=sg: e.tensor_tensor(out=gt[:], in0=gt[:], in1=sg[:], op=ALU.mult), reads=[B_gt, B_sg], writes=[B_gt])
                        S.op("dve", lambda e, l1=l1, gt=gt, j=j: e.scalar_tensor_tensor(out=act[:, j, :], in0=l1[:], scalar=1.0 - LIMIT, in1=gt[:],
                                                                                        op0=ALU.max, op1=ALU.mult), reads=[B_l1, B_gt], writes=[B_act[j]])
                for nb in range(4):
                    wd = load_w(w_dn[r0:r0 + D, 512 * nb:512 * nb + 512], ncols=512)
                    for st in range(NST):
                        po, B_po = next_ps()
                        pairs = [(act[:, j, st * 128:(st + 1) * 128], wd[0][:, j, :]) for j in range(KC)]
                        pairs.append((ones_b[0:1, :], bd[0:1, 512 * nb:512 * nb + 512]))
                        mm_group(po[:, 0:512], B_po, pairs, reads=[wd[1], B_bd, B_const] + B_act)
                        if (nb + st) % 2:
                            S.op("act", lambda e, po=po, st=st, nb=nb: e.copy(out=ost[:, st, 512 * nb:512 * nb + 512], in_=po[:, 0:512]),
                                 reads=[B_po], writes=[B_ost[st]])
                        else:
                            S.op("dve", lambda e, po=po, st=st, nb=nb: e.tensor_copy(out=ost[:, st, 512 * nb:512 * nb + 512], in_=po[:, 0:512]),
                                 reads=[B_po], writes=[B_ost[st]])
                for st in range(NST):
                    S.dma("sp", lambda e, st=st, ex_=ex_: e.dma_start(out=contrib[ex_ * CAP + st * 128:ex_ * CAP + (st + 1) * 128, :], in_=ost[:, st, :]),
                          reads=[B_ost[st]], writes=[])

        if upto == "C":
            arena_off[0] = 0

            def sbC(name, shape, dt):
                return carve(shape, dt)

            NG = 12
            G = [sbC(f"G{i}", [128, D], F32) for i in range(NG)]
            hh = [sbC(f"hh{i}", [128, D], F32) for i in range(2)]
            lg2 = sbC("lg2", [128, 2 * D], F32)
            stat2 = sbC("stat2", [128, 4, 6], F32)
            mv2 = sbC("mv2", [128, 8], F32)
            B_G = [Buf(f"G{i}") for i in range(NG)]
            B_hh = [Buf("hh0"), Buf("hh1")]
            B_lg2, B_stat2, B_mv2 = Buf("lg2"), Buf("stat2"), Buf("mv2")
            phase_barrier(B_G + B_hh + [B_lg2, B_stat2, B_mv2])
            S.dma("sp", lambda e: e.dma_start(out=lg2[:], in_=rvec[:, 0:2 * D]), writes=[B_lg2])
            final = []
            for ti in range(NT):
                h, B_h = hh[ti % 2], B_hh[ti % 2]
                S.dma("sp", lambda e, h=h, ti=ti: e.dma_start(out=h[:], in_=h1f_d[ti * 128:(ti + 1) * 128, :]), writes=[B_h])
                S.op("act", lambda e, h=h: e.mul(out=h[:], in_=h[:], mul=ALPHA), reads=[B_h], writes=[B_h])
                for k in range(TOPK):
                    gi = (ti * TOPK + k) % NG
                    S.dma("pool", lambda e, gi=gi, ti=ti, k=k: e.indirect_dma_start(
                        out=G[gi][:], out_offset=None, in_=contrib[:, :],
                        in_offset=bass.IndirectOffsetOnAxis(ap=rows[:, ti, k:k + 1], axis=0)),
                        reads=[B_rows[ti]], writes=[B_G[gi]])
                    S.op("dve", lambda e, gi=gi, h=h, ti=ti, k=k: e.scalar_tensor_tensor(out=h[:], in0=G[gi][:], scalar=w4[:, ti, k:k + 1], in1=h[:],
                                                                                         op0=ALU.mult, op1=ALU.add),
                         reads=[B_G[gi], B_h, B_w4[ti]], writes=[B_h])
                for c in range(4):
                    S.op("dve", lambda e, c=c, h=h: e.bn_stats(out=stat2[:, c, :], in_=h[:, c * 512:(c + 1) * 512]), reads=[B_h], writes=[B_stat2])
                S.op("dve", lambda e: e.bn_aggr(out=mv2[:, 0:2], in_=stat2[:, :, :].rearrange("p a b -> p (a b)")), reads=[B_stat2], writes=[B_mv2])
                S.op("dve", lambda e: e.tensor_scalar(out=mv2[:, 2:3], in0=mv2[:, 1:2], scalar1=LN_EPS, scalar2=None, op0=ALU.add),
                     reads=[B_mv2], writes=[B_mv2])
                S.op("act", lambda e: e.activation(out=mv2[:, 2:3], in_=mv2[:, 2:3], func=AF.Sqrt), reads=[B_mv2], writes=[B_mv2])
                S.op("dve", lambda e: e.reciprocal(out=mv2[:, 2:3], in_=mv2[:, 2:3]), reads=[B_mv2], writes=[B_mv2])
                S.op("dve", lambda e: e.tensor_scalar(out=mv2[:, 3:4], in0=mv2[:, 0:1], scalar1=mv2[:, 2:3], scalar2=-1.0, op0=ALU.mult, op1=ALU.mult),
                     reads=[B_mv2], writes=[B_mv2])
                S.op("act", lambda e, h=h: e.activation(out=h[:], in_=h[:], func=AF.Identity, bias=mv2[:, 3:4], scale=mv2[:, 2:3]),
                     reads=[B_mv2, B_h], writes=[B_h])
                S.op("dve", lambda e, h=h: e.tensor_tensor(out=h[:], in0=h[:], in1=lg2[:, 0:D], op=ALU.mult), reads=[B_h, B_lg2], writes=[B_h])
                S.op("dve", lambda e, h=h: e.tensor_tensor(out=h[:], in0=h[:], in1=lg2[:, D:2 * D], op=ALU.add), reads=[B_h, B_lg2], writes=[B_h])
                final.append(S.dma("sp", lambda e, h=h, ti=ti: e.dma_start(out=out[ti * 128:(ti + 1) * 128, :], in_=h[:]), reads=[B_h]))

        all_dma = [(S.dsem[q][i], S.dcnt[q][i]) for q in ("sp", "pool") for i in range(S.nds) if S.dcnt[q][i] > 0]
        with nc.Block() as block:
            @block.tensor
            def _(e):
                S.emit("pe", e)

            @block.scalar
            def _(e):
                S.emit("act", e)

            @block.vector
            def _(e):
                S.emit("dve", e)

            @block.gpsimd
            def _(e):
                S.emit("pool", e)

            @block.sync
            def _(e):
                S.emit("sp", e, final_waits=all_dma)
    return nc


def _fm(v):
    v = np.asarray(v, np.float32)
    return np.ascontiguousarray(v.reshape(-1, 128).T)


def make_core_inputs(c, T, NE, x, meta_tokens, ln_in_g, ln_in_b, w_in, conv_w, w_a_out, pool_w, pool_scale, w_o,
                     ln1_g, ln1_b, router_w, router_b, w_gate_up, b_gate_up, w_down, b_down, ln2_g, ln2_b, shared):
    halves = x.shape[1] // T
    b, h = divmod(c, halves)
    if h == 0:
        xin = np.concatenate([meta_tokens.astype(np.float32), x[b, 0:T]], axis=0)
    else:
        xin = x[b, h * T - HALO:(h + 1) * T]
    d = dict(shared)
    d["xin"] = np.ascontiguousarray(xin, dtype=np.float32)
    return d


def make_shared(NE, ln_in_g, ln_in_b, w_in, conv_w, w_a_out, pool_w, pool_scale, w_o, ln1_g, ln1_b, router_w, router_b,
                w_gate_up, b_gate_up, w_down, b_down, ln2_g, ln2_b):
    cols = [_fm(ln_in_g), _fm(ln_in_b), _fm(conv_w[0, 0]), _fm(conv_w[0, 1]), _fm(conv_w[0, 2]), _fm(pool_scale[0]),
            _fm(ln1_g[0]), _fm(ln1_b[0])]
    bgu = np.asarray(b_gate_up[0], np.float32)
    bg = np.concatenate([_fm(bgu[e, :D]) for e in range(NE)], axis=1)
    bl = np.concatenate([_fm(bgu[e, D:]) for e in range(NE)], axis=1)
    fvec = np.ascontiguousarray(np.concatenate(cols + [bg, bl], axis=1), dtype=np.float32)
    rrow = np.concatenate([np.asarray(ln2_g[0], np.float32), np.asarray(ln2_b[0], np.float32), np.asarray(router_b[0], np.float32)])
    rvec = np.ascontiguousarray(np.broadcast_to(rrow[None, :], (128, rrow.shape[0])), dtype=np.float32)
    return {
        "w_in": np.ascontiguousarray(w_in[0], dtype=np.float32),
        "w_a": np.ascontiguousarray(w_a_out[0], dtype=np.float32),
        "w_o": np.ascontiguousarray(w_o[0], dtype=np.float32),
        "pool_w": np.ascontiguousarray(np.asarray(pool_w[0], np.float32).reshape(1024, 512)),
        "router_w": np.ascontiguousarray(router_w[0], dtype=np.float32),
        "w_gu": np.ascontiguousarray(np.asarray(w_gate_up[0], np.float32).reshape(NE * D, 2 * D)),
        "w_dn": np.ascontiguousarray(np.asarray(w_down[0], np.float32).reshape(NE * D, D)),
        "b_dn": np.ascontiguousarray(b_down[0], dtype=np.float32),
        "fvec": fvec,
        "rvec": rvec,
    }


def kernel(**inputs):
    inputs = {k: np.asarray(v) for k, v in inputs.items()}
    x = inputs["x"]
    B, SEQ, _ = x.shape
    NE = inputs["router_w"].shape[-1]
    n_cores = 8
    T = B * SEQ // n_cores
    CAP = 384
    names = ["ln_in_g", "ln_in_b", "w_in", "conv_w", "w_a_out", "pool_w", "pool_scale", "w_o", "ln1_g", "ln1_b", "router_w",
             "router_b", "w_gate_up", "b_gate_up", "w_down", "b_down", "ln2_g", "ln2_b"]
    shared = make_shared(NE, *[inputs[n] for n in names])
    in_maps = [make_core_inputs(c, T, NE, x, inputs["meta_tokens"], *[inputs[n] for n in names], shared) for c in range(n_cores)]
    nc = build_nc(T, NE, CAP)
    res = run_bass_kernel_spmd(nc, in_maps, core_ids=list(range(n_cores)))
    halves = SEQ // T
    outp = np.empty((B, SEQ, D), np.float32)
    for c in range(n_cores):
        b, h = divmod(c, halves)
        outp[b, h * T:(h + 1) * T] = res.results[c]["out"]
    return outp
```

```python
import numpy as np
from contextlib import ExitStack
import concourse.bass as bass
import concourse.mybir as mybir
from concourse.bass_utils import run_bass_kernel_spmd

F32 = mybir.dt.float32
BF16 = mybir.dt.bfloat16
U32 = mybir.dt.uint32
I32 = mybir.dt.int32
AF = mybir.ActivationFunctionType
ALU = mybir.AluOpType
AX = mybir.AxisListType

D = 2048
KC = 16
DIN = 11264
NMETA = 16
HALO = 16
TOPK = 4
LN_EPS = 1e-5
ALPHA = 2.0 ** 0.25
LIMIT = 7.0
SW_ALPHA = 1.702
WCOLS = 256
NWR = 5


class Buf:
    __slots__ = ("name", "w", "r")

    def __init__(self, name):
        self.name = name
        self.w = None
        self.r = {}


class Sched:
    def __init__(self, nc, es):
        self.nc = nc
        self.es = es
        self.ops = {k: [] for k in ("pe", "act", "dve", "pool", "sp")}
        self.sem = {k: es.enter_context(nc.semaphore("s_" + k)) for k in ("pe", "act", "dve", "poolc")}
        self.cnt = {k: 0 for k in self.sem}
        self.nds = 8
        self.dsem = {q: [es.enter_context(nc.semaphore(f"d_{q}{i}")) for i in range(self.nds)] for q in ("sp", "pool")}
        self.dcnt = {q: [0] * self.nds for q in ("sp", "pool")}
        self.dnext = {q: 0 for q in ("sp", "pool")}

    def _deps(self, reads, writes):
        deps = []
        for b in reads:
            if b.w is not None:
                deps.append(b.w)
        for b in writes:
            if b.w is not None:
                deps.append(b.w)
            deps.extend(b.r.values())
        return deps

    def _commit(self, tok, reads, writes):
        for b in writes:
            b.w = tok
            b.r = {}
        for b in reads:
            if b in writes:
                continue
            s, v = tok
            if b.r.get(id(s), (None, 0))[1] < v:
                b.r[id(s)] = (s, v)

    def op(self, eng, fn, reads=(), writes=()):
        skey = "poolc" if eng == "pool" else eng
        deps = self._deps(reads, writes)
        self.cnt[skey] += 1
        tok = (self.sem[skey], self.cnt[skey])
        self.ops[eng].append((fn, deps, tok, 1))
        self._commit(tok, reads, writes)
        return tok

    def dma(self, q, fn, reads=(), writes=()):
        deps = self._deps(reads, writes)
        i = self.dnext[q]
        self.dnext[q] = (i + 1) % self.nds
        sem = self.dsem[q][i]
        if self.dcnt[q][i] > 0:
            deps.append((sem, self.dcnt[q][i]))
        self.dcnt[q][i] += 16
        tok = (sem, self.dcnt[q][i])
        self.ops[q].append((fn, deps, tok, 16))
        self._commit(tok, reads, writes)
        return tok

    def emit(self, eng, e, final_waits=()):
        waited = {}
        for fn, deps, tok, inc in self.ops[eng]:
            for s, v in deps:
                k = id(s)
                if waited.get(k, 0) < v:
                    e.wait_ge(s, v)
                    waited[k] = v
            ins = fn(e)
            ins.then_inc(tok[0], inc)
        for s, v in final_waits:
            e.wait_ge(s, v)


class _Stop(Exception):
    pass


def build_nc(T, NE, CAP, debug=False, upto="C", cutv=0):
    assert T % 512 == 0 and CAP % 128 == 0 and CAP <= 512
    NT = T // 128
    NST = CAP // 128
    NB = T // 512
    nc = bass.Bass("TRN2", target_bir_lowering=False)

    def din(name, shape, dt=F32):
        return nc.dram_tensor(name, shape, dt, kind="ExternalInput").ap()

    xin = din("xin", [T + HALO, D])
    w_in = din("w_in", [D, DIN])
    w_a = din("w_a", [D, D])
    w_o = din("w_o", [D, D])
    pool_w = din("pool_w", [1024, 512])
    router_w = din("router_w", [D, NE])
    w_gu = din("w_gu", [NE * D, 2 * D])
    w_dn = din("w_dn", [NE * D, D])
    b_dn = din("b_dn", [NE, D])
    NF = 8 * 16 + 2 * NE * 16
    fvec = din("fvec", [128, NF])
    NR = 2 * D + NE
    rvec = din("rvec", [128, NR])
    out = nc.dram_tensor("out", [T, D], F32, kind="ExternalOutput").ap()
    import os as _os
    SCR = _os.environ.get("SCRKIND", "Internal")
    h1f_d = nc.dram_tensor("h1f_d", [T, D], F32, kind=SCR).ap()
    h1b_d = nc.dram_tensor("h1b_d", [T, D], BF16, kind=SCR).ap()
    contrib = nc.dram_tensor("contrib", [NE * CAP, D], F32, kind=SCR).ap()
    dbg = {}
    if debug:
        dbg["h1"] = nc.dram_tensor("dbg_h1", [T, D], F32, kind="ExternalOutput").ap()
        dbg["route"] = nc.dram_tensor("dbg_route", [128, NT * 8], F32, kind="ExternalOutput").ap()
        dbg["za"] = nc.dram_tensor("dbg_za", [128, KC * 512], BF16, kind="ExternalOutput").ap()
        dbg["pl"] = nc.dram_tensor("dbg_pl", [128, 8 * 512], BF16, kind="ExternalOutput").ap()
        dbg["mg"] = nc.dram_tensor("dbg_mg", [128, KC * 512], BF16, kind="ExternalOutput").ap()
        dbg["h0b"] = nc.dram_tensor("dbg_h0b", [128, KC * 512], BF16, kind="ExternalOutput").ap()

    FO = dict(lng=0, lnb=16, cw0=32, cw1=48, cw2=64, psc=80, l1g=96, l1b=112, bg=128, bl=128 + NE * 16)

    with ExitStack() as es:
        S = Sched(nc, es)

        def sb(name, shape, dt):
            return es.enter_context(nc.sbuf_tensor(name, shape, dt))

        fv = sb("fv", [128, NF], F32)
        ident = sb("ident", [128, 128], F32)
        ones_f = sb("ones_f", [128, 128], F32)
        ones_b = sb("ones_b", [128, 128], BF16)
        triu_b = sb("triu_b", [128, 128], BF16)
        iota_i = sb("iota_i", [128, 512], I32)
        iota_f = sb("iota_f", [128, 512], F32)
        pidx_i = sb("pidx_i", [128, 1], I32)
        pidx_f = sb("pidx_f", [128, 1], F32)
        rw = sb("rw", [128, KC, NE], F32)
        rb = sb("rb", [128, NE], F32)
        maskb = sb("maskb", [128, NT, NE], BF16)
        posm = sb("posm", [128, NT, NE], F32)
        w4 = sb("w4", [128, NT, 4], F32)
        rows = sb("rows", [128, NT, 4], U32)
        B_fv, B_const, B_rw = Buf("fv"), Buf("const"), Buf("rw")
        B_maskb = [Buf(f"maskb{i}") for i in range(NT)]
        B_posm = [Buf(f"posm{i}") for i in range(NT)]
        B_w4 = [Buf(f"w4{i}") for i in range(NT)]
        B_rows = [Buf(f"rows{i}") for i in range(NT)]

        S.dma("sp", lambda e: e.dma_start(out=fv[:], in_=fvec[:, :]), writes=[B_fv])
        S.dma("sp", lambda e: e.dma_start(out=rw[:], in_=router_w.rearrange("(k p) n -> p k n", p=128)), writes=[B_rw])
        S.dma("sp", lambda e: e.dma_start(out=rb[:], in_=rvec[:, 2 * D:2 * D + NE]), writes=[B_rw])
        S.op("pool", lambda e: e.iota(iota_i[:], pattern=[[1, 512]], base=0, channel_multiplier=0), writes=[B_const])
        S.op("pool", lambda e: e.iota(pidx_i[:], pattern=[[1, 1]], base=0, channel_multiplier=1), writes=[B_const])
        S.op("dve", lambda e: e.tensor_copy(out=iota_f[:], in_=iota_i[:]), reads=[B_const], writes=[B_const])
        S.op("dve", lambda e: e.tensor_copy(out=pidx_f[:], in_=pidx_i[:]), reads=[B_const], writes=[B_const])
        S.op("dve", lambda e: e.tensor_scalar(out=ident[:], in0=iota_f[:, 0:128], scalar1=pidx_f[:, 0:1], scalar2=None,
                                              op0=ALU.is_equal), reads=[B_const], writes=[B_const])
        S.op("dve", lambda e: e.tensor_scalar(out=triu_b[:], in0=iota_f[:, 0:128], scalar1=pidx_f[:, 0:1], scalar2=None,
                                              op0=ALU.is_gt), reads=[B_const], writes=[B_const])
        S.op("dve", lambda e: e.memset(ones_f[:], 1.0), writes=[B_const])
        S.op("dve", lambda e: e.memset(ones_b[:], 1.0), writes=[B_const])
        S.op("dve", lambda e: e.tensor_scalar(out=fv[:, FO["bl"]:FO["bl"] + NE * 16], in0=fv[:, FO["bl"]:FO["bl"] + NE * 16],
                                              scalar1=1.0, scalar2=None, op0=ALU.add), reads=[B_fv], writes=[B_fv])

        def cut(k):
            if cutv == k:
                raise _Stop()

        def fcol(key, k):
            c = FO[key] + k
            return fv[:, c:c + 1]

        NARENA = 47616
        arena_t = sb("arena", [128, NARENA], F32)
        arena_off = [0]

        def carve(shape, dt):
            n = 1
            for d_ in shape[1:]:
                n *= d_
            nbytes = n * (2 if dt == BF16 else 4)
            nf = (nbytes + 7) // 8 * 2
            off = arena_off[0]
            assert off + nf <= NARENA, ("arena overflow", off, nf)
            arena_off[0] = off + nf
            v = arena_t[:, off:off + nf]
            if dt != F32:
                v = v.bitcast(dt)
            v = v[:, 0:n]
            if len(shape) == 3:
                v = v.rearrange("p (a b) -> p a b", a=shape[1])
            return v

        def phase_barrier(bufs):
            toks = [(S.sem[k], S.cnt[k]) for k in ("pe", "act", "dve") if S.cnt[k] > 0]
            toks += [(S.dsem[q][i], S.dcnt[q][i]) for q in ("sp", "pool") for i in range(S.nds) if S.dcnt[q][i] > 0]
            for bb in bufs:
                for tk in toks:
                    bb.r[id(tk[0])] = tk

        psum = [es.enter_context(nc.psum_tensor(f"ps{i}", [128, 512], F32)) for i in range(8)]
        B_ps = [Buf(f"ps{i}") for i in range(8)]
        ps_rr = [0]
        ps_reserved = set()

        def next_ps():
            while ps_rr[0] in ps_reserved:
                ps_rr[0] = (ps_rr[0] + 1) % 8
            i = ps_rr[0]
            ps_rr[0] = (i + 1) % 8
            return psum[i], B_ps[i]

        ring = {"bufs": [], "B": [], "rr": 0}

        def make_ring(n, elems):
            ring["bufs"] = [carve([128, elems], BF16) for _ in range(n)]
            ring["B"] = [Buf(f"wr{i}") for i in range(n)]
            ring["rr"] = 0
            return ring["B"]

        def load_w(src_ap, kk=KC, ncols=WCOLS):
            i = ring["rr"]
            ring["rr"] = (i + 1) % len(ring["bufs"])
            view = ring["bufs"][i][:, 0:kk * ncols].rearrange("p (k n) -> p k n", k=kk)
            Bw = ring["B"][i]
            S.dma("pool", lambda e: e.dma_start(out=view, in_=src_ap.rearrange("(k p) n -> p k n", p=128)), writes=[Bw])
            return view, Bw

        def mm_group(ps_ap, B_out, pairs, reads):
            def fn(e):
                ins = None
                n = len(pairs)
                for i, (l, r) in enumerate(pairs):
                    ins = e.matmul(ps_ap, lhsT=l, rhs=r, start=(i == 0), stop=(i == n - 1))
                return ins
            return S.op("pe", fn, reads=reads, writes=[B_out])

        def tr_group(B_out, items, reads):
            def fn(e):
                ins = None
                for o, i_, idn in items:
                    ins = e.transpose(o, i_, idn)
                return ins
            return S.op("pe", fn, reads=reads, writes=[B_out])

        if True:
            def sbA(name, shape, dt):
                return carve(shape, dt)

            make_ring(NWR, KC * WCOLS)
            h0f = sbA("h0f", [128, KC, 512], F32)
            h0b = sbA("h0b", [128, KC, 512], BF16)
            za = sbA("za", [128, KC, 512], BF16)
            pl = sbA("pl", [128, 8, 512], BF16)
            mg = sbA("mg", [128, KC, 512], BF16)
            xst = [sbA(f"xst{i}", [128, D], F32) for i in range(2)]
            hbst = [sbA(f"hbst{i}", [128, D], BF16) for i in range(2)]
            cuh = sbA("cuh", [128, KC, 16], F32)
            vh = sbA("vh", [128, 8, 16], F32)
            NTMP = 8
            tmp = [sbA(f"tmp{i}", [128, 528], F32) for i in range(NTMP)]
            stat = sbA("stat", [128, 4, 6], F32)
            mv = sbA("mv", [128, 8], F32)
            rt = sbA("rt", [128, 512], F32)
            rti = sbA("rti", [128, 8], U32)
            lgall = sbA("lgall", [128, 4, NE], F32)
            B_lg = [Buf(f"lg{i}") for i in range(4)]
            B_h0f = [Buf(f"h0f{k}") for k in range(KC)]
            B_h0b = [Buf(f"h0b{k}") for k in range(KC)]
            B_za = [Buf(f"za{k}") for k in range(KC)]
            B_pl = [Buf(f"pl{k}") for k in range(8)]
            B_mg = [Buf(f"mg{k}") for k in range(KC)]
            B_xst = [Buf("xst0"), Buf("xst1")]
            B_hbst = [Buf("hbst0"), Buf("hbst1")]
            B_cuh = [Buf(f"cuh{k}") for k in range(KC)]
            B_vh = [Buf(f"vh{k}") for k in range(8)]
            B_tmp = [Buf(f"tmp{i}") for i in range(NTMP)]
            B_stat, B_mv, B_rt = Buf("stat"), Buf("mv"), Buf("rt")
            wpl_t = sbA("wpl", [128, 8, 512], BF16)
            B_wpl = Buf("wpl")
            S.dma("pool", lambda e: e.dma_start(out=wpl_t, in_=pool_w.rearrange("(k p) n -> p k n", p=128)), writes=[B_wpl])
            tmp_rr = [0]

            def next_tmp():
                i = tmp_rr[0]
                tmp_rr[0] = (i + 1) % NTMP
                return tmp[i], B_tmp[i]

            ev_rr = [0]

            def evac_copy(out_ap, in_ap, reads, writes):
                ev_rr[0] ^= 1
                if ev_rr[0]:
                    return S.op("act", lambda e: e.copy(out=out_ap, in_=in_ap), reads=reads, writes=writes)
                return S.op("dve", lambda e: e.tensor_copy(out=out_ap, in_=in_ap), reads=reads, writes=writes)

            def ln_rows(x_ap, npart, B_x):
                for c in range(4):
                    S.op("dve", lambda e, c=c: e.bn_stats(out=stat[0:npart, c, :], in_=x_ap[:, c * 512:(c + 1) * 512]),
                         reads=[B_x], writes=[B_stat] if c == 0 else [B_stat])
                S.op("dve", lambda e: e.bn_aggr(out=mv[0:npart, 0:2], in_=stat[0:npart, :, :].rearrange("p a b -> p (a b)")),
                     reads=[B_stat], writes=[B_mv])
                S.op("dve", lambda e: e.tensor_scalar(out=mv[0:npart, 2:3], in0=mv[0:npart, 1:2], scalar1=LN_EPS, scalar2=None,
                                                      op0=ALU.add), reads=[B_mv], writes=[B_mv])
                S.op("act", lambda e: e.activation(out=mv[0:npart, 2:3], in_=mv[0:npart, 2:3], func=AF.Sqrt), reads=[B_mv], writes=[B_mv])
                S.op("dve", lambda e: e.reciprocal(out=mv[0:npart, 2:3], in_=mv[0:npart, 2:3]), reads=[B_mv], writes=[B_mv])
                S.op("dve", lambda e: e.tensor_scalar(out=mv[0:npart, 3:4], in0=mv[0:npart, 0:1], scalar1=mv[0:npart, 2:3], scalar2=-1.0,
                                                      op0=ALU.mult, op1=ALU.mult), reads=[B_mv], writes=[B_mv])
                S.op("act", lambda e: e.activation(out=x_ap, in_=x_ap, func=AF.Identity, bias=mv[0:npart, 3:4],
                                                   scale=mv[0:npart, 2:3]), reads=[B_mv, B_x], writes=[B_x])

            hhf = sbA("hhf", [128, KC, 16], F32)
            hhb = sbA("hhb", [128, KC, 16], BF16)
            B_hhf, B_hhb = Buf("hhf"), Buf("hhb")

            def load_ln_T(n, r0, dst_f, dst_b, Bf, Bb, keep_f32):
                nsub = max(1, n // 128)
                npart = min(n, 128)
                for sub in range(nsub):
                    xt, B_x = xst[sub % 2], B_xst[sub % 2]
                    S.dma("sp", lambda e, xt=xt, sub=sub: e.dma_start(out=xt[0:npart, :], in_=xin[r0 + sub * 128:r0 + sub * 128 + npart, :]),
                          writes=[B_x])
                    ln_rows(xt[0:npart, :], npart, B_x)
                    for q in range(4):
                        ps, B_p = next_ps()
                        items = [(ps[:, kk * npart:(kk + 1) * npart], xt[0:npart, (4 * q + kk) * 128:(4 * q + kk + 1) * 128],
                                  ident[0:npart, 0:npart]) for kk in range(4)]
                        tr_group(B_p, items, reads=[B_x, B_const])
                        evac_copy(dst_f[:, 4 * q:4 * q + 4, sub * 128:sub * 128 + npart],
                                  ps[:, 0:4 * npart].rearrange("p (k t) -> p k t", k=4),
                                  reads=[B_p], writes=[Bf[4 * q + kk] for kk in range(4)])
                for k in range(KC):
                    S.op("act", lambda e, k=k: e.activation(out=dst_b[:, k, 0:n], in_=dst_f[:, k, 0:n], func=AF.Identity,
                                                            bias=fcol("lnb", k), scale=fcol("lng", k)),
                         reads=[Bf[k], B_fv], writes=[Bb[k]])
                    if keep_f32:
                        S.op("dve", lambda e, k=k: e.tensor_scalar(out=dst_f[:, k, 0:n], in0=dst_f[:, k, 0:n], scalar1=fcol("lng", k),
                                                                   scalar2=fcol("lnb", k), op0=ALU.mult, op1=ALU.add),
                             reads=[Bf[k], B_fv], writes=[Bf[k]])

            def mixer_block(n, r0, halo, tile0, prev_tail, fold=False):
                nsub = max(1, n // 128)
                npart = min(n, 128)
                if fold:
                    load_ln_T(HALO, 0, hhf, hhb, [B_hhf] * KC, [B_hhb] * KC, False)
                load_ln_T(n, r0, h0f, h0b, B_h0f, B_h0b, not halo)

                cut(2 if halo else 4)

                def proj(wv, B_w, kk, reads_extra=()):
                    ps, B_p = next_ps()
                    pairs = [(wv[:, k, kk * 128:(kk + 1) * 128], h0b[:, k, 0:n]) for k in range(KC)]
                    mm_group(ps[:, 0:n], B_p, pairs, reads=[B_w] + B_h0b)
                    return ps, B_p

                def proj_h(wv, B_w, kk):
                    ps, B_p = next_ps()
                    pairs = [(wv[:, k, kk * 128:(kk + 1) * 128], hhb[:, k, :]) for k in range(KC)]
                    mm_group(ps[:, 0:HALO], B_p, pairs, reads=[B_w, B_hhb])
                    return ps, B_p

                for s in range(8):
                    if prev_tail is not None and 1 <= s <= len(prev_tail):
                        prev_tail[s - 1]()
                    if halo:
                        wb = None
                    else:
                        wb = load_w(w_in[:, 256 * s:256 * s + 256])
                    wc = load_w(w_in[:, 2048 + 256 * s:2048 + 256 * s + 256])
                    wu = load_w(w_in[:, 4096 + 256 * s:4096 + 256 * s + 256])
                    for kk in range(2):
                        i = 2 * s + kk
                        pc, B_pc = proj(wc[0], wc[1], kk)
                        pu, B_pu = proj(wu[0], wu[1], kk)
                        if halo:
                            ut, B_ut = next_tmp()
                            S.op("act", lambda e, ut=ut, pu=pu: e.copy(out=ut[:, 0:n], in_=pu[:, 0:n]), reads=[B_pu], writes=[B_ut])
                            S.op("dve", lambda e, ut=ut, pc=pc, i=i: e.tensor_tensor(out=cuh[:, i, :], in0=pc[:, 0:n], in1=ut[:, 0:n], op=ALU.mult),
                                 reads=[B_pc, B_ut], writes=[B_cuh[i]])
                            continue
                        if fold:
                            pcH, B_pcH = proj_h(wc[0], wc[1], kk)
                            puH, B_puH = proj_h(wu[0], wu[1], kk)
                            utH, B_utH = next_tmp()
                            S.op("act", lambda e, utH=utH, puH=puH: e.copy(out=utH[:, 0:HALO], in_=puH[:, 0:HALO]), reads=[B_puH], writes=[B_utH])
                            S.op("dve", lambda e, utH=utH, pcH=pcH, i=i: e.tensor_tensor(out=cuh[:, i, :], in0=pcH[:, 0:HALO], in1=utH[:, 0:HALO], op=ALU.mult),
                                 reads=[B_pcH, B_utH], writes=[B_cuh[i]])
                        pb, B_pb = proj(wb[0], wb[1], kk)
                        ut, B_ut = next_tmp()
                        cu, B_cu = next_tmp()
                        t1, B_t1 = next_tmp()
                        S.op("act", lambda e, ut=ut, pu=pu: e.copy(out=ut[:, 0:n], in_=pu[:, 0:n]), reads=[B_pu], writes=[B_ut])
                        S.op("act", lambda e, cu=cu, i=i: e.copy(out=cu[:, 0:16], in_=cuh[:, i, :]), reads=[B_cuh[i]], writes=[B_cu])
                        S.op("dve", lambda e, cu=cu, pc=pc, ut=ut: e.tensor_tensor(out=cu[:, 16:16 + n], in0=pc[:, 0:n], in1=ut[:, 0:n], op=ALU.mult),
                             reads=[B_pc, B_ut], writes=[B_cu])
                        S.op("act", lambda e, cu=cu, i=i: e.copy(out=cuh[:, i, :], in_=cu[:, n:n + 16]), reads=[B_cu], writes=[B_cuh[i]])
                        S.op("dve", lambda e, t1=t1, cu=cu, i=i: e.tensor_scalar(out=t1[:, 0:n], in0=cu[:, 16:16 + n], scalar1=fcol("cw2", i),
                                                                                 scalar2=None, op0=ALU.mult), reads=[B_cu, B_fv], writes=[B_t1])
                        S.op("dve", lambda e, t1=t1, cu=cu, i=i: e.scalar_tensor_tensor(out=t1[:, 0:n], in0=cu[:, 15:15 + n], scalar=fcol("cw1", i),
                                                                                        in1=t1[:, 0:n], op0=ALU.mult, op1=ALU.add),
                             reads=[B_cu, B_fv, B_t1], writes=[B_t1])
                        S.op("dve", lambda e, t1=t1, cu=cu, i=i: e.scalar_tensor_tensor(out=t1[:, 0:n], in0=cu[:, 14:14 + n], scalar=fcol("cw0", i),
                                                                                        in1=t1[:, 0:n], op0=ALU.mult, op1=ALU.add),
                             reads=[B_cu, B_fv, B_t1], writes=[B_t1])
                        S.op("dve", lambda e, t1=t1, pb=pb, i=i: e.tensor_tensor(out=za[:, i, 0:n], in0=pb[:, 0:n], in1=t1[:, 0:n], op=ALU.mult),
                             reads=[B_pb, B_t1], writes=[B_za[i]])

                if not halo:
                    cut(5)
                for g in range(4):
                    wv_ = load_w(w_in[:, 6144 + 256 * g:6144 + 256 * g + 256])
                    W = 2 << g
                    for kk in range(2):
                        c = 2 * g + kk
                        pv, B_pv = proj(wv_[0], wv_[1], kk)
                        if halo:
                            S.op("act", lambda e, pv=pv, c=c: e.copy(out=vh[:, c, :], in_=pv[:, 0:n]), reads=[B_pv], writes=[B_vh[c]])
                            continue
                        if fold:
                            pvH, B_pvH = proj_h(wv_[0], wv_[1], kk)
                            S.op("act", lambda e, pvH=pvH, c=c: e.copy(out=vh[:, c, :], in_=pvH[:, 0:HALO]), reads=[B_pvH], writes=[B_vh[c]])
                        vs, B_vs = next_tmp()
                        S.op("act", lambda e, vs=vs, c=c: e.copy(out=vs[:, 0:16], in_=vh[:, c, :]), reads=[B_vh[c]], writes=[B_vs])
                        S.op("act", lambda e, vs=vs, pv=pv: e.copy(out=vs[:, 16:16 + n], in_=pv[:, 0:n]), reads=[B_pv], writes=[B_vs])
                        S.op("act", lambda e, vs=vs, c=c: e.copy(out=vh[:, c, :], in_=vs[:, n:n + 16]), reads=[B_vs], writes=[B_vh[c]])
                        cur, B_cur = vs, B_vs
                        sh = 1
                        while sh < W:
                            nx, B_nx = next_tmp()
                            S.op("dve", lambda e, nx=nx, cur=cur, sh=sh: e.tensor_tensor(out=nx[:, sh:16 + n], in0=cur[:, sh:16 + n],
                                                                                          in1=cur[:, 0:16 + n - sh], op=ALU.add),
                                 reads=[B_cur], writes=[B_nx])
                            cur, B_cur = nx, B_nx
                            sh *= 2
                        S.op("dve", lambda e, cur=cur, vs=vs, c=c, W=W: e.scalar_tensor_tensor(out=pl[:, c, 0:n], in0=cur[:, 16:16 + n], scalar=1.0 / W,
                                                                                                in1=vs[:, 16:16 + n], op0=ALU.mult, op1=ALU.subtract),
                             reads=[B_cur, B_vs], writes=[B_pl[c]])
                if halo:
                    return
                cut(6)

                wpl = (wpl_t, B_wpl)
                for jp in range(8):
                    wa = load_w(w_a[:, 256 * jp:256 * jp + 256])
                    wga = load_w(w_in[:, 7168 + 256 * jp:7168 + 256 * jp + 256])
                    wgb = load_w(w_in[:, 9216 + 256 * jp:9216 + 256 * jp + 256])
                    for kk in range(2):
                        j = 2 * jp + kk
                        g = j // 4
                        pya, B_pya = next_ps()
                        mm_group(pya[:, 0:n], B_pya, [(wa[0][:, i, kk * 128:(kk + 1) * 128], za[:, i, 0:n]) for i in range(KC)],
                                 reads=[wa[1]] + B_za)
                        pyb, B_pyb = next_ps()
                        mm_group(pyb[:, 0:n], B_pyb, [(wpl[0][:, 2 * g + c2, (j % 4) * 128:(j % 4 + 1) * 128], pl[:, 2 * g + c2, 0:n]) for c2 in range(2)],
                                 reads=[wpl[1], B_pl[2 * g], B_pl[2 * g + 1]])
                        pga, B_pga = proj(wga[0], wga[1], kk)
                        pgb, B_pgb = proj(wgb[0], wgb[1], kk)
                        sga, B_sga = next_tmp()
                        sgb, B_sgb = next_tmp()
                        S.op("act", lambda e, sga=sga, pga=pga: e.activation(out=sga[:, 0:n], in_=pga[:, 0:n], func=AF.Sigmoid), reads=[B_pga], writes=[B_sga])
                        S.op("act", lambda e, sgb=sgb, pgb=pgb: e.activation(out=sgb[:, 0:n], in_=pgb[:, 0:n], func=AF.Sigmoid), reads=[B_pgb], writes=[B_sgb])
                        S.op("dve", lambda e, sga=sga, pya=pya: e.tensor_tensor(out=sga[:, 0:n], in0=pya[:, 0:n], in1=sga[:, 0:n], op=ALU.mult),
                             reads=[B_pya, B_sga], writes=[B_sga])
                        S.op("dve", lambda e, sgb=sgb, pyb=pyb, j=j: e.scalar_tensor_tensor(out=sgb[:, 0:n], in0=pyb[:, 0:n], scalar=fcol("psc", j),
                                                                                            in1=sgb[:, 0:n], op0=ALU.mult, op1=ALU.mult),
                             reads=[B_pyb, B_sgb, B_fv], writes=[B_sgb])
                        S.op("dve", lambda e, sga=sga, sgb=sgb, j=j: e.tensor_tensor(out=mg[:, j, 0:n], in0=sga[:, 0:n], in1=sgb[:, 0:n], op=ALU.add),
                             reads=[B_sga, B_sgb], writes=[B_mg[j]])

                if debug and tile0 == 0:
                    S.dma("sp", lambda e: e.dma_start(out=dbg["za"].rearrange("p (k n) -> p k n", k=KC), in_=za), reads=B_za)
                    S.dma("sp", lambda e: e.dma_start(out=dbg["pl"].rearrange("p (k n) -> p k n", k=8), in_=pl), reads=B_pl)
                    S.dma("sp", lambda e: e.dma_start(out=dbg["mg"].rearrange("p (k n) -> p k n", k=KC), in_=mg), reads=B_mg)
                    S.dma("sp", lambda e: e.dma_start(out=dbg["h0b"].rearrange("p (k n) -> p k n", k=KC), in_=h0b), reads=B_h0b)
                cut(7)
                psum_s, B_pss = next_ps()
                psum_q, B_psq = next_ps()
                res_banks = [i for i in range(8) if psum[i] is psum_s or psum[i] is psum_q]
                ps_reserved.update(res_banks)

                def ln1_stats(j, sq, B_sq):
                    def fn_s(e):
                        return e.matmul(psum_s[:, 0:n], lhsT=ones_f[:], rhs=h0f[:, j, 0:n], start=(j == 0), stop=(j == KC - 1))

                    def fn_q(e):
                        return e.matmul(psum_q[:, 0:n], lhsT=ones_f[:], rhs=sq[:, 0:n], start=(j == 0), stop=(j == KC - 1))
                    S.op("pe", fn_s, reads=[B_h0f[j], B_const] + ([B_pss] if j else []), writes=[B_pss])
                    S.op("pe", fn_q, reads=[B_sq] + ([B_psq] if j else []), writes=[B_psq])

                pend = None
                for jp in range(8):
                    wo = load_w(w_o[:, 256 * jp:256 * jp + 256])
                    for kk in range(2):
                        j = 2 * jp + kk
                        pm, B_pm = next_ps()
                        mm_group(pm[:, 0:n], B_pm, [(wo[0][:, i, kk * 128:(kk + 1) * 128], mg[:, i, 0:n]) for i in range(KC)],
                                 reads=[wo[1]] + B_mg)
                        S.op("dve", lambda e, pm=pm, j=j: e.scalar_tensor_tensor(out=h0f[:, j, 0:n], in0=h0f[:, j, 0:n], scalar=ALPHA, in1=pm[:, 0:n],
                                                                                 op0=ALU.mult, op1=ALU.add), reads=[B_pm, B_h0f[j]], writes=[B_h0f[j]])
                        sq, B_sq = next_tmp()
                        S.op("act", lambda e, sq=sq, j=j: e.activation(out=sq[:, 0:n], in_=h0f[:, j, 0:n], func=AF.Square), reads=[B_h0f[j]], writes=[B_sq])
                        if pend is not None:
                            ln1_stats(*pend)
                        pend = (j, sq, B_sq)
                ln1_stats(*pend)
                for i_ in res_banks:
                    ps_reserved.discard(i_)
                cut(8)
                mean, B_mean = next_tmp()
                rstd, B_rstd = next_tmp()
                S.op("dve", lambda e: e.tensor_scalar(out=mean[:, 0:n], in0=psum_s[:, 0:n], scalar1=1.0 / D, scalar2=None, op0=ALU.mult),
                     reads=[B_pss], writes=[B_mean])
                S.op("dve", lambda e: e.tensor_tensor(out=rstd[:, 0:n], in0=mean[:, 0:n], in1=mean[:, 0:n], op=ALU.mult), reads=[B_mean], writes=[B_rstd])
                S.op("dve", lambda e: e.scalar_tensor_tensor(out=rstd[:, 0:n], in0=psum_q[:, 0:n], scalar=1.0 / D, in1=rstd[:, 0:n],
                                                             op0=ALU.mult, op1=ALU.subtract), reads=[B_psq, B_rstd], writes=[B_rstd])
                S.op("dve", lambda e: e.tensor_scalar(out=rstd[:, 0:n], in0=rstd[:, 0:n], scalar1=LN_EPS, scalar2=None, op0=ALU.add),
                     reads=[B_rstd], writes=[B_rstd])
                S.op("act", lambda e: e.activation(out=rstd[:, 0:n], in_=rstd[:, 0:n], func=AF.Sqrt), reads=[B_rstd], writes=[B_rstd])
                S.op("dve", lambda e: e.reciprocal(out=rstd[:, 0:n], in_=rstd[:, 0:n]), reads=[B_rstd], writes=[B_rstd])
                for j in range(KC):
                    S.op("dve", lambda e, j=j: e.tensor_tensor(out=h0f[:, j, 0:n], in0=h0f[:, j, 0:n], in1=mean[:, 0:n], op=ALU.subtract),
                         reads=[B_h0f[j], B_mean], writes=[B_h0f[j]])
                    S.op("dve", lambda e, j=j: e.tensor_tensor(out=h0f[:, j, 0:n], in0=h0f[:, j, 0:n], in1=rstd[:, 0:n], op=ALU.mult),
                         reads=[B_h0f[j], B_rstd], writes=[B_h0f[j]])
                    S.op("act", lambda e, j=j: e.activation(out=h0f[:, j, 0:n], in_=h0f[:, j, 0:n], func=AF.Identity, bias=fcol("l1b", j),
                                                            scale=fcol("l1g", j)), reads=[B_h0f[j], B_fv], writes=[B_h0f[j]])

                cut(9)
                for sub in range(nsub):
                    ti = tile0 + sub
                    xt, B_x = xst[sub % 2], B_xst[sub % 2]
                    hb, B_hb = hbst[sub % 2], B_hbst[sub % 2]
                    for q in range(4):
                        ps, B_p = next_ps()
                        items = [(ps[:, kk * 128:(kk + 1) * 128], h0f[:, 4 * q + kk, sub * 128:(sub + 1) * 128], ident[:]) for kk in range(4)]
                        tr_group(B_p, items, reads=[B_h0f[4 * q + kk] for kk in range(4)] + [B_const])
                        A5 = int(_os.environ.get("A5MODE", "9"))
                        if A5 >= 2:
                            S.op("act", lambda e, xt=xt, ps=ps, q=q: e.copy(out=xt[:, q * 512:(q + 1) * 512], in_=ps[:, :]), reads=[B_p], writes=[B_x])
                        if A5 >= 3:
                            S.op("dve", lambda e, hb=hb, xt=xt, q=q: e.tensor_copy(out=hb[:, q * 512:(q + 1) * 512], in_=xt[:, q * 512:(q + 1) * 512]),
                                 reads=[B_x], writes=[B_hb])
                    if A5 >= 4:
                        S.dma("sp", lambda e, xt=xt, ti=ti: e.dma_start(out=h1f_d[ti * 128:(ti + 1) * 128, :], in_=xt[:, :]), reads=[B_x])
                    if A5 >= 5:
                        S.dma("sp", lambda e, hb=hb, ti=ti: e.dma_start(out=h1b_d[ti * 128:(ti + 1) * 128, :], in_=hb[:, :]), reads=[B_hb])
                    if debug and A5 >= 6:
                        S.dma("sp", lambda e, xt=xt, ti=ti: e.dma_start(out=dbg["h1"][ti * 128:(ti + 1) * 128, :], in_=xt[:, :]), reads=[B_x])
                    cut(10)
                    pl_, B_plg = next_ps()
                    mm_group(pl_[:, 0:NE], B_plg, [(h0f[:, k, sub * 128:(sub + 1) * 128], rw[:, k, :]) for k in range(KC)],
                             reads=B_h0f + [B_rw])
                    S.op("dve", lambda e, pl_=pl_, sub=sub: e.tensor_tensor(out=lgall[:, sub, :], in0=pl_[:, 0:NE], in1=rb[:], op=ALU.add),
                         reads=[B_plg, B_rw], writes=[B_lg[sub]])

                def routing_tail(sub):
                  if True:
                    ti = tile0 + sub
                    lg = lgall[:, sub, :]
                    m8 = rt[:, 32:40]
                    i8f = rt[:, 40:48]
                    oh = rt[:, 64:64 + 4 * NE].rearrange("p (k e) -> p k e", k=4)
                    msk = rt[:, 192:192 + NE]
                    ex = rt[:, 224:228]
                    sm = rt[:, 228:229]
                    nm = rt[:, 229:230]
                    pos = rt[:, 256:256 + NE]
                    prod = rt[:, 288:288 + 4 * NE].rearrange("p (k e) -> p k e", k=4)
                    pk = rt[:, 416:420]
                    rowf = rt[:, 420:424]
                    R = [B_rt]
                    S.op("dve", lambda e, lg=lg: e.max(out=m8, in_=lg), reads=R + [B_lg[sub]], writes=R)
                    S.op("dve", lambda e, lg=lg: e.max_index(out=rti[:], in_max=m8, in_values=lg), reads=R + [B_lg[sub]], writes=R)
                    S.op("dve", lambda e: e.tensor_copy(out=i8f, in_=rti[:]), reads=R, writes=R)
                    for k in range(TOPK):
                        S.op("dve", lambda e, k=k: e.tensor_scalar(out=oh[:, k, :], in0=iota_f[:, 0:NE], scalar1=i8f[:, k:k + 1], scalar2=None,
                                                                   op0=ALU.is_equal), reads=R + [B_const], writes=R)
                    S.op("dve", lambda e: e.tensor_tensor(out=msk, in0=oh[:, 0, :], in1=oh[:, 1, :], op=ALU.add), reads=R, writes=R)
                    S.op("dve", lambda e: e.tensor_tensor(out=msk, in0=msk, in1=oh[:, 2, :], op=ALU.add), reads=R, writes=R)
                    S.op("dve", lambda e: e.tensor_tensor(out=msk, in0=msk, in1=oh[:, 3, :], op=ALU.add), reads=R, writes=R)
                    S.op("dve", lambda e, ti=ti: e.tensor_copy(out=maskb[:, ti, :], in_=msk), reads=R, writes=[B_maskb[ti]])
                    S.op("dve", lambda e: e.tensor_scalar(out=nm, in0=m8[:, 0:1], scalar1=-1.0, scalar2=None, op0=ALU.mult), reads=R, writes=R)
                    S.op("act", lambda e: e.activation(out=ex, in_=m8[:, 0:4], func=AF.Exp, bias=nm, scale=1.0), reads=R, writes=R)
                    S.op("dve", lambda e: e.reduce_sum(out=sm, in_=ex, axis=AX.X), reads=R, writes=R)
                    S.op("dve", lambda e: e.reciprocal(out=sm, in_=sm), reads=R, writes=R)
                    S.op("dve", lambda e, ti=ti: e.tensor_scalar(out=w4[:, ti, :], in0=ex, scalar1=sm, scalar2=None, op0=ALU.mult), reads=R, writes=[B_w4[ti]])
                    pp, B_pp = next_ps()
                    pairs = [(triu_b[:], maskb[:, ti, :])] + [(ones_b[:], maskb[:, t2, :]) for t2 in range(ti)]
                    mm_group(pp[:, 0:NE], B_pp, pairs, reads=[B_const] + B_maskb[0:ti + 1])
                    S.op("dve", lambda e, pp=pp: e.tensor_copy(out=pos, in_=pp[:, 0:NE]), reads=[B_pp], writes=R)
                    S.op("dve", lambda e, ti=ti: e.scalar_tensor_tensor(out=posm[:, ti, :], in0=pos, scalar=1.0, in1=msk, op0=ALU.add, op1=ALU.mult),
                         reads=R, writes=[B_posm[ti]])
                    S.op("dve", lambda e, ti=ti: e.tensor_scalar(out=posm[:, ti, :], in0=posm[:, ti, :], scalar1=-1.0, scalar2=None, op0=ALU.add),
                         reads=[B_posm[ti]], writes=[B_posm[ti]])
                    for k in range(TOPK):
                        S.op("dve", lambda e, k=k: e.tensor_tensor(out=prod[:, k, :], in0=oh[:, k, :], in1=pos, op=ALU.mult), reads=R, writes=R)
                    S.op("dve", lambda e: e.tensor_reduce(out=pk, in_=prod, axis=AX.X, op=ALU.add), reads=R, writes=R)
                    S.op("dve", lambda e: e.scalar_tensor_tensor(out=rowf, in0=i8f[:, 0:4], scalar=float(CAP), in1=pk, op0=ALU.mult, op1=ALU.add),
                         reads=R, writes=R)
                    S.op("dve", lambda e, ti=ti: e.tensor_copy(out=rows[:, ti, :], in_=rowf), reads=R, writes=[B_rows[ti]])
                    if debug:
                        S.dma("sp", lambda e, ti=ti: e.dma_start(out=dbg["route"][:, ti * 8:ti * 8 + 4], in_=rowf), reads=R)
                        S.dma("sp", lambda e, ti=ti: e.dma_start(out=dbg["route"][:, ti * 8 + 4:ti * 8 + 8], in_=w4[:, ti, :]), reads=[B_w4[ti]])
                return [(lambda sub=sub: routing_tail(sub)) for sub in range(nsub)]

            try:
                cut(1)
                tail = None
                for b in range(NB):
                    tail = mixer_block(512, HALO + 512 * b, False, 4 * b, tail, fold=(b == 0))
                for t_ in tail:
                    t_()
            except _Stop:
                upto = "A"

        if upto in ("B", "C"):
            arena_off[0] = 0

            def sbB(name, shape, dt):
                return carve(shape, dt)

            B_ringB = make_ring(5, KC * 512)
            xs = sbB("xs", [128, KC, CAP], BF16)
            act = sbB("act", [128, KC, CAP], BF16)
            sel = sbB("sel", [128, NT, CAP], BF16)
            ost = sbB("ost", [128, NST, D], F32)
            bdn = [sbB(f"bdn{i}", [128, D], BF16) for i in range(2)]
            xg = [sbB(f"xg{i}", [128, NST, D], BF16) for i in range(2)]
            idxu = [sbB(f"idxu{i}", [128, 4], U32) for i in range(2)]
            idxf = sbB("idxf", [128, 16], F32)
            tokcol = sbB("tokcol", [128, NT, 2], BF16)
            ident_b = sbB("ident_b", [128, 128], BF16)
            NTB = 8
            tb = [sbB(f"tb{i}", [128, CAP], F32) for i in range(NTB)]
            B_xs = [Buf(f"xs{k}") for k in range(KC)]
            B_act = [Buf(f"act{k}") for k in range(KC)]
            B_sel = [Buf(f"sel{i}") for i in range(NT)]
            B_ost = [Buf(f"ost{i}") for i in range(NST)]
            B_bdn = [Buf("bdn0"), Buf("bdn1")]
            B_xg = [[Buf(f"xg{i}_{st}") for st in range(NST)] for i in range(2)]
            B_idxu = [Buf("idxu0"), Buf("idxu1")]
            B_idxf, B_tok = Buf("idxf"), Buf("tokcol")
            B_tb = [Buf(f"tb{i}") for i in range(NTB)]
            tb_rr = [0]

            def next_tb():
                i = tb_rr[0]
                tb_rr[0] = (i + 1) % NTB
                return tb[i], B_tb[i]

            phase_barrier(B_ringB + B_xs + B_act + B_sel + B_ost + B_bdn + B_xg[0] + B_xg[1] + B_idxu + [B_idxf, B_tok] + B_tb)

            for ti in range(NT):
                S.op("dve", lambda e, ti=ti: e.tensor_copy(out=tokcol[:, ti, 0:1], in_=pidx_f[:, 0:1]), reads=[B_const], writes=[B_tok])
                S.op("dve", lambda e, ti=ti: e.memset(tokcol[:, ti, 1:2], float(ti)), writes=[B_tok])
            S.op("dve", lambda e: e.tensor_copy(out=ident_b, in_=ident[:]), reads=[B_const], writes=[B_tok])

            def prep_sel(ex_):
                for ti in range(NT):
                    S.op("dve", lambda e, ti=ti, ex_=ex_: e.tensor_scalar(out=sel[:, ti, :], in0=iota_f[:, 0:CAP], scalar1=posm[:, ti, ex_:ex_ + 1],
                                                                          scalar2=None, op0=ALU.is_equal),
                         reads=[B_posm[ti], B_const], writes=[B_sel[ti]])

            def prep_idx(ex_):
                bi = ex_ % 2
                for st in range(NST):
                    pi, B_pi = next_ps()
                    mm_group(pi[:, 0:2], B_pi, [(sel[:, ti, st * 128:(st + 1) * 128], tokcol[:, ti, :]) for ti in range(NT)],
                             reads=B_sel + [B_tok])
                    S.op("dve", lambda e, pi=pi, st=st: e.tensor_copy(out=idxf[:, 4 + 2 * st:6 + 2 * st], in_=pi[:, 0:2]), reads=[B_pi], writes=[B_idxf])
                    S.op("dve", lambda e, st=st: e.scalar_tensor_tensor(out=idxf[:, st:st + 1], in0=idxf[:, 5 + 2 * st:6 + 2 * st], scalar=128.0,
                                                                        in1=idxf[:, 4 + 2 * st:5 + 2 * st], op0=ALU.mult, op1=ALU.add),
                         reads=[B_idxf], writes=[B_idxf])
                S.op("dve", lambda e, bi=bi: e.tensor_copy(out=idxu[bi][:, 0:NST], in_=idxf[:, 0:NST]), reads=[B_idxf], writes=[B_idxu[bi]])

            def prep_gather(ex_):
                bi = ex_ % 2
                for st in range(NST):
                    S.dma("pool", lambda e, bi=bi, st=st: e.indirect_dma_start(
                        out=xg[bi][:, st, :], out_offset=None, in_=h1b_d[:, :],
                        in_offset=bass.IndirectOffsetOnAxis(ap=idxu[bi][:, st:st + 1], axis=0)),
                        reads=[B_idxu[bi]], writes=[B_xg[bi][st]])

            prep_sel(0)
            prep_idx(0)
            prep_gather(0)
            for ex_ in range(NE):
                r0 = ex_ * D
                bi = ex_ % 2
                bd, B_bd = bdn[ex_ % 2], B_bdn[ex_ % 2]
                S.dma("pool", lambda e, bd=bd, ex_=ex_: e.dma_start(out=bd[0:1, :], in_=b_dn[ex_:ex_ + 1, :]), writes=[B_bd])
                if ex_ + 1 < NE:
                    prep_sel(ex_ + 1)
                for m in range(KC):
                    ps, B_p = next_ps()
                    psb = ps[:, 0:CAP // 2].bitcast(BF16)
                    items = [(psb[:, st * 128:(st + 1) * 128], xg[bi][:, st, m * 128:(m + 1) * 128], ident_b) for st in range(NST)]
                    tr_group(B_p, items, reads=B_xg[bi] + [B_tok])
                    S.op("act", lambda e, psb=psb, m=m: e.copy(out=xs[:, m, :], in_=psb), reads=[B_p], writes=[B_xs[m]])
                for q in range(4):
                    if q == 1 and ex_ + 1 < NE:
                        prep_idx(ex_ + 1)
                    if q == 2 and ex_ + 1 < NE:
                        prep_gather(ex_ + 1)
                    wg = load_w(w_gu[r0:r0 + D, 512 * q:512 * q + 512], ncols=512)
                    wl = load_w(w_gu[r0:r0 + D, D + 512 * q:D + 512 * q + 512], ncols=512)
                    for kk in range(4):
                        j = 4 * q + kk
                        pg, B_pg = next_ps()
                        mm_group(pg[:, 0:CAP], B_pg, [(wg[0][:, k, kk * 128:(kk + 1) * 128], xs[:, k, :]) for k in range(KC)], reads=[wg[1]] + B_xs)
                        pli, B_pli = next_ps()
                        mm_group(pli[:, 0:CAP], B_pli, [(wl[0][:, k, kk * 128:(kk + 1) * 128], xs[:, k, :]) for k in range(KC)], reads=[wl[1]] + B_xs)
                        gt, B_gt = next_tb()
                        sg, B_sg = next_tb()
                        l1, B_l1 = next_tb()
                        cb = FO["bg"] + ex_ * 16 + j
                        cl = FO["bl"] + ex_ * 16 + j
                        S.op("dve", lambda e, gt=gt, pg=pg, cb=cb: e.tensor_scalar(out=gt[:], in0=pg[:, 0:CAP], scalar1=fv[:, cb:cb + 1], scalar2=LIMIT,
                                                                                  op0=ALU.add, op1=ALU.min), reads=[B_pg, B_fv], writes=[B_gt])
                        S.op("act", lambda e, sg=sg, gt=gt: e.activation(out=sg[:], in_=gt[:], func=AF.Sigmoid, scale=SW_ALPHA), reads=[B_gt], writes=[B_sg])
                        S.op("dve", lambda e, l1=l1, pli=pli, cl=cl: e.tensor_scalar(out=l1[:], in0=pli[:, 0:CAP], scalar1=fv[:, cl:cl + 1], scalar2=LIMIT + 1.0,
                                                                                    op0=ALU.add, op1=ALU.min), reads=[B_pli, B_fv], writes=[B_l1])
                        S.op("dve", lambda e, gt=gt, sg=sg: e.tensor_tensor(out=gt[:], in0=gt[:], in1=sg[:], op=ALU.mult), reads=[B_gt, B_sg], writes=[B_gt])
                        S.op("dve", lambda e, l1=l1, gt=gt, j=j: e.scalar_tensor_tensor(out=act[:, j, :], in0=l1[:], scalar=1.0 - LIMIT, in1=gt[:],
                                                                                        op0=ALU.max, op1=ALU.mult), reads=[B_l1, B_gt], writes=[B_act[j]])
                for nb in range(4):
                    wd = load_w(w_dn[r0:r0 + D, 512 * nb:512 * nb + 512], ncols=512)
                    for st in range(NST):
                        po, B_po = next_ps()
                        pairs = [(act[:, j, st * 128:(st + 1) * 128], wd[0][:, j, :]) for j in range(KC)]
                        pairs.append((ones_b[0:1, :], bd[0:1, 512 * nb:512 * nb + 512]))
                        mm_group(po[:, 0:512], B_po, pairs, reads=[wd[1], B_bd, B_const] + B_act)
                        if (nb + st) % 2:
                            S.op("act", lambda e, po=po, st=st, nb=nb: e.copy(out=ost[:, st, 512 * nb:512 * nb + 512], in_=po[:, 0:512]),
                                 reads=[B_po], writes=[B_ost[st]])
                        else:
                            S.op("dve", lambda e, po=po, st=st, nb=nb: e.tensor_copy(out=ost[:, st, 512 * nb:512 * nb + 512], in_=po[:, 0:512]),
                                 reads=[B_po], writes=[B_ost[st]])
                for st in range(NST):
                    S.dma("sp", lambda e, st=st, ex_=ex_: e.dma_start(out=contrib[ex_ * CAP + st * 128:ex_ * CAP + (st + 1) * 128, :], in_=ost[:, st, :]),
                          reads=[B_ost[st]], writes=[])

        if upto == "C":
            arena_off[0] = 0

            def sbC(name, shape, dt):
                return carve(shape, dt)

            NG = 12
            G = [sbC(f"G{i}", [128, D], F32) for i in range(NG)]
            hh = [sbC(f"hh{i}", [128, D], F32) for i in range(2)]
            lg2 = sbC("lg2", [128, 2 * D], F32)
            stat2 = sbC("stat2", [128, 4, 6], F32)
            mv2 = sbC("mv2", [128, 8], F32)
            B_G = [Buf(f"G{i}") for i in range(NG)]
            B_hh = [Buf("hh0"), Buf("hh1")]
            B_lg2, B_stat2, B_mv2 = Buf("lg2"), Buf("stat2"), Buf("mv2")
            phase_barrier(B_G + B_hh + [B_lg2, B_stat2, B_mv2])
            S.dma("sp", lambda e: e.dma_start(out=lg2[:], in_=rvec[:, 0:2 * D]), writes=[B_lg2])
            final = []
            for ti in range(NT):
                h, B_h = hh[ti % 2], B_hh[ti % 2]
                S.dma("sp", lambda e, h=h, ti=ti: e.dma_start(out=h[:], in_=h1f_d[ti * 128:(ti + 1) * 128, :]), writes=[B_h])
                S.op("act", lambda e, h=h: e.mul(out=h[:], in_=h[:], mul=ALPHA), reads=[B_h], writes=[B_h])
                for k in range(TOPK):
                    gi = (ti * TOPK + k) % NG
                    S.dma("pool", lambda e, gi=gi, ti=ti, k=k: e.indirect_dma_start(
                        out=G[gi][:], out_offset=None, in_=contrib[:, :],
                        in_offset=bass.IndirectOffsetOnAxis(ap=rows[:, ti, k:k + 1], axis=0)),
                        reads=[B_rows[ti]], writes=[B_G[gi]])
                    S.op("dve", lambda e, gi=gi, h=h, ti=ti, k=k: e.scalar_tensor_tensor(out=h[:], in0=G[gi][:], scalar=w4[:, ti, k:k + 1], in1=h[:],
                                                                                         op0=ALU.mult, op1=ALU.add),
                         reads=[B_G[gi], B_h, B_w4[ti]], writes=[B_h])
                for c in range(4):
                    S.op("dve", lambda e, c=c, h=h: e.bn_stats(out=stat2[:, c, :], in_=h[:, c * 512:(c + 1) * 512]), reads=[B_h], writes=[B_stat2])
                S.op("dve", lambda e: e.bn_aggr(out=mv2[:, 0:2], in_=stat2[:, :, :].rearrange("p a b -> p (a b)")), reads=[B_stat2], writes=[B_mv2])
                S.op("dve", lambda e: e.tensor_scalar(out=mv2[:, 2:3], in0=mv2[:, 1:2], scalar1=LN_EPS, scalar2=None, op0=ALU.add),
                     reads=[B_mv2], writes=[B_mv2])
                S.op("act", lambda e: e.activation(out=mv2[:, 2:3], in_=mv2[:, 2:3], func=AF.Sqrt), reads=[B_mv2], writes=[B_mv2])
                S.op("dve", lambda e: e.reciprocal(out=mv2[:, 2:3], in_=mv2[:, 2:3]), reads=[B_mv2], writes=[B_mv2])
                S.op("dve", lambda e: e.tensor_scalar(out=mv2[:, 3:4], in0=mv2[:, 0:1], scalar1=mv2[:, 2:3], scalar2=-1.0, op0=ALU.mult, op1=ALU.mult),
                     reads=[B_mv2], writes=[B_mv2])
                S.op("act", lambda e, h=h: e.activation(out=h[:], in_=h[:], func=AF.Identity, bias=mv2[:, 3:4], scale=mv2[:, 2:3]),
                     reads=[B_mv2, B_h], writes=[B_h])
                S.op("dve", lambda e, h=h: e.tensor_tensor(out=h[:], in0=h[:], in1=lg2[:, 0:D], op=ALU.mult), reads=[B_h, B_lg2], writes=[B_h])
                S.op("dve", lambda e, h=h: e.tensor_tensor(out=h[:], in0=h[:], in1=lg2[:, D:2 * D], op=ALU.add), reads=[B_h, B_lg2], writes=[B_h])
                final.append(S.dma("sp", lambda e, h=h, ti=ti: e.dma_start(out=out[ti * 128:(ti + 1) * 128, :], in_=h[:]), reads=[B_h]))

        all_dma = [(S.dsem[q][i], S.dcnt[q][i]) for q in ("sp", "pool") for i in range(S.nds) if S.dcnt[q][i] > 0]
        with nc.Block() as block:
            @block.tensor
            def _(e):
                S.emit("pe", e)

            @block.scalar
            def _(e):
                S.emit("act", e)

            @block.vector
            def _(e):
                S.emit("dve", e)

            @block.gpsimd
            def _(e):
                S.emit("pool", e)

            @block.sync
            def _(e):
                S.emit("sp", e, final_waits=all_dma)
    return nc


def _fm(v):
    v = np.asarray(v, np.float32)
    return np.ascontiguousarray(v.reshape(-1, 128).T)


def make_core_inputs(c, T, NE, x, meta_tokens, ln_in_g, ln_in_b, w_in, conv_w, w_a_out, pool_w, pool_scale, w_o,
                     ln1_g, ln1_b, router_w, router_b, w_gate_up, b_gate_up, w_down, b_down, ln2_g, ln2_b, shared):
    halves = x.shape[1] // T
    b, h = divmod(c, halves)
    if h == 0:
        xin = np.concatenate([meta_tokens.astype(np.float32), x[b, 0:T]], axis=0)
    else:
        xin = x[b, h * T - HALO:(h + 1) * T]
    d = dict(shared)
    d["xin"] = np.ascontiguousarray(xin, dtype=np.float32)
    return d


def make_shared(NE, ln_in_g, ln_in_b, w_in, conv_w, w_a_out, pool_w, pool_scale, w_o, ln1_g, ln1_b, router_w, router_b,
                w_gate_up, b_gate_up, w_down, b_down, ln2_g, ln2_b):
    cols = [_fm(ln_in_g), _fm(ln_in_b), _fm(conv_w[0, 0]), _fm(conv_w[0, 1]), _fm(conv_w[0, 2]), _fm(pool_scale[0]),
            _fm(ln1_g[0]), _fm(ln1_b[0])]
    bgu = np.asarray(b_gate_up[0], np.float32)
    bg = np.concatenate([_fm(bgu[e, :D]) for e in range(NE)], axis=1)
    bl = np.concatenate([_fm(bgu[e, D:]) for e in range(NE)], axis=1)
    fvec = np.ascontiguousarray(np.concatenate(cols + [bg, bl], axis=1), dtype=np.float32)
    rrow = np.concatenate([np.asarray(ln2_g[0], np.float32), np.asarray(ln2_b[0], np.float32), np.asarray(router_b[0], np.float32)])
    rvec = np.ascontiguousarray(np.broadcast_to(rrow[None, :], (128, rrow.shape[0])), dtype=np.float32)
    return {
        "w_in": np.ascontiguousarray(w_in[0], dtype=np.float32),
        "w_a": np.ascontiguousarray(w_a_out[0], dtype=np.float32),
        "w_o": np.ascontiguousarray(w_o[0], dtype=np.float32),
        "pool_w": np.ascontiguousarray(np.asarray(pool_w[0], np.float32).reshape(1024, 512)),
        "router_w": np.ascontiguousarray(router_w[0], dtype=np.float32),
        "w_gu": np.ascontiguousarray(np.asarray(w_gate_up[0], np.float32).reshape(NE * D, 2 * D)),
        "w_dn": np.ascontiguousarray(np.asarray(w_down[0], np.float32).reshape(NE * D, D)),
        "b_dn": np.ascontiguousarray(b_down[0], dtype=np.float32),
        "fvec": fvec,
        "rvec": rvec,
    }


def kernel(**inputs):
    inputs = {k: np.asarray(v) for k, v in inputs.items()}
    x = inputs["x"]
    B, SEQ, _ = x.shape
    NE = inputs["router_w"].shape[-1]
    n_cores = 8
    T = B * SEQ // n_cores
    CAP = 384
    names = ["ln_in_g", "ln_in_b", "w_in", "conv_w", "w_a_out", "pool_w", "pool_scale", "w_o", "ln1_g", "ln1_b", "router_w",
             "router_b", "w_gate_up", "b_gate_up", "w_down", "b_down", "ln2_g", "ln2_b"]
    shared = make_shared(NE, *[inputs[n] for n in names])
    in_maps = [make_core_inputs(c, T, NE, x, inputs["meta_tokens"], *[inputs[n] for n in names], shared) for c in range(n_cores)]
    nc = build_nc(T, NE, CAP)
    res = run_bass_kernel_spmd(nc, in_maps, core_ids=list(range(n_cores)))
    halves = SEQ // T
    outp = np.empty((B, SEQ, D), np.float32)
    for c in range(n_cores):
        b, h = divmod(c, halves)
        outp[b, h * T:(h + 1) * T] = res.results[c]["out"]
    return outp
```

```python
import numpy as np
from contextlib import ExitStack
import concourse.bass as bass
import concourse.mybir as mybir
from concourse.bass_utils import run_bass_kernel_spmd

F32 = mybir.dt.float32
BF16 = mybir.dt.bfloat16
U32 = mybir.dt.uint32
I32 = mybir.dt.int32
AF = mybir.ActivationFunctionType
ALU = mybir.AluOpType
AX = mybir.AxisListType

D = 2048
KC = 16
DIN = 11264
NMETA = 16
HALO = 16
TOPK = 4
LN_EPS = 1e-5
ALPHA = 2.0 ** 0.25
LIMIT = 7.0
SW_ALPHA = 1.702
WCOLS = 256
NWR = 5


class Buf:
    __slots__ = ("name", "w", "r")

    def __init__(self, name):
        self.name = name
        self.w = None
        self.r = {}


class Sched:
    def __init__(self, nc, es):
        self.nc = nc
        self.es = es
        self.ops = {k: [] for k in ("pe", "act", "dve", "pool", "sp")}
        self.sem = {k: es.enter_context(nc.semaphore("s_" + k)) for k in ("pe", "act", "dve", "poolc")}
        self.cnt = {k: 0 for k in self.sem}
        self.nds = 8
        self.dsem = {q: [es.enter_context(nc.semaphore(f"d_{q}{i}")) for i in range(self.nds)] for q in ("sp", "pool")}
        self.dcnt = {q: [0] * self.nds for q in ("sp", "pool")}
        self.dnext = {q: 0 for q in ("sp", "pool")}

    def _deps(self, reads, writes):
        deps = []
        for b in reads:
            if b.w is not None:
                deps.append(b.w)
        for b in writes:
            if b.w is not None:
                deps.append(b.w)
            deps.extend(b.r.values())
        return deps

    def _commit(self, tok, reads, writes):
        for b in writes:
            b.w = tok
            b.r = {}
        for b in reads:
            if b in writes:
                continue
            s, v = tok
            if b.r.get(id(s), (None, 0))[1] < v:
                b.r[id(s)] = (s, v)

    def op(self, eng, fn, reads=(), writes=()):
        skey = "poolc" if eng == "pool" else eng
        deps = self._deps(reads, writes)
        self.cnt[skey] += 1
        tok = (self.sem[skey], self.cnt[skey])
        self.ops[eng].append((fn, deps, tok, 1))
        self._commit(tok, reads, writes)
        return tok

    def dma(self, q, fn, reads=(), writes=()):
        deps = self._deps(reads, writes)
        i = self.dnext[q]
        self.dnext[q] = (i + 1) % self.nds
        sem = self.dsem[q][i]
        if self.dcnt[q][i] > 0:
            deps.append((sem, self.dcnt[q][i]))
        self.dcnt[q][i] += 16
        tok = (sem, self.dcnt[q][i])
        self.ops[q].append((fn, deps, tok, 16))
        self._commit(tok, reads, writes)
        return tok

    def emit(self, eng, e, final_waits=()):
        waited = {}
        for fn, deps, tok, inc in self.ops[eng]:
            for s, v in deps:
                k = id(s)
                if waited.get(k, 0) < v:
                    e.wait_ge(s, v)
                    waited[k] = v
            ins = fn(e)
            ins.then_inc(tok[0], inc)
        for s, v in final_waits:
            e.wait_ge(s, v)


class _Stop(Exception):
    pass


def build_nc(T, NE, CAP, debug=False, upto="C", cutv=0):
    assert T % 512 == 0 and CAP % 128 == 0 and CAP <= 512
    NT = T // 128
    NST = CAP // 128
    NB = T // 512
    nc = bass.Bass("TRN2", target_bir_lowering=False)

    def din(name, shape, dt=F32):
        return nc.dram_tensor(name, shape, dt, kind="ExternalInput").ap()

    xin = din("xin", [T + HALO, D])
    w_in = din("w_in", [D, DIN])
    w_a = din("w_a", [D, D])
    w_o = din("w_o", [D, D])
    pool_w = din("pool_w", [1024, 512])
    router_w = din("router_w", [D, NE])
    w_gu = din("w_gu", [NE * D, 2 * D])
    w_dn = din("w_dn", [NE * D, D])
    b_dn = din("b_dn", [NE, D])
    NF = 8 * 16 + 2 * NE * 16
    fvec = din("fvec", [128, NF])
    NR = 2 * D + NE
    rvec = din("rvec", [128, NR])
    out = nc.dram_tensor("out", [T, D], F32, kind="ExternalOutput").ap()
    import os as _os
    SCR = _os.environ.get("SCRKIND", "Internal")
    h1f_d = nc.dram_tensor("h1f_d", [T, D], F32, kind=SCR).ap()
    h1b_d = nc.dram_tensor("h1b_d", [T, D], BF16, kind=SCR).ap()
    contrib = nc.dram_tensor("contrib", [NE * CAP, D], F32, kind=SCR).ap()
    dbg = {}
    if debug:
        dbg["h1"] = nc.dram_tensor("dbg_h1", [T, D], F32, kind="ExternalOutput").ap()
        dbg["route"] = nc.dram_tensor("dbg_route", [128, NT * 8], F32, kind="ExternalOutput").ap()
        dbg["za"] = nc.dram_tensor("dbg_za", [128, KC * 512], BF16, kind="ExternalOutput").ap()
        dbg["pl"] = nc.dram_tensor("dbg_pl", [128, 8 * 512], BF16, kind="ExternalOutput").ap()
        dbg["mg"] = nc.dram_tensor("dbg_mg", [128, KC * 512], BF16, kind="ExternalOutput").ap()
        dbg["h0b"] = nc.dram_tensor("dbg_h0b", [128, KC * 512], BF16, kind="ExternalOutput").ap()

    FO = dict(lng=0, lnb=16, cw0=32, cw1=48, cw2=64, psc=80, l1g=96, l1b=112, bg=128, bl=128 + NE * 16)

    with ExitStack() as es:
        S = Sched(nc, es)

        def sb(name, shape, dt):
            return es.enter_context(nc.sbuf_tensor(name, shape, dt))

        fv = sb("fv", [128, NF], F32)
        ident = sb("ident", [128, 128], F32)
        ones_f = sb("ones_f", [128, 128], F32)
        ones_b = sb("ones_b", [128, 128], BF16)
        triu_b = sb("triu_b", [128, 128], BF16)
        iota_i = sb("iota_i", [128, 512], I32)
        iota_f = sb("iota_f", [128, 512], F32)
        pidx_i = sb("pidx_i", [128, 1], I32)
        pidx_f = sb("pidx_f", [128, 1], F32)
        rw = sb("rw", [128, KC, NE], F32)
        rb = sb("rb", [128, NE], F32)
        maskb = sb("maskb", [128, NT, NE], BF16)
        posm = sb("posm", [128, NT, NE], F32)
        w4 = sb("w4", [128, NT, 4], F32)
        rows = sb("rows", [128, NT, 4], U32)
        B_fv, B_const, B_rw = Buf("fv"), Buf("const"), Buf("rw")
        B_maskb = [Buf(f"maskb{i}") for i in range(NT)]
        B_posm = [Buf(f"posm{i}") for i in range(NT)]
        B_w4 = [Buf(f"w4{i}") for i in range(NT)]
        B_rows = [Buf(f"rows{i}") for i in range(NT)]

        S.dma("sp", lambda e: e.dma_start(out=fv[:], in_=fvec[:, :]), writes=[B_fv])
        S.dma("sp", lambda e: e.dma_start(out=rw[:], in_=router_w.rearrange("(k p) n -> p k n", p=128)), writes=[B_rw])
        S.dma("sp", lambda e: e.dma_start(out=rb[:], in_=rvec[:, 2 * D:2 * D + NE]), writes=[B_rw])
        S.op("pool", lambda e: e.iota(iota_i[:], pattern=[[1, 512]], base=0, channel_multiplier=0), writes=[B_const])
        S.op("pool", lambda e: e.iota(pidx_i[:], pattern=[[1, 1]], base=0, channel_multiplier=1), writes=[B_const])
        S.op("dve", lambda e: e.tensor_copy(out=iota_f[:], in_=iota_i[:]), reads=[B_const], writes=[B_const])
        S.op("dve", lambda e: e.tensor_copy(out=pidx_f[:], in_=pidx_i[:]), reads=[B_const], writes=[B_const])
        S.op("dve", lambda e: e.tensor_scalar(out=ident[:], in0=iota_f[:, 0:128], scalar1=pidx_f[:, 0:1], scalar2=None,
                                              op0=ALU.is_equal), reads=[B_const], writes=[B_const])
        S.op("dve", lambda e: e.tensor_scalar(out=triu_b[:], in0=iota_f[:, 0:128], scalar1=pidx_f[:, 0:1], scalar2=None,
                                              op0=ALU.is_gt), reads=[B_const], writes=[B_const])
        S.op("dve", lambda e: e.memset(ones_f[:], 1.0), writes=[B_const])
        S.op("dve", lambda e: e.memset(ones_b[:], 1.0), writes=[B_const])
        S.op("dve", lambda e: e.tensor_scalar(out=fv[:, FO["bl"]:FO["bl"] + NE * 16], in0=fv[:, FO["bl"]:FO["bl"] + NE * 16],
                                              scalar1=1.0, scalar2=None, op0=ALU.add), reads=[B_fv], writes=[B_fv])

        def cut(k):
            if cutv == k:
                raise _Stop()

        def fcol(key, k):
            c = FO[key] + k
            return fv[:, c:c + 1]

        NARENA = 47616
        arena_t = sb("arena", [128, NARENA], F32)
        arena_off = [0]

        def carve(shape, dt):
            n = 1
            for d_ in shape[1:]:
                n *= d_
            nbytes = n * (2 if dt == BF16 else 4)
            nf = (nbytes + 7) // 8 * 2
            off = arena_off[0]
            assert off + nf <= NARENA, ("arena overflow", off, nf)
            arena_off[0] = off + nf
            v = arena_t[:, off:off + nf]
            if dt != F32:
                v = v.bitcast(dt)
            v = v[:, 0:n]
            if len(shape) == 3:
                v = v.rearrange("p (a b) -> p a b", a=shape[1])
            return v

        def phase_barrier(bufs):
            toks = [(S.sem[k], S.cnt[k]) for k in ("pe", "act", "dve") if S.cnt[k] > 0]
            toks += [(S.dsem[q][i], S.dcnt[q][i]) for q in ("sp", "pool") for i in range(S.nds) if S.dcnt[q][i] > 0]
            for bb in bufs:
                for tk in toks:
                    bb.r[id(tk[0])] = tk

        psum = [es.enter_context(nc.psum_tensor(f"ps{i}", [128, 512], F32)) for i in range(8)]
        B_ps = [Buf(f"ps{i}") for i in range(8)]
        ps_rr = [0]
        ps_reserved = set()

        def next_ps():
            while ps_rr[0] in ps_reserved:
                ps_rr[0] = (ps_rr[0] + 1) % 8
            i = ps_rr[0]
            ps_rr[0] = (i + 1) % 8
            return psum[i], B_ps[i]

        ring = {"bufs": [], "B": [], "rr": 0}

        def make_ring(n, elems):
            ring["bufs"] = [carve([128, elems], BF16) for _ in range(n)]
            ring["B"] = [Buf(f"wr{i}") for i in range(n)]
            ring["rr"] = 0
            return ring["B"]

        def load_w(src_ap, kk=KC, ncols=WCOLS):
            i = ring["rr"]
            ring["rr"] = (i + 1) % len(ring["bufs"])
            view = ring["bufs"][i][:, 0:kk * ncols].rearrange("p (k n) -> p k n", k=kk)
            Bw = ring["B"][i]
            S.dma("pool", lambda e: e.dma_start(out=view, in_=src_ap.rearrange("(k p) n -> p k n", p=128)), writes=[Bw])
            return view, Bw

        def mm_group(ps_ap, B_out, pairs, reads):
            def fn(e):
                ins = None
                n = len(pairs)
                for i, (l, r) in enumerate(pairs):
                    ins = e.matmul(ps_ap, lhsT=l, rhs=r, start=(i == 0), stop=(i == n - 1))
                return ins
            return S.op("pe", fn, reads=reads, writes=[B_out])

        def tr_group(B_out, items, reads):
            def fn(e):
                ins = None
                for o, i_, idn in items:
                    ins = e.transpose(o, i_, idn)
                return ins
            return S.op("pe", fn, reads=reads, writes=[B_out])

        if True:
            def sbA(name, shape, dt):
                return carve(shape, dt)

            make_ring(NWR, KC * WCOLS)
            h0f = sbA("h0f", [128, KC, 512], F32)
            h0b = sbA("h0b", [128, KC, 512], BF16)
            za = sbA("za", [128, KC, 512], BF16)
            pl = sbA("pl", [128, 8, 512], BF16)
            mg = sbA("mg", [128, KC, 512], BF16)
            xst = [sbA(f"xst{i}", [128, D], F32) for i in range(2)]
            hbst = [sbA(f"hbst{i}", [128, D], BF16) for i in range(2)]
            cuh = sbA("cuh", [128, KC, 16], F32)
            vh = sbA("vh", [128, 8, 16], F32)
            NTMP = 8
            tmp = [sbA(f"tmp{i}", [128, 528], F32) for i in range(NTMP)]
            stat = sbA("stat", [128, 4, 6], F32)
            mv = sbA("mv", [128, 8], F32)
            rt = sbA("rt", [128, 512], F32)
            rti = sbA("rti", [128, 8], U32)
            lgall = sbA("lgall", [128, 4, NE], F32)
            B_lg = [Buf(f"lg{i}") for i in range(4)]
            B_h0f = [Buf(f"h0f{k}") for k in range(KC)]
            B_h0b = [Buf(f"h0b{k}") for k in range(KC)]
            B_za = [Buf(f"za{k}") for k in range(KC)]
            B_pl = [Buf(f"pl{k}") for k in range(8)]
            B_mg = [Buf(f"mg{k}") for k in range(KC)]
            B_xst = [Buf("xst0"), Buf("xst1")]
            B_hbst = [Buf("hbst0"), Buf("hbst1")]
            B_cuh = [Buf(f"cuh{k}") for k in range(KC)]
            B_vh = [Buf(f"vh{k}") for k in range(8)]
            B_tmp = [Buf(f"tmp{i}") for i in range(NTMP)]
            B_stat, B_mv, B_rt = Buf("stat"), Buf("mv"), Buf("rt")
            wpl_t = sbA("wpl", [128, 8, 512], BF16)
            B_wpl = Buf("wpl")
            S.dma("pool", lambda e: e.dma_start(out=wpl_t, in_=pool_w.rearrange("(k p) n -> p k n", p=128)), writes=[B_wpl])
            tmp_rr = [0]

            def next_tmp():
                i = tmp_rr[0]
                tmp_rr[0] = (i + 1) % NTMP
                return tmp[i], B_tmp[i]

            ev_rr = [0]

            def evac_copy(out_ap, in_ap, reads, writes):
                ev_rr[0] ^= 1
                if ev_rr[0]:
                    return S.op("act", lambda e: e.copy(out=out_ap, in_=in_ap), reads=reads, writes=writes)
                return S.op("dve", lambda e: e.tensor_copy(out=out_ap, in_=in_ap), reads=reads, writes=writes)

            def ln_rows(x_ap, npart, B_x):
                for c in range(4):
                    S.op("dve", lambda e, c=c: e.bn_stats(out=stat[0:npart, c, :], in_=x_ap[:, c * 512:(c + 1) * 512]),
                         reads=[B_x], writes=[B_stat] if c == 0 else [B_stat])
                S.op("dve", lambda e: e.bn_aggr(out=mv[0:npart, 0:2], in_=stat[0:npart, :, :].rearrange("p a b -> p (a b)")),
                     reads=[B_stat], writes=[B_mv])
                S.op("dve", lambda e: e.tensor_scalar(out=mv[0:npart, 2:3], in0=mv[0:npart, 1:2], scalar1=LN_EPS, scalar2=None,
                                                      op0=ALU.add), reads=[B_mv], writes=[B_mv])
                S.op("act", lambda e: e.activation(out=mv[0:npart, 2:3], in_=mv[0:npart, 2:3], func=AF.Sqrt), reads=[B_mv], writes=[B_mv])
                S.op("dve", lambda e: e.reciprocal(out=mv[0:npart, 2:3], in_=mv[0:npart, 2:3]), reads=[B_mv], writes=[B_mv])
                S.op("dve", lambda e: e.tensor_scalar(out=mv[0:npart, 3:4], in0=mv[0:npart, 0:1], scalar1=mv[0:npart, 2:3], scalar2=-1.0,
                                                      op0=ALU.mult, op1=ALU.mult), reads=[B_mv], writes=[B_mv])
                S.op("act", lambda e: e.activation(out=x_ap, in_=x_ap, func=AF.Identity, bias=mv[0:npart, 3:4],
                                                   scale=mv[0:npart, 2:3]), reads=[B_mv, B_x], writes=[B_x])

            hhf = sbA("hhf", [128, KC, 16], F32)
            hhb = sbA("hhb", [128, KC, 16], BF16)
            B_hhf, B_hhb = Buf("hhf"), Buf("hhb")

            def load_ln_T(n, r0, dst_f, dst_b, Bf, Bb, keep_f32):
                nsub = max(1, n // 128)
                npart = min(n, 128)
                for sub in range(nsub):
                    xt, B_x = xst[sub % 2], B_xst[sub % 2]
                    S.dma("sp", lambda e, xt=xt, sub=sub: e.dma_start(out=xt[0:npart, :], in_=xin[r0 + sub * 128:r0 + sub * 128 + npart, :]),
                          writes=[B_x])
                    ln_rows(xt[0:npart, :], npart, B_x)
                    for q in range(4):
                        ps, B_p = next_ps()
                        items = [(ps[:, kk * npart:(kk + 1) * npart], xt[0:npart, (4 * q + kk) * 128:(4 * q + kk + 1) * 128],
                                  ident[0:npart, 0:npart]) for kk in range(4)]
                        tr_group(B_p, items, reads=[B_x, B_const])
                        evac_copy(dst_f[:, 4 * q:4 * q + 4, sub * 128:sub * 128 + npart],
                                  ps[:, 0:4 * npart].rearrange("p (k t) -> p k t", k=4),
                                  reads=[B_p], writes=[Bf[4 * q + kk] for kk in range(4)])
                for k in range(KC):
                    S.op("act", lambda e, k=k: e.activation(out=dst_b[:, k, 0:n], in_=dst_f[:, k, 0:n], func=AF.Identity,
                                                            bias=fcol("lnb", k), scale=fcol("lng", k)),
                         reads=[Bf[k], B_fv], writes=[Bb[k]])
                    if keep_f32:
                        S.op("dve", lambda e, k=k: e.tensor_scalar(out=dst_f[:, k, 0:n], in0=dst_f[:, k, 0:n], scalar1=fcol("lng", k),
                                                                   scalar2=fcol("lnb", k), op0=ALU.mult, op1=ALU.add),
                             reads=[Bf[k], B_fv], writes=[Bf[k]])

            def mixer_block(n, r0, halo, tile0, prev_tail, fold=False):
                nsub = max(1, n // 128)
                npart = min(n, 128)
                if fold:
                    load_ln_T(HALO, 0, hhf, hhb, [B_hhf] * KC, [B_hhb] * KC, False)
                load_ln_T(n, r0, h0f, h0b, B_h0f, B_h0b, not halo)

                cut(2 if halo else 4)

                def proj(wv, B_w, kk, reads_extra=()):
                    ps, B_p = next_ps()
                    pairs = [(wv[:, k, kk * 128:(kk + 1) * 128], h0b[:, k, 0:n]) for k in range(KC)]
                    mm_group(ps[:, 0:n], B_p, pairs, reads=[B_w] + B_h0b)
                    return ps, B_p

                def proj_h(wv, B_w, kk):
                    ps, B_p = next_ps()
                    pairs = [(wv[:, k, kk * 128:(kk + 1) * 128], hhb[:, k, :]) for k in range(KC)]
                    mm_group(ps[:, 0:HALO], B_p, pairs, reads=[B_w, B_hhb])
                    return ps, B_p

                for s in range(8):
                    if prev_tail is not None and 1 <= s <= len(prev_tail):
                        prev_tail[s - 1]()
                    if halo:
                        wb = None
                    else:
                        wb = load_w(w_in[:, 256 * s:256 * s + 256])
                    wc = load_w(w_in[:, 2048 + 256 * s:2048 + 256 * s + 256])
                    wu = load_w(w_in[:, 4096 + 256 * s:4096 + 256 * s + 256])
                    for kk in range(2):
                        i = 2 * s + kk
                        pc, B_pc = proj(wc[0], wc[1], kk)
                        pu, B_pu = proj(wu[0], wu[1], kk)
                        if halo:
                            ut, B_ut = next_tmp()
                            S.op("act", lambda e, ut=ut, pu=pu: e.copy(out=ut[:, 0:n], in_=pu[:, 0:n]), reads=[B_pu], writes=[B_ut])
                            S.op("dve", lambda e, ut=ut, pc=pc, i=i: e.tensor_tensor(out=cuh[:, i, :], in0=pc[:, 0:n], in1=ut[:, 0:n], op=ALU.mult),
                                 reads=[B_pc, B_ut], writes=[B_cuh[i]])
                            continue
                        if fold:
                            pcH, B_pcH = proj_h(wc[0], wc[1], kk)
                            puH, B_puH = proj_h(wu[0], wu[1], kk)
                            utH, B_utH = next_tmp()
                            S.op("act", lambda e, utH=utH, puH=puH: e.copy(out=utH[:, 0:HALO], in_=puH[:, 0:HALO]), reads=[B_puH], writes=[B_utH])
                            S.op("dve", lambda e, utH=utH, pcH=pcH, i=i: e.tensor_tensor(out=cuh[:, i, :], in0=pcH[:, 0:HALO], in1=utH[:, 0:HALO], op=ALU.mult),
                                 reads=[B_pcH, B_utH], writes=[B_cuh[i]])
                        pb, B_pb = proj(wb[0], wb[1], kk)
                        ut, B_ut = next_tmp()
                        cu, B_cu = next_tmp()
                        t1, B_t1 = next_tmp()
                        S.op("act", lambda e, ut=ut, pu=pu: e.copy(out=ut[:, 0:n], in_=pu[:, 0:n]), reads=[B_pu], writes=[B_ut])
                        S.op("act", lambda e, cu=cu, i=i: e.copy(out=cu[:, 0:16], in_=cuh[:, i, :]), reads=[B_cuh[i]], writes=[B_cu])
                        S.op("dve", lambda e, cu=cu, pc=pc, ut=ut: e.tensor_tensor(out=cu[:, 16:16 + n], in0=pc[:, 0:n], in1=ut[:, 0:n], op=ALU.mult),
                             reads=[B_pc, B_ut], writes=[B_cu])
                        S.op("act", lambda e, cu=cu, i=i: e.copy(out=cuh[:, i, :], in_=cu[:, n:n + 16]), reads=[B_cu], writes=[B_cuh[i]])
                        S.op("dve", lambda e, t1=t1, cu=cu, i=i: e.tensor_scalar(out=t1[:, 0:n], in0=cu[:, 16:16 + n], scalar1=fcol("cw2", i),
                                                                                 scalar2=None, op0=ALU.mult), reads=[B_cu, B_fv], writes=[B_t1])
                        S.op("dve", lambda e, t1=t1, cu=cu, i=i: e.scalar_tensor_tensor(out=t1[:, 0:n], in0=cu[:, 15:15 + n], scalar=fcol("cw1", i),
                                                                                        in1=t1[:, 0:n], op0=ALU.mult, op1=ALU.add),
                             reads=[B_cu, B_fv, B_t1], writes=[B_t1])
                        S.op("dve", lambda e, t1=t1, cu=cu, i=i: e.scalar_tensor_tensor(out=t1[:, 0:n], in0=cu[:, 14:14 + n], scalar=fcol("cw0", i),
                                                                                        in1=t1[:, 0:n], op0=ALU.mult, op1=ALU.add),
                             reads=[B_cu, B_fv, B_t1], writes=[B_t1])
                        S.op("dve", lambda e, t1=t1, pb=pb, i=i: e.tensor_tensor(out=za[:, i, 0:n], in0=pb[:, 0:n], in1=t1[:, 0:n], op=ALU.mult),
                             reads=[B_pb, B_t1], writes=[B_za[i]])

                if not halo:
                    cut(5)
                for g in range(4):
                    wv_ = load_w(w_in[:, 6144 + 256 * g:6144 + 256 * g + 256])
                    W = 2 << g
                    for kk in range(2):
                        c = 2 * g + kk
                        pv, B_pv = proj(wv_[0], wv_[1], kk)
                        if halo:
                            S.op("act", lambda e, pv=pv, c=c: e.copy(out=vh[:, c, :], in_=pv[:, 0:n]), reads=[B_pv], writes=[B_vh[c]])
                            continue
                        if fold:
                            pvH, B_pvH = proj_h(wv_[0], wv_[1], kk)
                            S.op("act", lambda e, pvH=pvH, c=c: e.copy(out=vh[:, c, :], in_=pvH[:, 0:HALO]), reads=[B_pvH], writes=[B_vh[c]])
                        vs, B_vs = next_tmp()
                        S.op("act", lambda e, vs=vs, c=c: e.copy(out=vs[:, 0:16], in_=vh[:, c, :]), reads=[B_vh[c]], writes=[B_vs])
                        S.op("act", lambda e, vs=vs, pv=pv: e.copy(out=vs[:, 16:16 + n], in_=pv[:, 0:n]), reads=[B_pv], writes=[B_vs])
                        S.op("act", lambda e, vs=vs, c=c: e.copy(out=vh[:, c, :], in_=vs[:, n:n + 16]), reads=[B_vs], writes=[B_vh[c]])
                        cur, B_cur = vs, B_vs
                        sh = 1
                        while sh < W:
                            nx, B_nx = next_tmp()
                            S.op("dve", lambda e, nx=nx, cur=cur, sh=sh: e.tensor_tensor(out=nx[:, sh:16 + n], in0=cur[:, sh:16 + n],
                                                                                          in1=cur[:, 0:16 + n - sh], op=ALU.add),
                                 reads=[B_cur], writes=[B_nx])
                            cur, B_cur = nx, B_nx
                            sh *= 2
                        S.op("dve", lambda e, cur=cur, vs=vs, c=c, W=W: e.scalar_tensor_tensor(out=pl[:, c, 0:n], in0=cur[:, 16:16 + n], scalar=1.0 / W,
                                                                                                in1=vs[:, 16:16 + n], op0=ALU.mult, op1=ALU.subtract),
                             reads=[B_cur, B_vs], writes=[B_pl[c]])
                if halo:
                    return
                cut(6)

                wpl = (wpl_t, B_wpl)
                for jp in range(8):
                    wa = load_w(w_a[:, 256 * jp:256 * jp + 256])
                    wga = load_w(w_in[:, 7168 + 256 * jp:7168 + 256 * jp + 256])
                    wgb = load_w(w_in[:, 9216 + 256 * jp:9216 + 256 * jp + 256])
                    for kk in range(2):
                        j = 2 * jp + kk
                        g = j // 4
                        pya, B_pya = next_ps()
                        mm_group(pya[:, 0:n], B_pya, [(wa[0][:, i, kk * 128:(kk + 1) * 128], za[:, i, 0:n]) for i in range(KC)],
                                 reads=[wa[1]] + B_za)
                        pyb, B_pyb = next_ps()
                        mm_group(pyb[:, 0:n], B_pyb, [(wpl[0][:, 2 * g + c2, (j % 4) * 128:(j % 4 + 1) * 128], pl[:, 2 * g + c2, 0:n]) for c2 in range(2)],
                                 reads=[wpl[1], B_pl[2 * g], B_pl[2 * g + 1]])
                        pga, B_pga = proj(wga[0], wga[1], kk)
                        pgb, B_pgb = proj(wgb[0], wgb[1], kk)
                        sga, B_sga = next_tmp()
                        sgb, B_sgb = next_tmp()
                        S.op("act", lambda e, sga=sga, pga=pga: e.activation(out=sga[:, 0:n], in_=pga[:, 0:n], func=AF.Sigmoid), reads=[B_pga], writes=[B_sga])
                        S.op("act", lambda e, sgb=sgb, pgb=pgb: e.activation(out=sgb[:, 0:n], in_=pgb[:, 0:n], func=AF.Sigmoid), reads=[B_pgb], writes=[B_sgb])
                        S.op("dve", lambda e, sga=sga, pya=pya: e.tensor_tensor(out=sga[:, 0:n], in0=pya[:, 0:n], in1=sga[:, 0:n], op=ALU.mult),
                             reads=[B_pya, B_sga], writes=[B_sga])
                        S.op("dve", lambda e, sgb=sgb, pyb=pyb, j=j: e.scalar_tensor_tensor(out=sgb[:, 0:n], in0=pyb[:, 0:n], scalar=fcol("psc", j),
                                                                                            in1=sgb[:, 0:n], op0=ALU.mult, op1=ALU.mult),
                             reads=[B_pyb, B_sgb, B_fv], writes=[B_sgb])
                        S.op("dve", lambda e, sga=sga, sgb=sgb, j=j: e.tensor_tensor(out=mg[:, j, 0:n], in0=sga[:, 0:n], in1=sgb[:, 0:n], op=ALU.add),
                             reads=[B_sga, B_sgb], writes=[B_mg[j]])

                if debug and tile0 == 0:
                    S.dma("sp", lambda e: e.dma_start(out=dbg["za"].rearrange("p (k n) -> p k n", k=KC), in_=za), reads=B_za)
                    S.dma("sp", lambda e: e.dma_start(out=dbg["pl"].rearrange("p (k n) -> p k n", k=8), in_=pl), reads=B_pl)
                    S.dma("sp", lambda e: e.dma_start(out=dbg["mg"].rearrange("p (k n) -> p k n", k=KC), in_=mg), reads=B_mg)
                    S.dma("sp", lambda e: e.dma_start(out=dbg["h0b"].rearrange("p (k n) -> p k n", k=KC), in_=h0b), reads=B_h0b)
                cut(7)
                psum_s, B_pss = next_ps()
                psum_q, B_psq = next_ps()
                res_banks = [i for i in range(8) if psum[i] is psum_s or psum[i] is psum_q]
                ps_reserved.update(res_banks)

                def ln1_stats(j, sq, B_sq):
                    def fn_s(e):
                        return e.matmul(psum_s[:, 0:n], lhsT=ones_f[:], rhs=h0f[:, j, 0:n], start=(j == 0), stop=(j == KC - 1))

                    def fn_q(e):
                        return e.matmul(psum_q[:, 0:n], lhsT=ones_f[:], rhs=sq[:, 0:n], start=(j == 0), stop=(j == KC - 1))
                    S.op("pe", fn_s, reads=[B_h0f[j], B_const] + ([B_pss] if j else []), writes=[B_pss])
                    S.op("pe", fn_q, reads=[B_sq] + ([B_psq] if j else []), writes=[B_psq])

                pend = None
                for jp in range(8):
                    wo = load_w(w_o[:, 256 * jp:256 * jp + 256])
                    for kk in range(2):
                        j = 2 * jp + kk
                        pm, B_pm = next_ps()
                        mm_group(pm[:, 0:n], B_pm, [(wo[0][:, i, kk * 128:(kk + 1) * 128], mg[:, i, 0:n]) for i in range(KC)],
                                 reads=[wo[1]] + B_mg)
                        S.op("dve", lambda e, pm=pm, j=j: e.scalar_tensor_tensor(out=h0f[:, j, 0:n], in0=h0f[:, j, 0:n], scalar=ALPHA, in1=pm[:, 0:n],
                                                                                 op0=ALU.mult, op1=ALU.add), reads=[B_pm, B_h0f[j]], writes=[B_h0f[j]])
                        sq, B_sq = next_tmp()
                        S.op("act", lambda e, sq=sq, j=j: e.activation(out=sq[:, 0:n], in_=h0f[:, j, 0:n], func=AF.Square), reads=[B_h0f[j]], writes=[B_sq])
                        if pend is not None:
                            ln1_stats(*pend)
                        pend = (j, sq, B_sq)
                ln1_stats(*pend)
                for i_ in res_banks:
                    ps_reserved.discard(i_)
                cut(8)
                mean, B_mean = next_tmp()
                rstd, B_rstd = next_tmp()
                S.op("dve", lambda e: e.tensor_scalar(out=mean[:, 0:n], in0=psum_s[:, 0:n], scalar1=1.0 / D, scalar2=None, op0=ALU.mult),
                     reads=[B_pss], writes=[B_mean])
                S.op("dve", lambda e: e.tensor_tensor(out=rstd[:, 0:n], in0=mean[:, 0:n], in1=mean[:, 0:n], op=ALU.mult), reads=[B_mean], writes=[B_rstd])
                S.op("dve", lambda e: e.scalar_tensor_tensor(out=rstd[:, 0:n], in0=psum_q[:, 0:n], scalar=1.0 / D, in1=rstd[:, 0:n],
                                                             op0=ALU.mult, op1=ALU.subtract), reads=[B_psq, B_rstd], writes=[B_rstd])
                S.op("dve", lambda e: e.tensor_scalar(out=rstd[:, 0:n], in0=rstd[:, 0:n], scalar1=LN_EPS, scalar2=None, op0=ALU.add),
                     reads=[B_rstd], writes=[B_rstd])
                S.op("act", lambda e: e.activation(out=rstd[:, 0:n], in_=rstd[:, 0:n], func=AF.Sqrt), reads=[B_rstd], writes=[B_rstd])
                S.op("dve", lambda e: e.reciprocal(out=rstd[:, 0:n], in_=rstd[:, 0:n]), reads=[B_rstd], writes=[B_rstd])
                for j in range(KC):
                    S.op("dve", lambda e, j=j: e.tensor_tensor(out=h0f[:, j, 0:n], in0=h0f[:, j, 0:n], in1=mean[:, 0:n], op=ALU.subtract),
                         reads=[B_h0f[j], B_mean], writes=[B_h0f[j]])
                    S.op("dve", lambda e, j=j: e.tensor_tensor(out=h0f[:, j, 0:n], in0=h0f[:, j, 0:n], in1=rstd[:, 0:n], op=ALU.mult),
                         reads=[B_h0f[j], B_rstd], writes=[B_h0f[j]])
                    S.op("act", lambda e, j=j: e.activation(out=h0f[:, j, 0:n], in_=h0f[:, j, 0:n], func=AF.Identity, bias=fcol("l1b", j),
                                                            scale=fcol("l1g", j)), reads=[B_h0f[j], B_fv], writes=[B_h0f[j]])

                cut(9)
                for sub in range(nsub):
                    ti = tile0 + sub
                    xt, B_x = xst[sub % 2], B_xst[sub % 2]
                    hb, B_hb = hbst[sub % 2], B_hbst[sub % 2]
                    for q in range(4):
                        ps, B_p = next_ps()
                        items = [(ps[:, kk * 128:(kk + 1) * 128], h0f[:, 4 * q + kk, sub * 128:(sub + 1) * 128], ident[:]) for kk in range(4)]
                        tr_group(B_p, items, reads=[B_h0f[4 * q + kk] for kk in range(4)] + [B_const])
                        A5 = int(_os.environ.get("A5MODE", "9"))
                        if A5 >= 2:
                            S.op("act", lambda e, xt=xt, ps=ps, q=q: e.copy(out=xt[:, q * 512:(q + 1) * 512], in_=ps[:, :]), reads=[B_p], writes=[B_x])
                        if A5 >= 3:
                            S.op("dve", lambda e, hb=hb, xt=xt, q=q: e.tensor_copy(out=hb[:, q * 512:(q + 1) * 512], in_=xt[:, q * 512:(q + 1) * 512]),
                                 reads=[B_x], writes=[B_hb])
                    if A5 >= 4:
                        S.dma("sp", lambda e, xt=xt, ti=ti: e.dma_start(out=h1f_d[ti * 128:(ti + 1) * 128, :], in_=xt[:, :]), reads=[B_x])
                    if A5 >= 5:
                        S.dma("sp", lambda e, hb=hb, ti=ti: e.dma_start(out=h1b_d[ti * 128:(ti + 1) * 128, :], in_=hb[:, :]), reads=[B_hb])
                    if debug and A5 >= 6:
                        S.dma("sp", lambda e, xt=xt, ti=ti: e.dma_start(out=dbg["h1"][ti * 128:(ti + 1) * 128, :], in_=xt[:, :]), reads=[B_x])
                    cut(10)
                    pl_, B_plg = next_ps()
                    mm_group(pl_[:, 0:NE], B_plg, [(h0f[:, k, sub * 128:(sub + 1) * 128], rw[:, k, :]) for k in range(KC)],
                             reads=B_h0f + [B_rw])
                    S.op("dve", lambda e, pl_=pl_, sub=sub: e.tensor_tensor(out=lgall[:, sub, :], in0=pl_[:, 0:NE], in1=rb[:], op=ALU.add),
                         reads=[B_plg, B_rw], writes=[B_lg[sub]])

                def routing_tail(sub):
                  if True:
                    ti = tile0 + sub
                    lg = lgall[:, sub, :]
                    m8 = rt[:, 32:40]
                    i8f = rt[:, 40:48]
                    oh = rt[:, 64:64 + 4 * NE].rearrange("p (k e) -> p k e", k=4)
                    msk = rt[:, 192:192 + NE]
                    ex = rt[:, 224:228]
                    sm = rt[:, 228:229]
                    nm = rt[:, 229:230]
                    pos = rt[:, 256:256 + NE]
                    prod = rt[:, 288:288 + 4 * NE].rearrange("p (k e) -> p k e", k=4)
                    pk = rt[:, 416:420]
                    rowf = rt[:, 420:424]
                    R = [B_rt]
                    S.op("dve", lambda e, lg=lg: e.max(out=m8, in_=lg), reads=R + [B_lg[sub]], writes=R)
                    S.op("dve", lambda e, lg=lg: e.max_index(out=rti[:], in_max=m8, in_values=lg), reads=R + [B_lg[sub]], writes=R)
                    S.op("dve", lambda e: e.tensor_copy(out=i8f, in_=rti[:]), reads=R, writes=R)
                    for k in range(TOPK):
                        S.op("dve", lambda e, k=k: e.tensor_scalar(out=oh[:, k, :], in0=iota_f[:, 0:NE], scalar1=i8f[:, k:k + 1], scalar2=None,
                                                                   op0=ALU.is_equal), reads=R + [B_const], writes=R)
                    S.op("dve", lambda e: e.tensor_tensor(out=msk, in0=oh[:, 0, :], in1=oh[:, 1, :], op=ALU.add), reads=R, writes=R)
                    S.op("dve", lambda e: e.tensor_tensor(out=msk, in0=msk, in1=oh[:, 2, :], op=ALU.add), reads=R, writes=R)
                    S.op("dve", lambda e: e.tensor_tensor(out=msk, in0=msk, in1=oh[:, 3, :], op=ALU.add), reads=R, writes=R)
                    S.op("dve", lambda e, ti=ti: e.tensor_copy(out=maskb[:, ti, :], in_=msk), reads=R, writes=[B_maskb[ti]])
                    S.op("dve", lambda e: e.tensor_scalar(out=nm, in0=m8[:, 0:1], scalar1=-1.0, scalar2=None, op0=ALU.mult), reads=R, writes=R)
                    S.op("act", lambda e: e.activation(out=ex, in_=m8[:, 0:4], func=AF.Exp, bias=nm, scale=1.0), reads=R, writes=R)
                    S.op("dve", lambda e: e.reduce_sum(out=sm, in_=ex, axis=AX.X), reads=R, writes=R)
                    S.op("dve", lambda e: e.reciprocal(out=sm, in_=sm), reads=R, writes=R)
                    S.op("dve", lambda e, ti=ti: e.tensor_scalar(out=w4[:, ti, :], in0=ex, scalar1=sm, scalar2=None, op0=ALU.mult), reads=R, writes=[B_w4[ti]])
                    pp, B_pp = next_ps()
                    pairs = [(triu_b[:], maskb[:, ti, :])] + [(ones_b[:], maskb[:, t2, :]) for t2 in range(ti)]
                    mm_group(pp[:, 0:NE], B_pp, pairs, reads=[B_const] + B_maskb[0:ti + 1])
                    S.op("dve", lambda e, pp=pp: e.tensor_copy(out=pos, in_=pp[:, 0:NE]), reads=[B_pp], writes=R)
                    S.op("dve", lambda e, ti=ti: e.scalar_tensor_tensor(out=posm[:, ti, :], in0=pos, scalar=1.0, in1=msk, op0=ALU.add, op1=ALU.mult),
                         reads=R, writes=[B_posm[ti]])
                    S.op("dve", lambda e, ti=ti: e.tensor_scalar(out=posm[:, ti, :], in0=posm[:, ti, :], scalar1=-1.0, scalar2=None, op0=ALU.add),
                         reads=[B_posm[ti]], writes=[B_posm[ti]])
                    for k in range(TOPK):
                        S.op("dve", lambda e, k=k: e.tensor_tensor(out=prod[:, k, :], in0=oh[:, k, :], in1=pos, op=ALU.mult), reads=R, writes=R)
                    S.op("dve", lambda e: e.tensor_reduce(out=pk, in_=prod, axis=AX.X, op=ALU.add), reads=R, writes=R)
                    S.op("dve", lambda e: e.scalar_tensor_tensor(out=rowf, in0=i8f[:, 0:4], scalar=float(CAP), in1=pk, op0=ALU.mult, op1=ALU.add),
                         reads=R, writes=R)
                    S.op("dve", lambda e, ti=ti: e.tensor_copy(out=rows[:, ti, :], in_=rowf), reads=R, writes=[B_rows[ti]])
                    if debug:
                        S.dma("sp", lambda e, ti=ti: e.dma_start(out=dbg["route"][:, ti * 8:ti * 8 + 4], in_=rowf), reads=R)
                        S.dma("sp", lambda e, ti=ti: e.dma_start(out=dbg["route"][:, ti * 8 + 4:ti * 8 + 8], in_=w4[:, ti, :]), reads=[B_w4[ti]])
                return [(lambda sub=sub: routing_tail(sub)) for sub in range(nsub)]

            try:
                cut(1)
                tail = None
                for b in range(NB):
                    tail = mixer_block(512, HALO + 512 * b, False, 4 * b, tail, fold=(b == 0))
                for t_ in tail:
                    t_()
            except _Stop:
                upto = "A"

        if upto in ("B", "C"):
            arena_off[0] = 0

            def sbB(name, shape, dt):
                return carve(shape, dt)

            B_ringB = make_ring(5, KC * 512)
            xs = sbB("xs", [128, KC, CAP], BF16)
            act = sbB("act", [128, KC, CAP], BF16)
            sel = sbB("sel", [128, NT, CAP], BF16)
            ost = sbB("ost", [128, NST, D], F32)
            bdn = [sbB(f"bdn{i}", [128, D], BF16) for i in range(2)]
            xg = [sbB(f"xg{i}", [128, NST, D], BF16) for i in range(2)]
            idxu = [sbB(f"idxu{i}", [128, 4], U32) for i in range(2)]
            idxf = sbB("idxf", [128, 16], F32)
            tokcol = sbB("tokcol", [128, NT, 2], BF16)
            ident_b = sbB("ident_b", [128, 128], BF16)
            NTB = 8
            tb = [sbB(f"tb{i}", [128, CAP], F32) for i in range(NTB)]
            B_xs = [Buf(f"xs{k}") for k in range(KC)]
            B_act = [Buf(f"act{k}") for k in range(KC)]
            B_sel = [Buf(f"sel{i}") for i in range(NT)]
            B_ost = [Buf(f"ost{i}") for i in range(NST)]
            B_bdn = [Buf("bdn0"), Buf("bdn1")]
            B_xg = [[Buf(f"xg{i}_{st}") for st in range(NST)] for i in range(2)]
            B_idxu = [Buf("idxu0"), Buf("idxu1")]
            B_idxf, B_tok = Buf("idxf"), Buf("tokcol")
            B_tb = [Buf(f"tb{i}") for i in range(NTB)]
            tb_rr = [0]

            def next_tb():
                i = tb_rr[0]
                tb_rr[0] = (i + 1) % NTB
                return tb[i], B_tb[i]

            phase_barrier(B_ringB + B_xs + B_act + B_sel + B_ost + B_bdn + B_xg[0] + B_xg[1] + B_idxu + [B_idxf, B_tok] + B_tb)

            for ti in range(NT):
                S.op("dve", lambda e, ti=ti: e.tensor_copy(out=tokcol[:, ti, 0:1], in_=pidx_f[:, 0:1]), reads=[B_const], writes=[B_tok])
                S.op("dve", lambda e, ti=ti: e.memset(tokcol[:, ti, 1:2], float(ti)), writes=[B_tok])
            S.op("dve", lambda e: e.tensor_copy(out=ident_b, in_=ident[:]), reads=[B_const], writes=[B_tok])

            def prep_sel(ex_):
                for ti in range(NT):
                    S.op("dve", lambda e, ti=ti, ex_=ex_: e.tensor_scalar(out=sel[:, ti, :], in0=iota_f[:, 0:CAP], scalar1=posm[:, ti, ex_:ex_ + 1],
                                                                          scalar2=None, op0=ALU.is_equal),
                         reads=[B_posm[ti], B_const], writes=[B_sel[ti]])

            def prep_idx(ex_):
                bi = ex_ % 2
                for st in range(NST):
                    pi, B_pi = next_ps()
                    mm_group(pi[:, 0:2], B_pi, [(sel[:, ti, st * 128:(st + 1) * 128], tokcol[:, ti, :]) for ti in range(NT)],
                             reads=B_sel + [B_tok])
                    S.op("dve", lambda e, pi=pi, st=st: e.tensor_copy(out=idxf[:, 4 + 2 * st:6 + 2 * st], in_=pi[:, 0:2]), reads=[B_pi], writes=[B_idxf])
                    S.op("dve", lambda e, st=st: e.scalar_tensor_tensor(out=idxf[:, st:st + 1], in0=idxf[:, 5 + 2 * st:6 + 2 * st], scalar=128.0,
                                                                        in1=idxf[:, 4 + 2 * st:5 + 2 * st], op0=ALU.mult, op1=ALU.add),
                         reads=[B_idxf], writes=[B_idxf])
                S.op("dve", lambda e, bi=bi: e.tensor_copy(out=idxu[bi][:, 0:NST], in_=idxf[:, 0:NST]), reads=[B_idxf], writes=[B_idxu[bi]])

            def prep_gather(ex_):
                bi = ex_ % 2
                for st in range(NST):
                    S.dma("pool", lambda e, bi=bi, st=st: e.indirect_dma_start(
                        out=xg[bi][:, st, :], out_offset=None, in_=h1b_d[:, :],
                        in_offset=bass.IndirectOffsetOnAxis(ap=idxu[bi][:, st:st + 1], axis=0)),
                        reads=[B_idxu[bi]], writes=[B_xg[bi][st]])

            prep_sel(0)
            prep_idx(0)
            prep_gather(0)
            for ex_ in range(NE):
                r0 = ex_ * D
                bi = ex_ % 2
                bd, B_bd = bdn[ex_ % 2], B_bdn[ex_ % 2]
                S.dma("pool", lambda e, bd=bd, ex_=ex_: e.dma_start(out=bd[0:1, :], in_=b_dn[ex_:ex_ + 1, :]), writes=[B_bd])
                if ex_ + 1 < NE:
                    prep_sel(ex_ + 1)
                for m in range(KC):
                    ps, B_p = next_ps()
                    psb = ps[:, 0:CAP // 2].bitcast(BF16)
                    items = [(psb[:, st * 128:(st + 1) * 128], xg[bi][:, st, m * 128:(m + 1) * 128], ident_b) for st in range(NST)]
                    tr_group(B_p, items, reads=B_xg[bi] + [B_tok])
                    S.op("act", lambda e, psb=psb, m=m: e.copy(out=xs[:, m, :], in_=psb), reads=[B_p], writes=[B_xs[m]])
                for q in range(4):
                    if q == 1 and ex_ + 1 < NE:
                        prep_idx(ex_ + 1)
                    if q == 2 and ex_ + 1 < NE:
                        prep_gather(ex_ + 1)
                    wg = load_w(w_gu[r0:r0 + D, 512 * q:512 * q + 512], ncols=512)
                    wl = load_w(w_gu[r0:r0 + D, D + 512 * q:D + 512 * q + 512], ncols=512)
                    for kk in range(4):
                        j = 4 * q + kk
                        pg, B_pg = next_ps()
                        mm_group(pg[:, 0:CAP], B_pg, [(wg[0][:, k, kk * 128:(kk + 1) * 128], xs[:, k, :]) for k in range(KC)], reads=[wg[1]] + B_xs)
                        pli, B_pli = next_ps()
                        mm_group(pli[:, 0:CAP], B_pli, [(wl[0][:, k, kk * 128:(kk + 1) * 128], xs[:, k, :]) for k in range(KC)], reads=[wl[1]] + B_xs)
                        gt, B_gt = next_tb()
                        sg, B_sg = next_tb()
                        l1, B_l1 = next_tb()
                        cb = FO["bg"] + ex_ * 16 + j
                        cl = FO["bl"] + ex_ * 16 + j
                        S.op("dve", lambda e, gt=gt, pg=pg, cb=cb: e.tensor_scalar(out=gt[:], in0=pg[:, 0:CAP], scalar1=fv[:, cb:cb + 1], scalar2=LIMIT,
                                                                                  op0=ALU.add, op1=ALU.min), reads=[B_pg, B_fv], writes=[B_gt])
                        S.op("act", lambda e, sg=sg, gt=gt: e.activation(out=sg[:], in_=gt[:], func=AF.Sigmoid, scale=SW_ALPHA), reads=[B_gt], writes=[B_sg])
                        S.op("dve", lambda e, l1=l1, pli=pli, cl=cl: e.tensor_scalar(out=l1[:], in0=pli[:, 0:CAP], scalar1=fv[:, cl:cl + 1], scalar2=LIMIT + 1.0,
                                                                                    op0=ALU.add, op1=ALU.min), reads=[B_pli, B_fv], writes=[B_l1])
                        S.op("dve", lambda e, gt=gt, sg=sg: e.tensor_tensor(out=gt[:], in0=gt[:], in1=sg[:], op=ALU.mult), reads=[B_gt, B_sg], writes=[B_gt])
                        S.op("dve", lambda e, l1=l1, gt=gt, j=j: e.scalar_tensor_tensor(out=act[:, j, :], in0=l1[:], scalar=1.0 - LIMIT, in1=gt[:],
                                                                                        op0=ALU.max, op1=ALU.mult), reads=[B_l1, B_gt], writes=[B_act[j]])
                for nb in range(4):
                    wd = load_w(w_dn[r0:r0 + D, 512 * nb:512 * nb + 512], ncols=512)
                    for st in range(NST):
                        po, B_po = next_ps()
                        pairs = [(act[:, j, st * 128:(st + 1) * 128], wd[0][:, j, :]) for j in range(KC)]
                        pairs.append((ones_b[0:1, :], bd[0:1, 512 * nb:512 * nb + 512]))
                        mm_group(po[:, 0:512], B_po, pairs, reads=[wd[1], B_bd, B_const] + B_act)
                        if (nb + st) % 2:
                            S.op("act", lambda e, po=po, st=st, nb=nb: e.copy(out=ost[:, st, 512 * nb:512 * nb + 512], in_=po[:, 0:512]),
                                 reads=[B_po], writes=[B_ost[st]])
                        else:
                            S.op("dve", lambda e, po=po, st=st, nb=nb: e.tensor_copy(out=ost[:, st, 512 * nb:512 * nb + 512], in_=po[:, 0:512]),
                                 reads=[B_po], writes=[B_ost[st]])
                for st in range(NST):
                    S.dma("sp", lambda e, st=st, ex_=ex_: e.dma_start(out=contrib[ex_ * CAP + st * 128:ex_ * CAP + (st + 1) * 128, :], in_=ost[:, st, :]),
                          reads=[B_ost[st]], writes=[])

        if upto == "C":
            arena_off[0] = 0

            def sbC(name, shape, dt):
                return carve(shape, dt)

            NG = 16
            NHH = 4
            G = [sbC(f"G{i}", [128, D], F32) for i in range(NG)]
            hh = [sbC(f"hh{i}", [128, D], F32) for i in range(NHH)]
            lg2 = sbC("lg2", [128, 2 * D], F32)
            stat2 = sbC("stat2", [128, 4, 6], F32)
            mv2 = sbC("mv2", [128, 8], F32)
            B_G = [Buf(f"G{i}") for i in range(NG)]
            B_hh = [Buf(f"hh{i}") for i in range(NHH)]
            B_lg2, B_stat2, B_mv2 = Buf("lg2"), Buf("stat2"), Buf("mv2")
            phase_barrier(B_G + B_hh + [B_lg2, B_stat2, B_mv2])
            S.dma("sp", lambda e: e.dma_start(out=lg2[:], in_=rvec[:, 0:2 * D]), writes=[B_lg2])
            def stage1(ti):
                h, B_h = hh[ti % NHH], B_hh[ti % NHH]
                S.dma("sp", lambda e, h=h, ti=ti: e.dma_start(out=h[:], in_=h1f_d[ti * 128:(ti + 1) * 128, :]), writes=[B_h])
                S.op("act", lambda e, h=h: e.mul(out=h[:], in_=h[:], mul=ALPHA), reads=[B_h], writes=[B_h])
                for k in range(TOPK):
                    gi = (ti * TOPK + k) % NG
                    S.dma("pool", lambda e, gi=gi, ti=ti, k=k: e.indirect_dma_start(
                        out=G[gi][:], out_offset=None, in_=contrib[:, :],
                        in_offset=bass.IndirectOffsetOnAxis(ap=rows[:, ti, k:k + 1], axis=0)),
                        reads=[B_rows[ti]], writes=[B_G[gi]])
                    S.op("dve", lambda e, gi=gi, h=h, ti=ti, k=k: e.scalar_tensor_tensor(out=h[:], in0=G[gi][:], scalar=w4[:, ti, k:k + 1], in1=h[:],
                                                                                         op0=ALU.mult, op1=ALU.add),
                         reads=[B_G[gi], B_h, B_w4[ti]], writes=[B_h])
                for c in range(4):
                    S.op("dve", lambda e, c=c, h=h: e.bn_stats(out=stat2[:, c, :], in_=h[:, c * 512:(c + 1) * 512]), reads=[B_h], writes=[B_stat2])
                S.op("dve", lambda e: e.bn_aggr(out=mv2[:, 0:2], in_=stat2[:, :, :].rearrange("p a b -> p (a b)")), reads=[B_stat2], writes=[B_mv2])
                S.op("dve", lambda e: e.tensor_scalar(out=mv2[:, 2:3], in0=mv2[:, 1:2], scalar1=LN_EPS, scalar2=None, op0=ALU.add),
                     reads=[B_mv2], writes=[B_mv2])
                S.op("act", lambda e: e.activation(out=mv2[:, 2:3], in_=mv2[:, 2:3], func=AF.Sqrt), reads=[B_mv2], writes=[B_mv2])
                S.op("dve", lambda e: e.reciprocal(out=mv2[:, 2:3], in_=mv2[:, 2:3]), reads=[B_mv2], writes=[B_mv2])
                S.op("dve", lambda e: e.tensor_scalar(out=mv2[:, 3:4], in0=mv2[:, 0:1], scalar1=mv2[:, 2:3], scalar2=-1.0, op0=ALU.mult, op1=ALU.mult),
                     reads=[B_mv2], writes=[B_mv2])
                S.op("act", lambda e, h=h: e.activation(out=h[:], in_=h[:], func=AF.Identity, bias=mv2[:, 3:4], scale=mv2[:, 2:3]),
                     reads=[B_mv2, B_h], writes=[B_h])

            def stage2(ti):
                h, B_h = hh[ti % NHH], B_hh[ti % NHH]
                S.op("dve", lambda e, h=h: e.tensor_tensor(out=h[:], in0=h[:], in1=lg2[:, 0:D], op=ALU.mult), reads=[B_h, B_lg2], writes=[B_h])
                S.op("dve", lambda e, h=h: e.tensor_tensor(out=h[:], in0=h[:], in1=lg2[:, D:2 * D], op=ALU.add), reads=[B_h, B_lg2], writes=[B_h])
                S.dma("sp", lambda e, h=h, ti=ti: e.dma_start(out=out[ti * 128:(ti + 1) * 128, :], in_=h[:]), reads=[B_h])

            stage1(0)
            for ti in range(NT):
                if ti + 1 < NT:
                    stage1(ti + 1)
                stage2(ti)

        all_dma = [(S.dsem[q][i], S.dcnt[q][i]) for q in ("sp", "pool") for i in range(S.nds) if S.dcnt[q][i] > 0]
        with nc.Block() as block:
            @block.tensor
            def _(e):
                S.emit("pe", e)

            @block.scalar
            def _(e):
                S.emit("act", e)

            @block.vector
            def _(e):
                S.emit("dve", e)

            @block.gpsimd
            def _(e):
                S.emit("pool", e)

            @block.sync
            def _(e):
                S.emit("sp", e, final_waits=all_dma)
    return nc


def _fm(v):
    v = np.asarray(v, np.float32)
    return np.ascontiguousarray(v.reshape(-1, 128).T)


def make_core_inputs(c, T, NE, x, meta_tokens, ln_in_g, ln_in_b, w_in, conv_w, w_a_out, pool_w, pool_scale, w_o,
                     ln1_g, ln1_b, router_w, router_b, w_gate_up, b_gate_up, w_down, b_down, ln2_g, ln2_b, shared):
    halves = x.shape[1] // T
    b, h = divmod(c, halves)
    if h == 0:
        xin = np.concatenate([meta_tokens.astype(np.float32), x[b, 0:T]], axis=0)
    else:
        xin = x[b, h * T - HALO:(h + 1) * T]
    d = dict(shared)
    d["xin"] = np.ascontiguousarray(xin, dtype=np.float32)
    return d


def make_shared(NE, ln_in_g, ln_in_b, w_in, conv_w, w_a_out, pool_w, pool_scale, w_o, ln1_g, ln1_b, router_w, router_b,
                w_gate_up, b_gate_up, w_down, b_down, ln2_g, ln2_b):
    cols = [_fm(ln_in_g), _fm(ln_in_b), _fm(conv_w[0, 0]), _fm(conv_w[0, 1]), _fm(conv_w[0, 2]), _fm(pool_scale[0]),
            _fm(ln1_g[0]), _fm(ln1_b[0])]
    bgu = np.asarray(b_gate_up[0], np.float32)
    bg = np.concatenate([_fm(bgu[e, :D]) for e in range(NE)], axis=1)
    bl = np.concatenate([_fm(bgu[e, D:]) for e in range(NE)], axis=1)
    fvec = np.ascontiguousarray(np.concatenate(cols + [bg, bl], axis=1), dtype=np.float32)
    rrow = np.concatenate([np.asarray(ln2_g[0], np.float32), np.asarray(ln2_b[0], np.float32), np.asarray(router_b[0], np.float32)])
    rvec = np.ascontiguousarray(np.broadcast_to(rrow[None, :], (128, rrow.shape[0])), dtype=np.float32)
    return {
        "w_in": np.ascontiguousarray(w_in[0], dtype=np.float32),
        "w_a": np.ascontiguousarray(w_a_out[0], dtype=np.float32),
        "w_o": np.ascontiguousarray(w_o[0], dtype=np.float32),
        "pool_w": np.ascontiguousarray(np.asarray(pool_w[0], np.float32).reshape(1024, 512)),
        "router_w": np.ascontiguousarray(router_w[0], dtype=np.float32),
        "w_gu": np.ascontiguousarray(np.asarray(w_gate_up[0], np.float32).reshape(NE * D, 2 * D)),
        "w_dn": np.ascontiguousarray(np.asarray(w_down[0], np.float32).reshape(NE * D, D)),
        "b_dn": np.ascontiguousarray(b_down[0], dtype=np.float32),
        "fvec": fvec,
        "rvec": rvec,
    }


def kernel(**inputs):
    inputs = {k: np.asarray(v) for k, v in inputs.items()}
    x = inputs["x"]
    B, SEQ, _ = x.shape
    NE = inputs["router_w"].shape[-1]
    n_cores = 8
    T = B * SEQ // n_cores
    CAP = 384
    names = ["ln_in_g", "ln_in_b", "w_in", "conv_w", "w_a_out", "pool_w", "pool_scale", "w_o", "ln1_g", "ln1_b", "router_w",
             "router_b", "w_gate_up", "b_gate_up", "w_down", "b_down", "ln2_g", "ln2_b"]
    shared = make_shared(NE, *[inputs[n] for n in names])
    in_maps = [make_core_inputs(c, T, NE, x, inputs["meta_tokens"], *[inputs[n] for n in names], shared) for c in range(n_cores)]
    nc = build_nc(T, NE, CAP)
    res = run_bass_kernel_spmd(nc, in_maps, core_ids=list(range(n_cores)))
    halves = SEQ // T
    outp = np.empty((B, SEQ, D), np.float32)
    for c in range(n_cores):
        b, h = divmod(c, halves)
        outp[b, h * T:(h + 1) * T] = res.results[c]["out"]
    return outp
```
